# Optimizing a Trainium2 kernel written in Bass

```python
import math
import jax, jax.numpy as jnp
from jax import lax
import numpy as np

D_MODEL = 1024
BATCH = 16
SEQ = 2048
DEPTH = 4

N_MIXERS = 2
N_A = (DEPTH + N_MIXERS - 1) // N_MIXERS
N_B = DEPTH // N_MIXERS

RWKV_HEAD = 64
RWKV_HEADS = D_MODEL // RWKV_HEAD
D_DECAY_LORA = max(32, int(round(1.8 * D_MODEL ** 0.5 / 32)) * 32)
D_AAA_LORA = max(32, int(round(1.8 * D_MODEL ** 0.5 / 32)) * 32)
D_MV_LORA = max(32, int(round(1.3 * D_MODEL ** 0.5 / 32)) * 32)
D_GATE_LORA = max(32, int(round(0.6 * D_MODEL ** 0.8 / 32)) * 32)
GN_EPS = RWKV_HEAD * 1e-5

ATT_HEADS = 16
ATT_HEAD_DIM = D_MODEL // ATT_HEADS
KV_LATENT = 128
IDX_HEADS = 8
IDX_DIM = 64
TOPK_MAX = 256
TOPK_DIV = 4
Q_BLOCK = 128
DSA_PROJ = ATT_HEADS * ATT_HEAD_DIM + KV_LATENT + IDX_HEADS * IDX_DIM + IDX_DIM + IDX_HEADS

REL_BUCKETS = 32
REL_MAX_DIST = 128

FFN_HIDDEN = -(-8 * D_MODEL // (3 * 256)) * 256

DEEPNORM_ALPHA = (2 * DEPTH) ** 0.25
DEEPNORM_BETA = (8 * DEPTH) ** -0.25
LN_EPS = 1e-5

kernel_name = 'hybrid_rwkv7_dsa_deepnorm_adaln'


def _layernorm(x, g, b):
    xf = x.astype(jnp.float32)
    mu = xf.mean(-1, keepdims=True)
    var = jnp.square(xf - mu).mean(-1, keepdims=True)
    y = (xf - mu) * lax.rsqrt(var + LN_EPS) * g.astype(jnp.float32) + b.astype(jnp.float32)
    return y.astype(x.dtype)


def _rmsnorm(x, g):
    xf = x.astype(jnp.float32)
    y = xf * lax.rsqrt(jnp.mean(xf * xf, -1, keepdims=True) + 1e-6) * g.astype(jnp.float32)
    return y.astype(x.dtype)


def _t5_bucket(n):
    n = jnp.maximum(n, 0)
    max_exact = REL_BUCKETS // 2
    nf = jnp.maximum(n, 1).astype(jnp.float32)
    large = max_exact + (jnp.log(nf / max_exact) / math.log(REL_MAX_DIST / max_exact)
                         * (REL_BUCKETS - max_exact)).astype(jnp.int32)
    large = jnp.minimum(large, REL_BUCKETS - 1)
    return jnp.where(n < max_exact, n, large)


def _rwkv7_step(S, inp):
    r_t, w_t, k_t, v_t, a_t, b_t = inp
    sa = jnp.einsum('bhij,bhj->bhi', S, a_t)
    S = S * w_t[:, :, None, :] + sa[..., None] * b_t[:, :, None, :] + v_t[..., None] * k_t[:, :, None, :]
    y = jnp.einsum('bhij,bhj->bhi', S, r_t)
    return S, y


def _rwkv7_time_mix(h, v_first, mu, w_rkv, w0, w1, w2, a0, a1, a2, vres,
                    g1, g2, k_k, k_a, r_k, lnx_g, lnx_b, w_out):
    B, T, D = h.shape
    H, N = RWKV_HEADS, RWKV_HEAD
    f32 = jnp.float32
    h_prev = jnp.pad(h, ((0, 0), (1, 0), (0, 0)))[:, :-1]
    xx = h_prev - h
    xs = h[None] + xx[None] * mu[:, None, None, :]
    r, k, v = jnp.einsum('pbtd,pde->pbte', xs[:3], w_rkv)
    xv, xw, xa, xg = xs[2], xs[3], xs[4], xs[5]
    w = -jax.nn.softplus(-(w0 + jnp.tanh(xw @ w1) @ w2)) - 0.5
    if vres is None:
        v_first = v
    else:
        v0, v1, v2 = vres
        v = v + (v_first - v) * jax.nn.sigmoid(v0 + (xv @ v1) @ v2)
    a = jax.nn.sigmoid(a0 + (xa @ a1) @ a2)
    g = jax.nn.sigmoid(xg @ g1) @ g2

    def heads(t):
        return t.reshape(B, T, H, N).astype(f32)

    kk = heads(k * k_k)
    kk = kk / jnp.maximum(jnp.sqrt(jnp.sum(kk * kk, -1, keepdims=True)), 1e-12)
    k = k * (1 + (a - 1) * k_a)
    rh, kh, vh, ah = heads(r), heads(k), heads(v), heads(a)
    decay = jnp.exp(-jnp.exp(heads(w)))

    def tm(t):
        return jnp.swapaxes(t, 0, 1)

    S0 = jnp.zeros((B, H, N, N), f32)
    _, y = lax.scan(_rwkv7_step, S0,
                    (tm(rh), tm(decay), tm(kh), tm(vh), tm(-kk), tm(kk * ah)))
    y = tm(y)
    ym = y.mean(-1, keepdims=True)
    yv = jnp.square(y - ym).mean(-1, keepdims=True)
    yn = ((y - ym) * lax.rsqrt(yv + GN_EPS)).reshape(B, T, D)
    yn = yn * lnx_g.astype(f32) + lnx_b.astype(f32)
    bonus = jnp.sum(rh * kh * r_k.astype(f32), -1, keepdims=True) * vh
    out = (yn + bonus.reshape(B, T, D)).astype(h.dtype) * g
    return out @ w_out, v_first


def _dsa_attention(h, w_in, kv_norm, w_uk, w_uv, w_out, rel_bias):
    B, T, D = h.shape
    H, dh = ATT_HEADS, ATT_HEAD_DIM
    f32 = jnp.float32
    proj = h @ w_in
    c1 = H * dh
    c2 = c1 + KV_LATENT
    c3 = c2 + IDX_HEADS * IDX_DIM
    c4 = c3 + IDX_DIM
    q = proj[..., :c1].reshape(B, T, H, dh)
    ckv = _rmsnorm(proj[..., c1:c2], kv_norm)
    q_idx = proj[..., c2:c3].reshape(B, T, IDX_HEADS, IDX_DIM)
    k_idx = proj[..., c3:c4]
    w_idx = proj[..., c4:] * (IDX_HEADS ** -0.5 * IDX_DIM ** -0.5)

    k_sel = min(TOPK_MAX, T // TOPK_DIV)
    nblk = T // Q_BLOCK

    def to_blocks(t):
        return jnp.swapaxes(t.reshape(B, nblk, Q_BLOCK, *t.shape[2:]), 0, 1)

    pos_blocks = jnp.arange(T, dtype=jnp.int32).reshape(nblk, Q_BLOCK)
    key_pos = jnp.arange(T, dtype=jnp.int32)

    def block(args):
        qb, qib, wib, tpos = args
        s = jnp.einsum('bthd,bsd->bths', qib, k_idx)
        score = jnp.einsum('bth,bths->bts', wib, jax.nn.relu(s))
        causal = key_pos[None, :] <= tpos[:, None]
        score = jnp.where(causal[None], score, -jnp.inf)
        _, idx = lax.top_k(score, k_sel)
        sel = jax.vmap(lambda kv, ix: kv[ix])(ckv, idx)
        q_lat = jnp.einsum('bthd,hdc->bthc', qb, w_uk)
        logits = jnp.einsum('bthc,btkc->bthk', q_lat, sel).astype(f32) * (dh ** -0.5)
        rel = tpos[None, :, None] - idx
        bias = jnp.moveaxis(rel_bias[_t5_bucket(rel)], -1, 2)
        logits = logits + bias.astype(f32)
        logits = jnp.where((rel >= 0)[:, :, None, :], logits, -jnp.inf)
        p = jax.nn.softmax(logits, axis=-1).astype(h.dtype)
        o_lat = jnp.einsum('bthk,btkc->bthc', p, sel)
        o = jnp.einsum('bthc,hcd->bthd', o_lat, w_uv)
        return o.reshape(B, Q_BLOCK, H * dh)

    out = lax.map(block, (to_blocks(q), to_blocks(q_idx), to_blocks(w_idx), pos_blocks))
    out = jnp.swapaxes(out, 0, 1).reshape(B, T, H * dh)
    return out @ w_out


def _swiglu(h, w_in, w_out):
    gate, up = jnp.split(h @ w_in, 2, axis=-1)
    return (jax.nn.silu(gate) * up) @ w_out


def setup_inputs(seed: int = 0) -> dict:
    key = jax.random.key(seed)
    ks = iter(jax.random.split(key, 48))
    f32 = jnp.float32
    D = D_MODEL

    def nrm(shape, scale):
        return jax.random.normal(next(ks), shape, f32) * scale

    inp = {}
    inp['x'] = nrm((BATCH, SEQ, D), 1.0)
    inp['c'] = nrm((BATCH, D), 1.0)
    inp['ada_w'] = nrm((DEPTH, D, 6 * D), 0.5 * D ** -0.5)
    inp['ada_b'] = nrm((DEPTH, 6 * D), 0.02)
    inp['ln_g'] = 1.0 + nrm((DEPTH, 2, D), 0.02)
    inp['ln_b'] = nrm((DEPTH, 2, D), 0.02)
    inp['ffn_w_in'] = nrm((DEPTH, D, 2 * FFN_HIDDEN), D ** -0.5)
    inp['ffn_w_out'] = nrm((DEPTH, FFN_HIDDEN, D), FFN_HIDDEN ** -0.5 * DEEPNORM_BETA)
    inp['rwkv_mu'] = jax.random.uniform(next(ks), (N_A, 6, D), f32)
    inp['rwkv_w_rkv'] = nrm((N_A, 3, D, D), D ** -0.5)
    inp['rwkv_w0'] = jax.random.uniform(next(ks), (N_A, D), f32, -4.0, 1.0)
    inp['rwkv_w1'] = nrm((N_A, D, D_DECAY_LORA), D ** -0.5)
    inp['rwkv_w2'] = nrm((N_A, D_DECAY_LORA, D), 0.1 * D_DECAY_LORA ** -0.5)
    inp['rwkv_a0'] = nrm((N_A, D), 0.5)
    inp['rwkv_a1'] = nrm((N_A, D, D_AAA_LORA), D ** -0.5)
    inp['rwkv_a2'] = nrm((N_A, D_AAA_LORA, D), 0.1 * D_AAA_LORA ** -0.5)
    inp['rwkv_v0'] = nrm((N_A - 1, D), 0.5)
    inp['rwkv_v1'] = nrm((N_A - 1, D, D_MV_LORA), D ** -0.5)
    inp['rwkv_v2'] = nrm((N_A - 1, D_MV_LORA, D), 0.1 * D_MV_LORA ** -0.5)
    inp['rwkv_g1'] = nrm((N_A, D, D_GATE_LORA), D ** -0.5)
    inp['rwkv_g2'] = nrm((N_A, D_GATE_LORA, D), D_GATE_LORA ** -0.5)
    inp['rwkv_k_k'] = 0.85 + nrm((N_A, D), 0.02)
    inp['rwkv_k_a'] = 1.0 + nrm((N_A, D), 0.02)
    inp['rwkv_r_k'] = nrm((N_A, RWKV_HEADS, RWKV_HEAD), 0.1)
    inp['rwkv_lnx_g'] = 1.0 + nrm((N_A, D), 0.02)
    inp['rwkv_lnx_b'] = nrm((N_A, D), 0.02)
    inp['rwkv_w_out'] = nrm((N_A, D, D), D ** -0.5 * DEEPNORM_BETA)
    inp['dsa_w_in'] = nrm((N_B, D, DSA_PROJ), D ** -0.5)
    inp['dsa_kv_norm'] = 1.0 + nrm((N_B, KV_LATENT), 0.02)
    inp['dsa_w_uk'] = nrm((N_B, ATT_HEADS, ATT_HEAD_DIM, KV_LATENT), ATT_HEAD_DIM ** -0.5)
    inp['dsa_w_uv'] = nrm((N_B, ATT_HEADS, KV_LATENT, ATT_HEAD_DIM), KV_LATENT ** -0.5)
    inp['dsa_w_out'] = nrm((N_B, ATT_HEADS * ATT_HEAD_DIM, D),
                           (ATT_HEADS * ATT_HEAD_DIM) ** -0.5 * DEEPNORM_BETA)
    inp['rel_bias'] = nrm((REL_BUCKETS, ATT_HEADS), 0.3)
    return inp


def reference(x, c, ada_w, ada_b, ln_g, ln_b, ffn_w_in, ffn_w_out,
              rwkv_mu, rwkv_w_rkv, rwkv_w0, rwkv_w1, rwkv_w2, rwkv_a0, rwkv_a1, rwkv_a2,
              rwkv_v0, rwkv_v1, rwkv_v2, rwkv_g1, rwkv_g2, rwkv_k_k, rwkv_k_a, rwkv_r_k,
              rwkv_lnx_g, rwkv_lnx_b, rwkv_w_out,
              dsa_w_in, dsa_kv_norm, dsa_w_uk, dsa_w_uv, dsa_w_out, rel_bias):
    cond = jax.nn.silu(c)
    v_first = None
    for i in range(DEPTH):
        mod = cond @ ada_w[i] + ada_b[i]
        sh1, sc1, gt1, sh2, sc2, gt2 = jnp.split(mod[:, None, :], 6, axis=-1)
        hin = x * (1 + sc1) + sh1
        j = i // N_MIXERS
        if i % N_MIXERS == 0:
            vres = None if j == 0 else (rwkv_v0[j - 1], rwkv_v1[j - 1], rwkv_v2[j - 1])
            y, v_first = _rwkv7_time_mix(
                hin, v_first, rwkv_mu[j], rwkv_w_rkv[j], rwkv_w0[j], rwkv_w1[j], rwkv_w2[j],
                rwkv_a0[j], rwkv_a1[j], rwkv_a2[j], vres, rwkv_g1[j], rwkv_g2[j],
                rwkv_k_k[j], rwkv_k_a[j], rwkv_r_k[j], rwkv_lnx_g[j], rwkv_lnx_b[j], rwkv_w_out[j])
        else:
            y = _dsa_attention(hin, dsa_w_in[j], dsa_kv_norm[j], dsa_w_uk[j], dsa_w_uv[j],
                               dsa_w_out[j], rel_bias)
        x = _layernorm(DEEPNORM_ALPHA * x + (1 + gt1) * y, ln_g[i, 0], ln_b[i, 0])
        hin = x * (1 + sc2) + sh2
        y = _swiglu(hin, ffn_w_in[i], ffn_w_out[i])
        x = _layernorm(DEEPNORM_ALPHA * x + (1 + gt2) * y, ln_g[i, 1], ln_b[i, 1])
    return x
```

```python
import contextlib
import math
import numpy as np
import concourse.bass as bass
import concourse.mybir as mybir
from concourse.bass_utils import run_bass_kernel_spmd

F32 = mybir.dt.float32
BF16 = mybir.dt.bfloat16
U8 = mybir.dt.uint8
AF = mybir.ActivationFunctionType
ALU = mybir.AluOpType
AX = mybir.AxisListType

D = 1024
T = 2048
NT = T // 128
DC = D // 128
DEPTH = 4
FH = 2816
ALPHA = (2 * DEPTH) ** 0.25
LN_EPS = 1e-5
GN_EPS = 64 * 1e-5
NCONST = 1024
WDECAY = math.exp(-0.5)
NSEQ = 2
DBG = {"stage": 99, "tiles": NT, "sub": 99}
ENGS = ("pe", "act", "pool", "dve", "sp")
DT_SIZE = {F32: 4, BF16: 2, U8: 1}


class _Rec:
    def __getattr__(self, name):
        def f(*args, **kw):
            return (name, args, kw)
        return f


E = _Rec()


class Ev:
    __slots__ = ("kind", "eng", "seq", "needed", "value")

    def __init__(self, kind, eng, seq):
        self.kind = kind
        self.eng = eng
        self.seq = seq
        self.needed = False
        self.value = None


class Res:
    __slots__ = ("name", "w", "r", "excl")

    def __init__(self, name="", excl=False):
        self.name = name
        self.w = None
        self.r = []
        self.excl = excl


class Prog:
    def __init__(self, nc, n_dma_sems=48):
        self.nc = nc
        self.streams = {e: [] for e in ENGS}
        self.n_dma_sems = n_dma_sems
        self.dma_last = [None] * n_dma_sems
        self.dma_cnt = [0] * n_dma_sems
        self.dma_rr = 0
        self.dma_rr_sw = 0
        self.known = {e: {} for e in ENGS}
        self.last_ev = {e: None for e in ENGS}

    def _collect(self, eng, reads, writes, include_same=False):
        deps = []
        for r in reads:
            if r.w is not None:
                deps.append(r.w)
        for w in writes:
            if w.w is not None:
                deps.append(w.w)
            deps.extend(w.r)
        kn = self.known[eng]
        best = {}
        for d in deps:
            if d.kind == "eng" and d.eng == eng and not include_same:
                continue
            key = (d.kind, d.eng)
            if kn.get(key, -1) >= d.seq:
                continue
            if key not in best or best[key].seq < d.seq:
                best[key] = d
        out = []
        for key, d in best.items():
            kn[key] = d.seq
            d.needed = True
            out.append(d)
        return out

    def op(self, eng, fn, reads=(), writes=()):
        ex = [r for r in reads if r.excl]
        if ex:
            writes = list(writes) + ex
        waits = self._collect(eng, reads, writes)
        if eng != "pe":
            kn = self.known[eng]
            key = ("eng", eng)
            m = None
            for r in reads:
                d = r.w
                if d is not None and d.kind == "eng" and d.eng == eng and kn.get(key, -1) < d.seq:
                    if m is None or m.seq < d.seq:
                        m = d
            if m is not None:
                kn[key] = m.seq
                m.needed = True
                waits.append(m)
        st = self.streams[eng]
        ev = Ev("eng", eng, len(st))
        st.append((fn, waits, ev))
        self.last_ev[eng] = ev
        for r in reads:
            r.r.append(ev)
        for w in writes:
            w.w = ev
            w.r = []
        return ev

    def dma(self, q, out_ap, in_ap, reads=(), writes=(), **kw):
        nh = self.n_dma_sems // 2
        if q == "pool":
            k = nh + self.dma_rr_sw
            self.dma_rr_sw = (self.dma_rr_sw + 1) % (self.n_dma_sems - nh)
        else:
            k = self.dma_rr
            self.dma_rr = (self.dma_rr + 1) % nh
        waits = self._collect(q, reads, writes, include_same=True)
        prev = self.dma_last[k]
        if prev is not None:
            key = ("dma", k)
            if self.known[q].get(key, -1) < prev.seq:
                self.known[q][key] = prev.seq
                waits.append(prev)
        self.dma_cnt[k] += 1
        ev = Ev("dma", k, self.dma_cnt[k])
        ev.needed = True
        ev.value = 16 * self.dma_cnt[k]
        self.dma_last[k] = ev

        def fn(e, out_ap=out_ap, in_ap=in_ap, kw=kw):
            return e.dma_start(out=out_ap, in_=in_ap, **kw)
        self.streams[q].append((fn, waits, ev))
        for r in reads:
            r.r.append(ev)
        for w in writes:
            w.w = ev
            w.r = []
        return ev

    def barrier(self):
        evs = [self.last_ev[e] for e in ENGS if self.last_ev[e] is not None]
        evs += [d for d in self.dma_last if d is not None]
        for e in ENGS:
            waits = []
            kn = self.known[e]
            for d in evs:
                if d.kind == "eng" and d.eng == e:
                    continue
                key = (d.kind, d.eng)
                if kn.get(key, -1) >= d.seq:
                    continue
                kn[key] = d.seq
                d.needed = True
                waits.append(d)
            if waits:
                self.streams[e].append((None, waits, None))

    def emit(self):
        nc = self.nc
        self.barrier()
        for e in ENGS:
            c = 0
            for fn, waits, ev in self.streams[e]:
                if ev is not None and ev.kind == "eng" and ev.needed:
                    c += 1
                    ev.value = c
        with contextlib.ExitStack() as es:
            esem = {e: es.enter_context(nc.semaphore("s_" + e)) for e in ENGS}
            dsem = [es.enter_context(nc.semaphore("d_%d" % k)) for k in range(self.n_dma_sems)]

            def semof(ev):
                return esem[ev.eng] if ev.kind == "eng" else dsem[ev.eng]

            block = es.enter_context(nc.Block())

            def make(ename):
                def body(eh):
                    for fn, waits, ev in self.streams[ename]:
                        for w in waits:
                            eh.wait_ge(semof(w), w.value)
                        if fn is None:
                            continue
                        if isinstance(fn, tuple):
                            ins = getattr(eh, fn[0])(*fn[1], **fn[2])
                        else:
                            ins = fn(eh)
                        if ev.kind == "dma":
                            ins.then_inc(dsem[ev.eng], 16)
                        elif ev.needed:
                            ins.then_inc(esem[ename], 1)
                return body

            block.tensor(make("pe"))
            block.scalar(make("act"))
            block.gpsimd(make("pool"))
            block.vector(make("dve"))
            block.sync(make("sp"))

    def stats(self):
        return {e: len(self.streams[e]) for e in ENGS}


class Tile:
    def __init__(self, ap, name=""):
        self.ap = ap
        self.res = Res(name)
        self.sub = {}

    def r(self, key=None):
        if key is None:
            return self.res
        if key not in self.sub:
            self.sub[key] = Res()
        return self.sub[key]


class Builder:
    def __init__(self, plan, debug=False):
        self.plan = plan
        self.nc = nc = bass.Bass("TRN2", target_bir_lowering=False)
        self.P = Prog(nc)
        self.es = contextlib.ExitStack()
        self.ARENA = 206 * 1024
        self.arena = self.es.enter_context(nc.sbuf_tensor("arena", [128, self.ARENA], U8))
        self.psum = self.es.enter_context(nc.psum_tensor("psum", [128, 8, 512], F32))
        self.off = 0
        self.persist_off = 0
        self.rr = {"ev": 0}
        self.psb = [Res("bank%d" % i, excl=True) for i in range(8)]
        self.moddram_res = Res("moddram")
        self.outres = Res("out")
        self.xres_ = [[Res() for _ in range(NT)] for _ in range(NSEQ)]
        self.vf_res = [[Res() for _ in range(NT)] for _ in range(NSEQ)]
        self.og_res = [[Res() for _ in range(NT)] for _ in range(NSEQ)]
        self.decl_io()

    def sb(self, shape, dt, name=""):
        n = int(np.prod(shape[1:])) * DT_SIZE[dt]
        n_al = (n + 63) // 64 * 64
        assert self.off + n_al <= self.ARENA, ("SBUF arena overflow", name, self.off, n_al)
        v = self.arena[0:shape[0], self.off:self.off + n].bitcast(dt)
        if not hasattr(self, 'alloc_log'):
            self.alloc_log = []
        self.alloc_log.append((name, self.off, n, dt, list(shape)))
        self.off += n_al
        if len(shape) == 3:
            v = v.rearrange("p (a b) -> p a b", b=shape[2])
        elif len(shape) == 4:
            v = v.rearrange("p (a b c) -> p a b c", b=shape[2], c=shape[3])
        elif len(shape) == 5:
            v = v.rearrange("p (a b c d) -> p a b c d", b=shape[2], c=shape[3], d=shape[4])
        return Tile(v, name)

    def phase_reset(self):
        self.P.barrier()
        self.off = self.persist_off

    def bank(self, i, dt=F32):
        v = self.psum[:, i, :]
        if dt == BF16:
            v = v.bitcast(BF16)
        return v

    def dram_in(self, name, shape, dt=F32):
        return self.nc.dram_tensor(name, list(shape), dt, kind="ExternalInput").ap()

    def dram_tmp(self, name, shape, dt=F32):
        return self.nc.dram_tensor(name, list(shape), dt, kind="Internal").ap()

    def decl_io(self):
        di = self.dram_in
        self.x_in = di("x", [NSEQ, T, D])
        self.c_col = di("c_col", [NSEQ, 128, DC])
        self.ada_w = di("ada_w", [DEPTH, D, 6 * D])
        self.ada_b = di("ada_b", [DEPTH, 6 * D])
        self.ln_g = di("ln_g", [DEPTH, 2, D])
        self.ln_b = di("ln_b", [DEPTH, 2, D])
        self.ffn_w_in = di("ffn_w_in", [DEPTH, D, 2 * FH])
        self.ffn_w_out = di("ffn_w_out", [DEPTH, FH, D])
        self.ident_f = di("ident_f", [128, 128])
        self.consts = di("consts", [128, NCONST])
        kinds = set(k for k, _ in self.plan)
        if "dsa" in kinds:
            self.dsa_w_in = di("dsa_w_in", [2, D, 1736])
            self.dsa_kv_norm = di("dsa_kv_norm", [2, 128])
            self.dsa_w_uk = di("dsa_w_uk", [2, 16, 64, 128])
            self.dsa_w_uv = di("dsa_w_uv", [2, 16, 128, 64])
            self.dsa_w_out = di("dsa_w_out", [2, D, D])
            self.dsa_caus = di("dsa_caus", [128, 4, 512])
            self.dsa_band = di("dsa_band", [128, 16, 256])
            self.dsa_c31 = di("dsa_c31", [128, 16])
            if "rwkv" not in kinds:
                self.ogdram = (self.nc.dram_tensor("ogdram", [NSEQ, T, D], BF16, kind="ExternalOutput").ap() if DBG.get("dump") else self.dram_tmp("ogdram", [NSEQ, T, D], BF16))
        if "rwkv" in kinds:
            self.rw_mu = di("rw_mu", [2, 128, 6, DC])
            self.rw_wrkv = di("rwkv_w_rkv", [2, 3, D, D])
            self.rw_w1 = di("rwkv_w1", [2, D, 64])
            self.rw_w2a = di("rw_w2a", [2, 65, D])
            self.rw_a1 = di("rwkv_a1", [2, D, 64])
            self.rw_a2a = di("rw_a2a", [2, 65, D])
            self.rw_v1 = di("rwkv_v1", [1, D, 32])
            self.rw_v2a = di("rw_v2a", [1, 33, D])
            self.rw_g1 = di("rwkv_g1", [2, D, 160])
            self.rw_g2 = di("rwkv_g2", [2, 160, D])
            self.rw_vec = di("rw_vec", [2, 5, D])
            self.rw_wout = di("rwkv_w_out", [2, D, D])
            self.vfirst = self.dram_tmp("vfirst", [NSEQ, T, D])
            self.ogdram = (self.nc.dram_tensor("ogdram", [NSEQ, T, D], BF16, kind="ExternalOutput").ap() if DBG.get("dump") else self.dram_tmp("ogdram", [NSEQ, T, D], BF16))
        self.out = self.nc.dram_tensor("out", [NSEQ, T, D], F32, kind="ExternalOutput").ap()
        self.xbuf = self.dram_tmp("xbuf", [NSEQ, T, D])
        self.moddram = self.dram_tmp("moddram", [DEPTH, NSEQ, 2, D])

    def ev_eng(self):
        self.rr["ev"] ^= 1
        return "dve" if self.rr["ev"] else "act"

    def prologue(self):
        P = self.P
        self.ident = self.sb([128, 128], F32, "ident")
        P.dma("sp", self.ident.ap, self.ident_f, writes=[self.ident.res])
        self.identb = self.sb([128, 128], BF16, "identb")
        P.op("dve", E.tensor_copy(self.identb.ap, self.ident.ap),
             reads=[self.ident.res], writes=[self.identb.res])
        self.modcol = self.sb([128, DEPTH * NSEQ * 4, DC], F32, "modcol")
        self.persist_off = self.off
        ccol = self.sb([128, NSEQ, DC], F32, "ccol")
        cond = self.sb([128, DC, NSEQ], F32, "cond")
        P.dma("sp", ccol.ap, self.c_col.rearrange("s p k -> p s k"), writes=[ccol.res])
        for s in range(NSEQ):
            P.op("act", E.activation(cond.ap[:, :, s], ccol.ap[:, s, :], AF.Silu),
                 reads=[ccol.res], writes=[cond.res])
        wbuf = [self.sb([128, DC, 512], F32, "adaw%d" % i) for i in range(2)]
        bbuf = [self.sb([NSEQ, 512], F32, "adab%d" % i) for i in range(2)]
        rowb = [self.sb([NSEQ, 512], F32, "modrow%d" % i) for i in range(2)]
        cnt = 0
        for L in range(DEPTH):
            for ch in range(12):
                wb, bb, rb = wbuf[cnt % 2], bbuf[cnt % 2], rowb[cnt % 2]
                pb = cnt % 2
                cnt += 1
                P.dma("sp", wb.ap, self.ada_w[L, :, ch * 512:(ch + 1) * 512].rearrange("(k p) n -> p k n", p=128),
                      writes=[wb.res])
                P.dma("act", bb.ap, self.ada_b[L:L + 1, ch * 512:(ch + 1) * 512].broadcast_to([NSEQ, 512]),
                      writes=[bb.res])
                pr = self.psb[pb]
                for k in range(DC):
                    P.op("pe", E.matmul(self.bank(pb)[0:NSEQ, :], cond.ap[:, k, :], wb.ap[:, k, :],
                                                                  start=(k == 0), stop=(k == DC - 1)),
                         reads=[cond.res, wb.res], writes=[pr])
                vec = ch // 2
                half = ch % 2
                addone = 0.0 if vec in (0, 3) else 1.0
                P.op("dve", E.scalar_tensor_tensor(
                    rb.ap, self.bank(pb)[0:NSEQ, :], addone, bb.ap, ALU.add, ALU.add),
                    reads=[pr, bb.res], writes=[rb.res])
                if vec in (2, 5):
                    P.dma("sp", self.moddram[L, :, 0 if vec == 2 else 1, half * 512:(half + 1) * 512], rb.ap,
                          reads=[rb.res], writes=[self.moddram_res])
                else:
                    vi = {0: 0, 1: 1, 3: 2, 4: 3}[vec]
                    pc = self.psb[2 + pb]
                    for s in range(NSEQ):
                        for kk in range(4):
                            P.op("pe", E.matmul(
                                self.bank(2 + pb)[:, s * 4 + kk:s * 4 + kk + 1], rb.ap[:, kk * 128:(kk + 1) * 128],
                                self.ident.ap[0:NSEQ, s:s + 1], start=True, stop=True),
                                reads=[rb.res, self.ident.res], writes=[pc])
                    for s in range(NSEQ):
                        idx = (L * NSEQ + s) * 4 + vi
                        P.op("dve", E.tensor_copy(
                            self.modcol.ap[:, idx, half * 4:(half + 1) * 4], self.bank(2 + pb)[:, s * 4:(s + 1) * 4]),
                            reads=[pc], writes=[self.modcol.res])
        self.phase_reset()

    def mcol(self, L, s, vi, k):
        return self.modcol.ap[:, (L * NSEQ + s) * 4 + vi, k:k + 1]

    def make_hT(self, x_src, L, s, vsh, vsc, hT):
        P = self.P
        xt = self.xring
        n = 0
        for tg in range(4):
            tiles = []
            for j in range(4):
                t_ = xt[n % 4]
                n += 1
                tok = (tg * 4 + j) * 128
                P.dma("sp", t_.ap, x_src[tok:tok + 128, :], reads=[self.xres(s, tg * 4 + j)], writes=[t_.res])
                tiles.append(t_)
            for dc in range(DC):
                pb = dc % 2
                pr = self.psb[pb]
                for j in range(4):
                    P.op("pe", E.transpose(
                        self.bank(pb)[:, j * 128:(j + 1) * 128], tiles[j].ap[:, dc * 128:(dc + 1) * 128], self.ident.ap),
                        reads=[tiles[j].res, self.ident.res], writes=[pr])
                eng = self.ev_eng()
                dst = hT.ap[:, dc, tg * 512:(tg + 1) * 512]
                sc = self.mcol(L, s, vsc, dc)
                sh = self.mcol(L, s, vsh, dc)
                if eng == "dve":
                    P.op("dve", E.tensor_scalar(
                        dst, self.bank(pb), sc, sh, ALU.mult, ALU.add),
                        reads=[pr, self.modcol.res], writes=[hT.r(tg)])
                else:
                    P.op("act", E.activation(
                        dst, self.bank(pb), AF.Identity, bias=sh, scale=sc),
                        reads=[pr, self.modcol.res], writes=[hT.r(tg)])

    def load_ln_consts(self, L, s, which, need_z=True):
        P = self.P
        G = self.sb([128, D], F32, "G")
        g = self.sb([128, D], F32, "lng")
        b = self.sb([128, D], F32, "lnb")
        P.dma("sp", G.ap, self.moddram[L, s, which:which + 1, :].broadcast_to([128, D]),
              reads=[self.moddram_res], writes=[G.res])
        P.dma("sp", g.ap, self.ln_g[L, which:which + 1, :].broadcast_to([128, D]), writes=[g.res])
        P.dma("sp", b.ap, self.ln_b[L, which:which + 1, :].broadcast_to([128, D]), writes=[b.res])
        self.lnc = (G, g, b)
        self.ln_x = self.xring
        self.ln_z = [self.sb([128, D], F32, "lnz%d" % i) for i in range(3)] if need_z else None
        self.ln_st = [self.sb([128, 16], F32, "lnst%d" % i) for i in range(4)]
        self.ln_n = 0

    def ln_epilogue(self, s, tile, y_aps, y_res, x_src, x_dst, dst_res=None, z_tile=None):
        P = self.P
        G, g, b = self.lnc
        i = self.ln_n
        self.ln_n += 1
        xt, st = self.ln_x[i % 4], self.ln_st[i % 4]
        z = self.ln_z[i % 3] if z_tile is None else z_tile
        tok = tile * 128
        P.dma("sp", xt.ap, x_src[tok:tok + 128, :], reads=[self.xres(s, tile)], writes=[xt.res])
        for ap, c0, w in y_aps:
            P.op("dve", E.tensor_tensor(z.ap[:, c0:c0 + w], ap, G.ap[:, c0:c0 + w], ALU.mult),
                 reads=list(y_res) + [G.res], writes=[z.res])
        P.op("dve", E.scalar_tensor_tensor(z.ap, xt.ap, float(ALPHA), z.ap, ALU.mult, ALU.add),
             reads=[xt.res, z.res], writes=[z.res])
        P.op("dve", E.bn_stats(st.ap[:, 0:6], z.ap[:, 0:512]), reads=[z.res], writes=[st.r("a")])
        P.op("dve", E.bn_stats(st.ap[:, 6:12], z.ap[:, 512:1024]), reads=[z.res], writes=[st.r("b")])
        P.op("dve", E.bn_aggr(st.ap[:, 12:14], st.ap[:, 0:12]), reads=[st.r("a"), st.r("b")], writes=[st.r("c")])
        P.op("act", E.activation(st.ap[:, 14:15], st.ap[:, 13:14], AF.Sqrt, bias=LN_EPS),
             reads=[st.r("c")], writes=[st.r("d")])
        P.op("dve", E.reciprocal(st.ap[:, 14:15], st.ap[:, 14:15]), reads=[st.r("d")], writes=[st.r("d")])
        P.op("dve", E.tensor_scalar(st.ap[:, 15:16], st.ap[:, 12:13], st.ap[:, 14:15], -1.0, ALU.mult, ALU.mult),
             reads=[st.r("c"), st.r("d")], writes=[st.r("e")])
        P.op("act", E.activation(z.ap, z.ap, AF.Identity, bias=st.ap[:, 15:16], scale=st.ap[:, 14:15]),
             reads=[z.res, st.r("d"), st.r("e")], writes=[z.res])
        P.op("pool", E.tensor_tensor(z.ap, z.ap, g.ap, ALU.mult), reads=[z.res, g.res], writes=[z.res])
        P.op("pool", E.tensor_tensor(z.ap, z.ap, b.ap, ALU.add), reads=[z.res, b.res], writes=[z.res])
        P.dma("pool", x_dst[tok:tok + 128, :], z.ap, reads=[z.res],
              writes=[dst_res if dst_res is not None else self.xres(s, tile)])

    def xres(self, s, tile):
        return self.xres_[s][tile]

    def ffn(self, L, s, x_src, x_dst, last=False):
        P = self.P
        hT = self.sb([128, DC, T], BF16, "hT")
        self.xring = [self.sb([128, D], F32, "xr%d" % i) for i in range(4)]
        self.make_hT(x_src, L, s, 2, 3, hT)
        yacc = self.sb([128, NT, D], F32, "yacc")
        wg = [self.sb([128, DC, 512], BF16, "wg%d" % i) for i in range(2)]
        wu = [self.sb([128, DC, 512], BF16, "wu%d" % i) for i in range(2)]
        wo = [self.sb([128, 4, D], BF16, "wo%d" % i) for i in range(2)]
        hh = [self.sb([128, 4, 512], BF16, "hh%d" % i) for i in range(2)]
        sg = [self.sb([128, 512], F32, "sg%d" % i) for i in range(2)]
        groups = [(g * 512, 512) for g in range(5)] + [(2560, 256)]
        nhh = 0
        nsg = 0
        npy = 0
        for gi, (h0, gs) in enumerate(groups):
            nhc = gs // 128
            a, u, o = wg[gi % 2], wu[gi % 2], wo[gi % 2]
            P.dma("pool", a.ap[:, :, 0:gs], self.ffn_w_in[L, :, h0:h0 + gs].rearrange("(k p) n -> p k n", p=128),
                  writes=[a.res])
            P.dma("pool", u.ap[:, :, 0:gs], self.ffn_w_in[L, :, FH + h0:FH + h0 + gs].rearrange("(k p) n -> p k n", p=128),
                  writes=[u.res])
            P.dma("pool", o.ap[:, 0:nhc, :], self.ffn_w_out[L, h0:h0 + gs, :].rearrange("(c p) n -> p c n", p=128),
                  writes=[o.res])
            for tb in range(4):
                hb = hh[nhh % 2]
                nhh += 1
                for hc in range(nhc):
                    pg, pu = self.psb[0 + (hc % 2)], self.psb[2 + (hc % 2)]
                    bg, bu = 0 + (hc % 2), 2 + (hc % 2)
                    for k in range(DC):
                        P.op("pe", E.matmul(
                            self.bank(bg), a.ap[:, k, hc * 128:(hc + 1) * 128], hT.ap[:, k, tb * 512:(tb + 1) * 512],
                            start=(k == 0), stop=(k == DC - 1)), reads=[a.res, hT.r(tb)], writes=[pg])
                    for k in range(DC):
                        P.op("pe", E.matmul(
                            self.bank(bu), u.ap[:, k, hc * 128:(hc + 1) * 128], hT.ap[:, k, tb * 512:(tb + 1) * 512],
                            start=(k == 0), stop=(k == DC - 1)), reads=[u.res, hT.r(tb)], writes=[pu])
                    sgt = sg[nsg % 2]
                    nsg += 1
                    P.op("act", E.activation(sgt.ap, self.bank(bg), AF.Silu),
                         reads=[pg], writes=[sgt.res])
                    P.op("dve", E.tensor_tensor(
                        hb.ap[:, hc, :], sgt.ap, self.bank(bu), ALU.mult),
                        reads=[sgt.res, pu], writes=[hb.res])
                for tt in range(4):
                    tile = tb * 4 + tt
                    for half in range(2):
                        bpy = 4 + (npy % 4)
                        npy += 1
                        py = self.psb[bpy]
                        for hc in range(nhc):
                            P.op("pe", E.matmul(
                                self.bank(bpy), hb.ap[:, hc, tt * 128:(tt + 1) * 128], o.ap[:, hc, half * 512:(half + 1) * 512],
                                start=(hc == 0), stop=(hc == nhc - 1)), reads=[hb.res, o.res], writes=[py])
                        dst = yacc.ap[:, tile, half * 512:(half + 1) * 512]
                        if gi == 0:
                            P.op("act", E.copy(dst, self.bank(bpy)),
                                 reads=[py], writes=[yacc.r((tile, half))])
                        else:
                            P.op("dve", E.tensor_tensor(dst, dst, self.bank(bpy), ALU.add),
                                 reads=[py, yacc.r((tile, half))], writes=[yacc.r((tile, half))])
        self.load_ln_consts(L, s, 1, need_z=False)
        for tile in range(NT):
            zt = Tile(yacc.ap[:, tile, :])
            zt.res = yacc.r((tile, 0))
            self.ln_epilogue(s, tile, [(yacc.ap[:, tile, :], 0, D)], [yacc.r((tile, 0)), yacc.r((tile, 1))],
                             x_src, x_dst, dst_res=(self.outres if last else None), z_tile=zt)
        self.phase_reset()

    def rwkv(self, L, s, x_src, x_dst, last=False):
        P = self.P
        j = L // 2
        CW = float(WDECAY)
        bank = self.bank
        psb = self.psb
        cst = self.sb([128, NCONST], F32, "cst")
        P.dma("sp", cst.ap, self.consts, writes=[cst.res])
        maskG = cst.ap[:, 0:512]
        maskNT = cst.ap[:, 512:640]
        triBD = cst.ap[:, 640:768]
        sel63 = cst.ap[:, 768:770]
        Wrkv = [self.sb([128, DC, D], BF16, "Wrkv%d" % p) for p in range(3)]
        for p in range(3):
            P.dma("pool", Wrkv[p].ap, self.rw_wrkv[j, p].rearrange("(k p) n -> p k n", p=128), writes=[Wrkv[p].res])
        w1 = self.sb([128, DC, 64], BF16, "w1")
        a1 = self.sb([128, DC, 64], BF16, "a1")
        g1 = self.sb([128, DC, 160], BF16, "g1")
        P.dma("pool", w1.ap, self.rw_w1[j].rearrange("(k p) n -> p k n", p=128), writes=[w1.res])
        P.dma("pool", a1.ap, self.rw_a1[j].rearrange("(k p) n -> p k n", p=128), writes=[a1.res])
        P.dma("pool", g1.ap, self.rw_g1[j].rearrange("(k p) n -> p k n", p=128), writes=[g1.res])
        w2a = self.sb([65, D], BF16, "w2a")
        a2a = self.sb([65, D], BF16, "a2a")
        g2a = self.sb([128, D], BF16, "g2a")
        g2b = self.sb([32, D], BF16, "g2b")
        P.dma("pool", w2a.ap, self.rw_w2a[j], writes=[w2a.res])
        P.dma("pool", a2a.ap, self.rw_a2a[j], writes=[a2a.res])
        P.dma("pool", g2a.ap, self.rw_g2[j, 0:128, :], writes=[g2a.res])
        P.dma("pool", g2b.ap, self.rw_g2[j, 128:160, :], writes=[g2b.res])
        if j > 0:
            v1 = self.sb([128, DC, 32], BF16, "v1")
            v2a = self.sb([33, D], BF16, "v2a")
            P.dma("pool", v1.ap, self.rw_v1[j - 1].rearrange("(k p) n -> p k n", p=128), writes=[v1.res])
            P.dma("pool", v2a.ap, self.rw_v2a[j - 1], writes=[v2a.res])
        mu = self.sb([128, 6, DC], F32, "mu")
        P.dma("sp", mu.ap, self.rw_mu[j], writes=[mu.res])
        vec = self.sb([128, 5, D], F32, "vec")
        P.dma("sp", vec.ap, self.rw_vec[j:j + 1].broadcast_to([128, 5, D]), writes=[vec.res])
        hT = self.sb([128, DC, T], BF16, "hT")
        ring_off = self.off
        self.xring = [self.sb([128, D], F32, "xr%d" % i) for i in range(4)]
        self.make_hT(x_src, L, s, 0, 1, hT)
        P.barrier()
        self.off = ring_off
        f32t = lambda n: self.sb([128, 512], F32, n)
        bft = lambda n: self.sb([128, 512], BF16, n)
        xx = self.sb([128, DC, 128], BF16, "xx")
        xs = self.sb([128, 6, DC, 128], BF16, "xs")
        r_sb, k_sb, v_sb, a_sb, sgw, kk, kp, u_, rk, eL, enL, eLm, g_sb, sq = [
            f32t(n) for n in "r k v a sgw kk kp u rk eL enL eLm g sq".split()]
        y_sb, yc = eLm, u_
        bonus = rk
        At, Bt, Kt, Rt, Vt = [bft(n) for n in "At Bt Kt Rt Vt".split()]
        FT = self.sb([128, 4, 4, 128], BF16, "FT")
        Gm = self.sb([128, 4, 512], BF16, "Gm")
        Pm = [self.sb([128, 2, 4, 128], BF16, "Pm%d" % i) for i in range(2)]
        X = [self.sb([128, 4, 128], BF16, "X%d" % i) for i in range(2)]
        MT = self.sb([128, 2, 4, 64], BF16, "MT")
        Zs = self.sb([128, 2, 4, 64], F32, "Zs")
        RhF = self.sb([128, 4, 128], BF16, "RhF")
        Y2s = self.sb([128, 512], F32, "Y2s")
        gcol = self.sb([128, 2, 16], F32, "gcol")
        MTbd = self.sb([128, 2, 4, 2, 64], BF16, "MTbd")
        Zbd = self.sb([128, 2, 4, 2, 64], F32, "Zbd")
        mbd = cst.ap[:, 896:1024].rearrange("p (a b) -> p a b", b=64)
        vf, vg = Y2s, sq
        S = self.sb([128, 2, 4, 128], F32, "S")
        Sb = self.sb([128, 2, 4, 128], BF16, "Sb")
        sm = self.sb([128, 64], F32, "sm")
        l1w = self.sb([65, 128], BF16, "l1w")
        l1a = self.sb([65, 128], BF16, "l1a")
        l1v = self.sb([33, 128], BF16, "l1v")
        l1g = self.sb([128, 128], BF16, "l1g")
        l1g2 = self.sb([32, 128], BF16, "l1g2")
        P.op("pool", E.memset(S.ap, 0.0), writes=[S.r(0), S.r(1)])
        P.op("pool", E.memset(Sb.ap, 0.0), writes=[Sb.r(0), Sb.r(1)])
        P.op("pool", E.memset(l1w.ap, 1.0), writes=[l1w.res])
        P.op("pool", E.memset(l1a.ap, 1.0), writes=[l1a.res])
        P.op("pool", E.memset(l1v.ap, 1.0), writes=[l1v.res])
        identb = self.identb
        nb = [0]

        def gbank():
            nb[0] ^= 1
            return nb[0]

        def bc8(ap8):
            return ap8.unsqueeze(2).to_broadcast([128, 8, 64])

        def v3(ap):
            return ap.rearrange("p (a b) -> p a b", b=64)

        for tile in range(min(NT, DBG["tiles"])):
            t0 = tile * 128
            tg = tile // 4
            if tile == 0:
                P.op("dve", E.tensor_tensor(xx.ap[:, :, 1:128], hT.ap[:, :, 0:127], hT.ap[:, :, 1:128], ALU.subtract),
                     reads=[hT.r(0)], writes=[xx.res])
                P.op("dve", E.tensor_scalar(xx.ap[:, :, 0:1], hT.ap[:, :, 0:1], -1.0, None, ALU.mult),
                     reads=[hT.r(0)], writes=[xx.res])
            else:
                P.op("dve", E.tensor_tensor(xx.ap, hT.ap[:, :, t0 - 1:t0 + 127], hT.ap[:, :, t0:t0 + 128], ALU.subtract),
                     reads=[hT.r(tg), hT.r((t0 - 1) // 512)], writes=[xx.res])
            for p in range(6):
                for dc in range(DC):
                    eng = "dve" if (p * DC + dc) % 2 == 0 else "dve"
                    P.op(eng, E.scalar_tensor_tensor(
                        xs.ap[:, p, dc, :], xx.ap[:, dc, :], mu.ap[:, p, dc:dc + 1], hT.ap[:, dc, t0:t0 + 128], ALU.mult, ALU.add),
                        reads=[xx.res, mu.res, hT.r(tg)], writes=[xs.r(p)])
            def lora1(wt, p, n, dst, func, b):
                for dc in range(DC):
                    P.op("pe", E.matmul(bank(b)[0:n, 0:128], wt.ap[:, dc, 0:n], xs.ap[:, p, dc, :],
                                                         start=(dc == 0), stop=(dc == DC - 1)),
                         reads=[wt.res, xs.r(p)], writes=[psb[b]])
                P.op("act", E.activation(dst.ap[0:n, :], bank(b)[0:n, 0:128], func),
                     reads=[psb[b]], writes=[dst.res])
            lora1(w1, 3, 64, l1w, AF.Tanh, gbank())
            lora1(a1, 4, 64, l1a, AF.Copy, gbank())
            if j > 0:
                lora1(v1, 2, 32, l1v, AF.Copy, gbank())
            b = gbank()
            for dc in range(DC):
                P.op("pe", E.matmul(bank(b)[:, 0:128], g1.ap[:, dc, 0:128], xs.ap[:, 5, dc, :],
                                                          start=(dc == 0), stop=(dc == DC - 1)),
                     reads=[g1.res, xs.r(5)], writes=[psb[b]])
            P.op("act", E.activation(l1g.ap, bank(b)[:, 0:128], AF.Sigmoid), reads=[psb[b]], writes=[l1g.res])
            b = gbank()
            for dc in range(DC):
                P.op("pe", E.matmul(bank(b)[0:32, 0:128], g1.ap[:, dc, 128:160], xs.ap[:, 5, dc, :],
                                                          start=(dc == 0), stop=(dc == DC - 1)),
                     reads=[g1.res, xs.r(5)], writes=[psb[b]])
            P.op("act", E.activation(l1g2.ap, bank(b)[0:32, 0:128], AF.Sigmoid), reads=[psb[b]], writes=[l1g2.res])

            if DBG["stage"] < 2:
                continue
            for hf in range(2):
                c0 = hf * 512
                def proj(p, W, dst, eng):
                    b = gbank()
                    for dc in range(DC):
                        P.op("pe", E.matmul(bank(b), xs.ap[:, p, dc, :], W.ap[:, dc, c0:c0 + 512],
                                                                  start=(dc == 0), stop=(dc == DC - 1)),
                             reads=[xs.r(p), W.res], writes=[psb[b]])
                    if eng == "act":
                        P.op("act", E.copy(dst.ap, bank(b)), reads=[psb[b]], writes=[dst.res])
                    else:
                        P.op("dve", E.tensor_copy(dst.ap, bank(b)), reads=[psb[b]], writes=[dst.res])
                proj(0, Wrkv[0], r_sb, "act")
                proj(1, Wrkv[1], k_sb, "dve")
                proj(2, Wrkv[2], v_sb, "act")

                def lora2(l1t, n, w2t, dst, func):
                    b = gbank()
                    P.op("pe", E.matmul(bank(b), l1t.ap[0:n, :], w2t.ap[0:n, c0:c0 + 512], start=True, stop=True),
                         reads=[l1t.res, w2t.res], writes=[psb[b]])
                    P.op("act", E.activation(dst.ap, bank(b), func), reads=[psb[b]], writes=[dst.res])
                lora2(l1w, 65, w2a, sgw, AF.Sigmoid)
                lora2(l1a, 65, a2a, a_sb, AF.Sigmoid)
                b = gbank()
                P.op("pe", E.matmul(bank(b), l1g.ap, g2a.ap[:, c0:c0 + 512], start=True, stop=False),
                     reads=[l1g.res, g2a.res], writes=[psb[b]])
                P.op("pe", E.matmul(bank(b), l1g2.ap, g2b.ap[:, c0:c0 + 512], start=False, stop=True),
                     reads=[l1g2.res, g2b.res], writes=[psb[b]])
                P.op("act", E.copy(g_sb.ap, bank(b)), reads=[psb[b]], writes=[g_sb.res])
                if j == 0:
                    P.dma("sp", self.vfirst[s, t0:t0 + 128, c0:c0 + 512], v_sb.ap, reads=[v_sb.res], writes=[self.vf_res[s][tile]])
                else:
                    lora2(l1v, 33, v2a, vg, AF.Sigmoid)
                    P.dma("sp", vf.ap, self.vfirst[s, t0:t0 + 128, c0:c0 + 512], reads=[self.vf_res[s][tile]], writes=[vf.res])
                    P.op("pool", E.tensor_tensor(vf.ap, vf.ap, v_sb.ap, ALU.subtract), reads=[vf.res, v_sb.res], writes=[vf.res])
                    P.op("pool", E.tensor_tensor(vf.ap, vf.ap, vg.ap, ALU.mult), reads=[vf.res, vg.res], writes=[vf.res])
                    P.op("pool", E.tensor_tensor(v_sb.ap, v_sb.ap, vf.ap, ALU.add), reads=[vf.res, v_sb.res], writes=[v_sb.res])
                bL = gbank()
                P.op("pe", E.matmul(bank(bL), triBD, sgw.ap, start=True, stop=True),
                     reads=[cst.res, sgw.res], writes=[psb[bL]])
                P.op("act", E.activation(eL.ap, bank(bL), AF.Exp, scale=-CW), reads=[psb[bL]], writes=[eL.res])
                P.op("act", E.activation(enL.ap, bank(bL), AF.Exp, scale=CW), reads=[psb[bL]], writes=[enL.res])
                P.op("dve", E.tensor_tensor(eLm.ap, bank(bL), sgw.ap, ALU.subtract), reads=[psb[bL], sgw.res], writes=[eLm.res])
                P.op("act", E.activation(eLm.ap, eLm.ap, AF.Exp, scale=-CW), reads=[eLm.res], writes=[eLm.res])
                P.op("pool", E.tensor_tensor(kk.ap, k_sb.ap, vec.ap[:, 0, c0:c0 + 512], ALU.mult),
                     reads=[k_sb.res, vec.res], writes=[kk.res])
                P.op("pool", E.tensor_tensor(sq.ap, kk.ap, kk.ap, ALU.mult), reads=[kk.res], writes=[sq.res])
                P.op("dve", E.tensor_reduce(sm.ap[:, 0:8], v3(sq.ap), AX.X, ALU.add), reads=[sq.res], writes=[sm.r("ss")])
                P.op("act", E.activation(sm.ap[:, 0:8], sm.ap[:, 0:8], AF.Sqrt), reads=[sm.r("ss")], writes=[sm.r("ss")])
                P.op("dve", E.tensor_scalar_max(sm.ap[:, 0:8], sm.ap[:, 0:8], 1e-12), reads=[sm.r("ss")], writes=[sm.r("ss")])
                P.op("dve", E.reciprocal(sm.ap[:, 0:8], sm.ap[:, 0:8]), reads=[sm.r("ss")], writes=[sm.r("ss")])
                P.op("dve", E.tensor_tensor(v3(kk.ap), v3(kk.ap), bc8(sm.ap[:, 0:8]), ALU.mult),
                     reads=[kk.res, sm.r("ss")], writes=[kk.res])
                P.op("dve", E.scalar_tensor_tensor(u_.ap, a_sb.ap, -1.0, vec.ap[:, 1, c0:c0 + 512], ALU.add, ALU.mult),
                     reads=[a_sb.res, vec.res], writes=[u_.res])
                P.op("dve", E.scalar_tensor_tensor(kp.ap, u_.ap, 1.0, k_sb.ap, ALU.add, ALU.mult),
                     reads=[u_.res, k_sb.res], writes=[kp.res])
                P.op("dve", E.scalar_tensor_tensor(At.ap, kk.ap, -1.0, eLm.ap, ALU.mult, ALU.mult),
                     reads=[kk.res, eLm.res], writes=[At.res])
                P.op("pool", E.tensor_tensor(u_.ap, kk.ap, a_sb.ap, ALU.mult), reads=[kk.res, a_sb.res, u_.res], writes=[u_.res])
                P.op("pool", E.tensor_tensor(Bt.ap, u_.ap, enL.ap, ALU.mult), reads=[u_.res, enL.res], writes=[Bt.res])
                P.op("pool", E.tensor_tensor(Kt.ap, kp.ap, enL.ap, ALU.mult), reads=[kp.res, enL.res], writes=[Kt.res])
                P.op("dve", E.tensor_tensor(Rt.ap, r_sb.ap, eL.ap, ALU.mult), reads=[r_sb.res, eL.res], writes=[Rt.res])
                P.op("act", E.copy(Vt.ap, v_sb.ap), reads=[v_sb.res], writes=[Vt.res])
                P.op("pool", E.tensor_tensor(rk.ap, r_sb.ap, kp.ap, ALU.mult), reads=[r_sb.res, kp.res], writes=[rk.res])
                P.op("pool", E.tensor_tensor(rk.ap, rk.ap, vec.ap[:, 2, c0:c0 + 512], ALU.mult), reads=[rk.res, vec.res], writes=[rk.res])
                P.op("dve", E.tensor_reduce(sm.ap[:, 16:24], v3(rk.ap), AX.X, ALU.add), reads=[rk.res], writes=[sm.r("rk")])
                P.op("dve", E.tensor_tensor(v3(bonus.ap), v3(v_sb.ap), bc8(sm.ap[:, 16:24]), ALU.mult),
                     reads=[v_sb.res, sm.r("rk")], writes=[rk.res])
                if DBG["stage"] < 3:
                    continue
                bg = gbank()
                for q in range(4):
                    P.op("pe", E.matmul(bank(bg)[:, q * 2:q * 2 + 2], eL.ap[:, q * 128:(q + 1) * 128], sel63,
                                                              start=True, stop=True),
                         reads=[eL.res, cst.res], writes=[psb[bg]])
                P.op("dve", E.tensor_copy(gcol.ap[:, :, 0:4], bank(bg)[:, 0:8].rearrange("p (q c) -> p c q", c=2)),
                     reads=[psb[bg]], writes=[gcol.res])
                ops4 = [At, Bt, Kt, Rt]
                for oi in range(4):
                    for q in range(4):
                        bt = 6 + (oi // 2)
                        col = ((oi % 2) * 4 + q) * 128
                        P.op("pe", E.transpose(
                            bank(bt, BF16)[:, col:col + 128], ops4[oi].ap[:, q * 128:(q + 1) * 128], identb.ap),
                            reads=[ops4[oi].res, identb.res], writes=[psb[bt]])
                P.op("act", E.copy(FT.ap[:, 0:2, :, :], bank(6, BF16).rearrange("p (a b c) -> p a b c", a=2, b=4)),
                     reads=[psb[6]], writes=[FT.r(0)])
                P.op("dve", E.tensor_copy(FT.ap[:, 2:4, :, :], bank(7, BF16).rearrange("p (a b c) -> p a b c", a=2, b=4)),
                     reads=[psb[7]], writes=[FT.r(1)])
                FTr = [FT.r(0), FT.r(1)]
                for grp in range(2 if DBG["stage"] >= 4 else 0):
                    for hl in range(4):
                        q = grp * 2 + hl // 2
                        m = hl % 2
                        rows = slice(m * 64, m * 64 + 64)
                        bgm = 2 + m
                        hcol = q * 128 + m * 64
                        for gi, (lo, ro) in enumerate([(1, 0), (1, 3), (2, 0), (2, 3)]):
                            P.op("pe", E.matmul(
                                bank(bgm)[:, gi * 128:(gi + 1) * 128], FT.ap[rows, lo, q, :], FT.ap[rows, ro, q, :],
                                start=True, stop=True, tile_position=(m * 64, 0)),
                                reads=FTr, writes=[psb[bgm]])
                        P.op("pe", E.matmul(
                            bank(4 + m)[:, (hl // 2) * 128:(hl // 2 + 1) * 128], FT.ap[rows, 0, q, :], FT.ap[rows, 1, q, :],
                            start=True, stop=True, tile_position=(m * 64, 0)),
                            reads=FTr, writes=[psb[4 + m]])
                        P.op("dve", E.tensor_tensor(Gm.ap[:, hl, :], bank(bgm), maskG, ALU.mult),
                             reads=[psb[bgm], cst.res], writes=[Gm.r(hl)])
                    for m in range(2):
                        P.op("dve", E.tensor_tensor(
                            Pm[0].ap[:, 1, m::2, :], bank(4 + m)[:, 0:256].rearrange("p (a b) -> p a b", b=128),
                            maskNT.unsqueeze(1).to_broadcast([128, 2, 128]), ALU.mult),
                            reads=[psb[4 + m], cst.res], writes=[Pm[0].r(("t", m))])
                    P.op("pool", E.tensor_copy(Pm[0].ap[:, 0, :, :], Gm.ap[:, :, 0:128]),
                         reads=[Gm.r(h_) for h_ in range(4)], writes=[Pm[0].r("p")])
                    if DBG["stage"] < 5:
                        continue
                    bw = gbank()
                    for hl in range(4):
                        hcol = (grp * 2 + hl // 2) * 128 + (hl % 2) * 64
                        P.op("pe", E.matmul(
                            bank(bw)[:, hl * 64:(hl + 1) * 64], Gm.ap[:, hl, 256:384], Vt.ap[:, hcol:hcol + 64], start=True, stop=True),
                            reads=[Gm.r(hl), Vt.res], writes=[psb[bw]])
                    P.op("act", E.copy(X[0].ap[:, :, 64:128], bank(bw)[:, 0:256].rearrange("p (a b) -> p a b", b=64)),
                         reads=[psb[bw]], writes=[X[0].r("u")])
                    P.op("pool", E.tensor_copy(
                        X[0].ap[:, :, 0:64], At.ap[:, grp * 256:(grp + 1) * 256].rearrange("p (a b) -> p a b", b=64)),
                        reads=[At.res], writes=[X[0].r("a")])
                    Xr = lambda i: [X[i].r("u"), X[i].r("a")]
                    Pr = lambda i: [Pm[i].r("p"), Pm[i].r(("t", 0)), Pm[i].r(("t", 1))]
                    for lvl in range(6):
                        ci, ni = lvl % 2, (lvl + 1) % 2
                        for hl in range(4):
                            P.op("pe", E.matmul(
                                bank(2)[:, hl * 128:(hl + 1) * 128], Pm[ci].ap[:, 0, hl, :], X[ci].ap[:, hl, :], start=True, stop=True),
                                reads=Pr(ci) + Xr(ci), writes=[psb[2]])
                        P.op("dve", E.tensor_tensor(
                            X[ni].ap, bank(2).rearrange("p (a b) -> p a b", b=128), X[ci].ap, ALU.add),
                            reads=[psb[2]] + Xr(ci), writes=Xr(ni))
                        if lvl < 5:
                            for hl in range(4):
                                P.op("pe", E.matmul(
                                    bank(4)[:, hl * 128:(hl + 1) * 128], Pm[ci].ap[:, 1, hl, :], Pm[ci].ap[:, 0, hl, :], start=True, stop=True),
                                    reads=Pr(ci), writes=[psb[4]])
                                P.op("pe", E.matmul(
                                    bank(5)[:, hl * 128:(hl + 1) * 128], Pm[ci].ap[:, 0, hl, :], Pm[ci].ap[:, 1, hl, :], start=True, stop=True),
                                    reads=Pr(ci), writes=[psb[5]])
                            P.op("act", E.copy(Pm[ni].ap[:, 0, :, :], bank(4).rearrange("p (a b) -> p a b", b=128)),
                                 reads=[psb[4]], writes=[Pm[ni].r("p")])
                            P.op("act", E.copy(Pm[ni].ap[:, 1, :, :], bank(5).rearrange("p (a b) -> p a b", b=128)),
                                 reads=[psb[5]], writes=[Pm[ni].r(("t", 0)), Pm[ni].r(("t", 1))])
                    if DBG["stage"] < 6:
                        continue
                    XF = X[0]
                    XFr = Xr(0)
                    for hl in range(4):
                        q = grp * 2 + hl // 2
                        m = hl % 2
                        hcol = q * 128 + m * 64
                        for c in range(2):
                            rc = slice(c * 64, c * 64 + 64)
                            bmz = 6 + c
                            P.op("pe", E.matmul(
                                bank(bmz)[m * 64:m * 64 + 64, q * 64:q * 64 + 64], XF.ap[rc, hl, 0:64], Bt.ap[rc, hcol:hcol + 64],
                                start=True, stop=True, tile_position=(c * 64, m * 64)),
                                reads=XFr + [Bt.res], writes=[psb[bmz]])
                            P.op("pe", E.matmul(
                                bank(bmz)[m * 64:m * 64 + 64, 256 + q * 64:256 + q * 64 + 64], Bt.ap[rc, hcol:hcol + 64], XF.ap[rc, hl, 64:128],
                                start=True, stop=False, tile_position=(c * 64, m * 64)),
                                reads=XFr + [Bt.res], writes=[psb[bmz]])
                            P.op("pe", E.matmul(
                                bank(bmz)[m * 64:m * 64 + 64, 256 + q * 64:256 + q * 64 + 64], Kt.ap[rc, hcol:hcol + 64], Vt.ap[rc, hcol:hcol + 64],
                                start=False, stop=True, tile_position=(c * 64, m * 64)),
                                reads=[Kt.res, Vt.res], writes=[psb[bmz]])
                        if DBG["sub"] >= 2:
                          P.op("pe", E.matmul(
                            bank(3)[m * 64:m * 64 + 64, (hl // 2) * 128:(hl // 2) * 128 + 128], XF.ap[:, hl, 0:64], Gm.ap[:, hl, 128:256],
                            start=True, stop=True, tile_position=(0, m * 64)),
                            reads=XFr + [Gm.r(hl)], writes=[psb[3]])
                    qs = slice(grp * 2, grp * 2 + 2)
                    for c in range(2):
                        bmz = 6 + c
                        P.op("dve", E.tensor_tensor(
                            MT.ap[:, c, qs, :], bank(bmz)[:, grp * 128:grp * 128 + 128].rearrange("p (a b) -> p a b", b=64),
                            cst.ap[:, 832:896].unsqueeze(1).to_broadcast([128, 2, 64]), ALU.add),
                            reads=[psb[bmz], cst.res], writes=[MT.r((c, grp))])
                        P.op("act", E.copy(
                            Zs.ap[:, c, qs, :], bank(bmz)[:, 256 + grp * 128:256 + grp * 128 + 128].rearrange("p (a b) -> p a b", b=64)),
                            reads=[psb[bmz]], writes=[Zs.r((c, grp))])
                        for ql in range(2):
                            q = grp * 2 + ql
                            P.op("pool", E.tensor_tensor(
                                MTbd.ap[:, c, q, :, :], MT.ap[:, c, q, :].unsqueeze(1).to_broadcast([128, 2, 64]), mbd, ALU.mult),
                                reads=[MT.r((c, grp)), cst.res], writes=[MTbd.r((c, q))])
                            P.op("pool", E.tensor_tensor(
                                Zbd.ap[:, c, q, :, :], Zs.ap[:, c, q, :].unsqueeze(1).to_broadcast([128, 2, 64]), mbd, ALU.mult),
                                reads=[Zs.r((c, grp)), cst.res], writes=[Zbd.r((c, q))])
                    P.op("dve", E.tensor_tensor(
                        RhF.ap[:, qs, :], bank(3)[:, 0:256].rearrange("p (a b) -> p a b", b=128), FT.ap[:, 3, qs, :], ALU.add),
                        reads=[psb[3]] + FTr, writes=[RhF.r(grp)])
                    by2 = gbank()
                    for hl in range(4):
                        hcol = (grp * 2 + hl // 2) * 128 + (hl % 2) * 64
                        P.op("pe", E.matmul(
                            bank(by2)[:, hl * 64:(hl + 1) * 64], Gm.ap[:, hl, 128:256], XF.ap[:, hl, 64:128], start=True, stop=False),
                            reads=[Gm.r(hl)] + XFr, writes=[psb[by2]])
                        P.op("pe", E.matmul(
                            bank(by2)[:, hl * 64:(hl + 1) * 64], Gm.ap[:, hl, 384:512], Vt.ap[:, hcol:hcol + 64], start=False, stop=True),
                            reads=[Gm.r(hl), Vt.res], writes=[psb[by2]])
                    P.op("act", E.copy(Y2s.ap[:, grp * 256:(grp + 1) * 256], bank(by2)[:, 0:256]),
                         reads=[psb[by2]], writes=[Y2s.res])
                if DBG["stage"] < 7:
                    continue
                allq = [MTbd.r((c_, q_)) for c_ in range(2) for q_ in range(4)]
                for c in range(2):
                    for q in range(4):
                        P.op("pe", E.matmul(
                            bank(2)[c * 64:c * 64 + 64, q * 128:(q + 1) * 128], RhF.ap[:, q, c * 64:c * 64 + 64], Sb.ap[:, hf, q, :],
                            start=True, stop=True, tile_position=(0, c * 64)),
                            reads=[RhF.r(0), RhF.r(1), Sb.r(hf)], writes=[psb[2]])
                    for q in range(4):
                        P.op("pe", E.matmul(
                            bank(3)[:, q * 128:(q + 1) * 128], MTbd.ap[:, c, q, :, :].rearrange("p a b -> p (a b)"), Sb.ap[:, hf, q, :],
                            start=True, stop=True),
                            reads=[MTbd.r((c, q)), Sb.r(hf)], writes=[psb[3]])
                    P.op("dve", E.tensor_tensor(
                        S.ap[:, hf, :, :], bank(3).rearrange("p (a b) -> p a b", b=128),
                        Zbd.ap[:, c, :, :, :].rearrange("p q a b -> p q (a b)"), ALU.add),
                        reads=[psb[3]] + [Zbd.r((c, q_)) for q_ in range(4)], writes=[S.r(hf)])
                    P.op("dve", E.tensor_tensor(
                        S.ap[:, hf, :, :], S.ap[:, hf, :, :], gcol.ap[:, c, 0:4].unsqueeze(2).to_broadcast([128, 4, 128]), ALU.mult),
                        reads=[S.r(hf), gcol.res], writes=[S.r(hf)])
                    P.op("act", E.copy(Sb.ap[:, hf, :, :], S.ap[:, hf, :, :]), reads=[S.r(hf)], writes=[Sb.r(hf)])
                P.op("dve", E.tensor_tensor(y_sb.ap, bank(2), Y2s.ap, ALU.add),
                     reads=[psb[2], Y2s.res], writes=[y_sb.res])
                if DBG["stage"] < 8:
                    continue
                yres = [y_sb.res]
                P.op("dve", E.tensor_reduce(sm.ap[:, 32:40], v3(y_sb.ap), AX.X, ALU.add), reads=yres, writes=[sm.r("m")])
                P.op("dve", E.tensor_scalar_mul(sm.ap[:, 32:40], sm.ap[:, 32:40], 1.0 / 64), reads=[sm.r("m")], writes=[sm.r("m")])
                P.op("dve", E.tensor_tensor(v3(yc.ap), v3(y_sb.ap), bc8(sm.ap[:, 32:40]), ALU.subtract),
                     reads=yres + [sm.r("m")], writes=[yc.res])
                P.op("pool", E.tensor_tensor(sq.ap, yc.ap, yc.ap, ALU.mult), reads=[yc.res], writes=[sq.res])
                P.op("dve", E.tensor_reduce(sm.ap[:, 48:56], v3(sq.ap), AX.X, ALU.add), reads=[sq.res], writes=[sm.r("v")])
                P.op("act", E.activation(sm.ap[:, 48:56], sm.ap[:, 48:56], AF.Sqrt, bias=GN_EPS, scale=1.0 / 64),
                     reads=[sm.r("v")], writes=[sm.r("v")])
                P.op("dve", E.reciprocal(sm.ap[:, 48:56], sm.ap[:, 48:56]), reads=[sm.r("v")], writes=[sm.r("v")])
                P.op("dve", E.tensor_tensor(v3(yc.ap), v3(yc.ap), bc8(sm.ap[:, 48:56]), ALU.mult),
                     reads=[yc.res, sm.r("v")], writes=[yc.res])
                P.op("pool", E.tensor_tensor(yc.ap, yc.ap, vec.ap[:, 3, c0:c0 + 512], ALU.mult), reads=[yc.res, vec.res], writes=[yc.res])
                P.op("pool", E.tensor_tensor(yc.ap, yc.ap, vec.ap[:, 4, c0:c0 + 512], ALU.add), reads=[yc.res, vec.res], writes=[yc.res])
                P.op("pool", E.tensor_tensor(yc.ap, yc.ap, bonus.ap, ALU.add), reads=[yc.res, rk.res], writes=[yc.res])
                P.op("pool", E.tensor_tensor(At.ap, yc.ap, g_sb.ap, ALU.mult), reads=[yc.res, g_sb.res, At.res], writes=[At.res])
                P.dma("pool", self.ogdram[s, t0:t0 + 128, c0:c0 + 512], At.ap, reads=[At.res], writes=[self.og_res[s][tile]])
        self.phase_reset()
        if DBG["stage"] < 9:
            return
        Wo = self.sb([128, DC, D], BF16, "Wo")
        P.dma("pool", Wo.ap, self.rw_wout[j].rearrange("(k p) n -> p k n", p=128), writes=[Wo.res])
        self.out_proj_ln(L, s, Wo, self.ogdram[s], self.og_res[s], x_src, x_dst, last)
        self.phase_reset()

    def out_proj_ln(self, L, s, Wo, og, og_res, x_src, x_dst, last):
        P = self.P
        bank = self.bank
        psb = self.psb
        self.xring = [self.sb([128, D], F32, "xr%d" % i) for i in range(4)]
        self.load_ln_consts(L, s, 0, need_z=True)
        ogt = [self.sb([128, D], BF16, "ogt%d" % i) for i in range(2)]
        ogT = [self.sb([128, DC, 128], BF16, "ogT%d" % i) for i in range(2)]
        for tile in range(NT):
            t0 = tile * 128
            o, oT = ogt[tile % 2], ogT[tile % 2]
            P.dma("sp", o.ap, og[t0:t0 + 128, :], reads=[og_res[tile]], writes=[o.res])
            bt = 4 + tile % 2
            for dc in range(DC):
                P.op("pe", E.transpose(bank(bt, BF16)[:, dc * 128:(dc + 1) * 128], o.ap[:, dc * 128:(dc + 1) * 128], self.identb.ap),
                     reads=[o.res, self.identb.res], writes=[psb[bt]])
            P.op("act", E.copy(oT.ap, bank(bt, BF16).rearrange("p (a b) -> p a b", b=128)),
                 reads=[psb[bt]], writes=[oT.res])
            b0 = (tile % 2) * 2
            for half in range(2):
                for dc in range(DC):
                    P.op("pe", E.matmul(
                        bank(b0 + half), oT.ap[:, dc, :], Wo.ap[:, dc, half * 512:(half + 1) * 512], start=(dc == 0), stop=(dc == DC - 1)),
                        reads=[oT.res, Wo.res], writes=[psb[b0 + half]])
            self.ln_epilogue(s, tile, [(bank(b0), 0, 512), (bank(b0 + 1), 512, 512)], [psb[b0], psb[b0 + 1]],
                             x_src, x_dst, dst_res=(self.outres if last else None))

    def dsa(self, L, s, x_src, x_dst, last=False):
        P = self.P
        j = L // 2
        bank = self.bank
        psb = self.psb
        identb = self.identb
        SCL = 0.125
        WSC = 1.0 / math.sqrt(8 * 64)
        win = self.dsa_w_in[j]
        ldw = lambda t, ap: P.dma("pool", t.ap, ap, writes=[t.res])
        Wq = self.sb([128, DC, 1024], BF16, "Wq")
        Wc = self.sb([128, DC, 128], BF16, "Wc")
        Wqi = self.sb([128, DC, 512], BF16, "Wqi")
        Wki = self.sb([128, DC, 64], BF16, "Wki")
        Wwi = self.sb([128, DC, 8], BF16, "Wwi")
        for t_, c0, c1 in [(Wq, 0, 1024), (Wc, 1024, 1152), (Wqi, 1152, 1664), (Wki, 1664, 1728), (Wwi, 1728, 1736)]:
            ldw(t_, win[:, c0:c1].rearrange("(k p) n -> p k n", p=128))
        wuk = self.sb([64, 16, 128], BF16, "wuk")
        ldw(wuk, self.dsa_w_uk[j].rearrange("h d c -> d h c"))
        wuv = self.sb([128, 16, 64], BF16, "wuv")
        ldw(wuv, self.dsa_w_uv[j].rearrange("h c d -> c h d"))
        gkv = self.sb([128, 128], F32, "gkv")
        P.dma("sp", gkv.ap, self.dsa_kv_norm[j:j + 1, :].broadcast_to([128, 128]), writes=[gkv.res])
        caus = self.sb([128, 4, 512], F32, "caus")
        P.dma("sp", caus.ap, self.dsa_caus, writes=[caus.res])
        braw = self.sb([128, 16, 256], F32, "braw")
        c31 = self.sb([128, 16], F32, "c31")
        band = self.sb([128, 16, 256], BF16, "band")
        P.dma("sp", braw.ap, self.dsa_band, writes=[braw.res])
        P.dma("sp", c31.ap, self.dsa_c31, writes=[c31.res])
        P.op("dve", E.tensor_tensor(band.ap, braw.ap, c31.ap.unsqueeze(2).to_broadcast([128, 16, 256]), ALU.subtract),
             reads=[braw.res, c31.res], writes=[band.res])
        ones = self.sb([128, 128], BF16, "ones")
        P.op("pool", E.memset(ones.ap, 1.0), writes=[ones.res])
        half = self.sb([128, 1], F32, "half")
        P.op("pool", E.memset(half.ap, 0.5), writes=[half.res])
        ckvT = self.sb([128, NT, 128], BF16, "ckvT")
        ckvF = self.sb([128, T], BF16, "ckvF")
        kiT = self.sb([64, T], BF16, "kiT")
        hT = self.sb([128, DC, T], BF16, "hT")
        ring_off = self.off
        self.xring = [self.sb([128, D], F32, "xr%d" % i) for i in range(4)]
        self.make_hT(x_src, L, s, 0, 1, hT)
        P.barrier()
        self.off = ring_off
        nb = [0]

        def gbank():
            nb[0] ^= 1
            return nb[0]
        ck = self.sb([128, 128], F32, "ck")
        cs = self.sb([128, 8], F32, "cs")
        for tile in range(NT):
            b = gbank()
            for dc in range(DC):
                P.op("pe", E.matmul(bank(b)[:, 0:128], hT.ap[:, dc, tile * 128:(tile + 1) * 128], Wc.ap[:, dc, :],
                                    start=(dc == 0), stop=(dc == DC - 1)), reads=[hT.r(tile // 4), Wc.res], writes=[psb[b]])
            P.op("act", E.activation(ck.ap, bank(b)[:, 0:128], AF.Square, accum_out=cs.ap[:, 0:1]), reads=[psb[b]], writes=[ck.res, cs.res])
            P.op("act", E.activation(cs.ap[:, 0:1], cs.ap[:, 0:1], AF.Sqrt, bias=1e-6, scale=1.0 / 128), reads=[cs.res], writes=[cs.res])
            P.op("dve", E.reciprocal(cs.ap[:, 0:1], cs.ap[:, 0:1]), reads=[cs.res], writes=[cs.res])
            P.op("dve", E.scalar_tensor_tensor(ckvT.ap[:, tile, :], bank(b)[:, 0:128], cs.ap[:, 0:1], gkv.ap, ALU.mult, ALU.mult),
                 reads=[psb[b], cs.res, gkv.res], writes=[ckvT.r(tile)])
            b2 = gbank()
            P.op("pe", E.transpose(bank(b2, BF16)[:, 0:128], ckvT.ap[:, tile, :], identb.ap), reads=[ckvT.r(tile), identb.res], writes=[psb[b2]])
            P.op("act", E.copy(ckvF.ap[:, tile * 128:(tile + 1) * 128], bank(b2, BF16)[:, 0:128]), reads=[psb[b2]], writes=[ckvF.r(tile)])
        for tb in range(4):
            b = gbank()
            for dc in range(DC):
                P.op("pe", E.matmul(bank(b)[0:64, :], Wki.ap[:, dc, :], hT.ap[:, dc, tb * 512:(tb + 1) * 512],
                                    start=(dc == 0), stop=(dc == DC - 1)), reads=[hT.r(tb), Wki.res], writes=[psb[b]])
            P.op("act", E.copy(kiT.ap[:, tb * 512:(tb + 1) * 512], bank(b)[0:64, :]), reads=[psb[b]], writes=[kiT.r(tb)])
        qTh = self.sb([64, 16, 512], BF16, "qTh")
        qlat = self.sb([128, 16, 512], BF16, "qlat")
        qiT = self.sb([64, 8, 512], BF16, "qiT")
        widx = self.sb([128, 4, 8], F32, "widx")
        acc = self.sb([128, T], F32, "acc")
        junk = self.sb([128, T], F32, "junk")
        relu = [self.sb([128, 512], F32, "relu%d" % i) for i in range(2)]
        mb = self.sb([128, T], BF16, "mb")
        mT = self.sb([128, NT, 128], BF16, "mT")
        eT = [self.sb([128, 4, 128], BF16, "eT%d" % i) for i in range(2)]
        olTn = self.sb([128, 4, 128], BF16, "olTn")
        rinv = self.sb([128, 512], F32, "rinv")
        ogt = self.sb([128, D], BF16, "ogt")
        bs = self.sb([128, 16], F32, "bs")
        ntl = min(NT, DBG["tiles"])
        for tg in range(4):
            if tg * 4 >= ntl:
                break
            tsl = slice(tg * 512, (tg + 1) * 512)
            for h in range(16):
                b = gbank()
                for dc in range(DC):
                    P.op("pe", E.matmul(bank(b)[0:64, :], Wq.ap[:, dc, h * 64:(h + 1) * 64], hT.ap[:, dc, tsl],
                                        start=(dc == 0), stop=(dc == DC - 1)), reads=[hT.r(tg), Wq.res], writes=[psb[b]])
                P.op(self.ev_eng(), E.copy(qTh.ap[:, h, :], bank(b)[0:64, :]) if self.rr["ev"] == 0 else E.tensor_copy(qTh.ap[:, h, :], bank(b)[0:64, :]),
                     reads=[psb[b]], writes=[qTh.r(h)])
            for h in range(16):
                b = gbank()
                P.op("pe", E.matmul(bank(b), wuk.ap[:, h, :], qTh.ap[:, h, :], start=True, stop=True, tile_position=(0, 0)),
                     reads=[wuk.res, qTh.r(h)], writes=[psb[b]])
                P.op("act", E.activation(qlat.ap[:, h, :], bank(b), AF.Identity, scale=SCL), reads=[psb[b]], writes=[qlat.r(h)])
            for hi in range(8):
                b = gbank()
                for dc in range(DC):
                    P.op("pe", E.matmul(bank(b)[0:64, :], Wqi.ap[:, dc, hi * 64:(hi + 1) * 64], hT.ap[:, dc, tsl],
                                        start=(dc == 0), stop=(dc == DC - 1)), reads=[hT.r(tg), Wqi.res], writes=[psb[b]])
                P.op("dve", E.tensor_copy(qiT.ap[:, hi, :], bank(b)[0:64, :]), reads=[psb[b]], writes=[qiT.r(hi)])
            for tt in range(4):
                b = gbank()
                tile = tg * 4 + tt
                for dc in range(DC):
                    P.op("pe", E.matmul(bank(b)[:, 0:8], hT.ap[:, dc, tile * 128:(tile + 1) * 128], Wwi.ap[:, dc, :],
                                        start=(dc == 0), stop=(dc == DC - 1)), reads=[hT.r(tg), Wwi.res], writes=[psb[b]])
                P.op("act", E.activation(widx.ap[:, tt, :], bank(b)[:, 0:8], AF.Identity, scale=WSC), reads=[psb[b]], writes=[widx.r(tt)])
            qlr = [qlat.r(h) for h in range(16)]
            qir = [qiT.r(h) for h in range(8)]
            for tt in range(4):
                tile = tg * 4 + tt
                if tile >= ntl:
                    break
                nsb = tile // 4 + 1
                ncol = nsb * 512
                tcs = slice(tt * 128, (tt + 1) * 128)
                nr = 0
                for sbk in range(nsb):
                    for hi in range(8):
                        b = gbank()
                        P.op("pe", E.matmul(bank(b), qiT.ap[:, hi, tcs], kiT.ap[:, sbk * 512:(sbk + 1) * 512], start=True, stop=True,
                                            tile_position=(0, 0)), reads=[qiT.r(hi), kiT.r(sbk)], writes=[psb[b]])
                        rl = relu[nr % 2]
                        nr += 1
                        P.op("act", E.activation(rl.ap, bank(b), AF.Relu), reads=[psb[b]], writes=[rl.res])
                        dst = acc.ap[:, sbk * 512:(sbk + 1) * 512]
                        if hi == 0:
                            P.op("dve", E.tensor_scalar(dst, rl.ap, widx.ap[:, tt, 0:1], None, ALU.mult), reads=[rl.res, widx.r(tt)], writes=[acc.r(sbk)])
                        else:
                            P.op("dve", E.scalar_tensor_tensor(dst, rl.ap, widx.ap[:, tt, hi:hi + 1], dst, ALU.mult, ALU.add),
                                 reads=[rl.res, widx.r(tt), acc.r(sbk)], writes=[acc.r(sbk)])
                accr = [acc.r(k_) for k_ in range(nsb)]
                if tile >= 2:
                    P.op("dve", E.tensor_reduce(bs.ap[:, 0:1], acc.ap[:, 0:ncol], AX.X, ALU.min), reads=accr, writes=[bs.res])
                    P.op("dve", E.tensor_reduce(bs.ap[:, 1:2], acc.ap[:, 0:ncol], AX.X, ALU.max), reads=accr, writes=[bs.res])
                    P.op("dve", E.tensor_scalar_add(bs.ap[:, 0:1], bs.ap[:, 0:1], -1e-3), reads=[bs.res], writes=[bs.res])
                    P.op("dve", E.tensor_scalar_add(bs.ap[:, 1:2], bs.ap[:, 1:2], 1e-3), reads=[bs.res], writes=[bs.res])
                lastb = acc.ap[:, (nsb - 1) * 512:nsb * 512]
                P.op("dve", E.tensor_tensor(lastb, lastb, caus.ap[:, tile % 4, :], ALU.add), reads=accr + [caus.res, bs.res], writes=[acc.r(nsb - 1)])
                if tile >= 2:
                    for it in range(26):
                        P.op("dve", E.scalar_tensor_tensor(bs.ap[:, 2:3], bs.ap[:, 0:1], bs.ap[:, 1:2], half.ap, ALU.add, ALU.mult),
                             reads=[bs.res, half.res], writes=[bs.res])
                        P.op("dve", E.tensor_scalar(junk.ap[:, 0:ncol], acc.ap[:, 0:ncol], bs.ap[:, 2:3], 0.0, ALU.is_ge, ALU.add, accum_out=bs.ap[:, 3:4]),
                             reads=accr + [bs.res], writes=[junk.res, bs.res])
                        P.op("dve", E.tensor_scalar(bs.ap[:, 4:5], bs.ap[:, 3:4], 256.0, None, ALU.is_ge), reads=[bs.res], writes=[bs.res])
                        P.op("dve", E.tensor_tensor(bs.ap[:, 5:6], bs.ap[:, 2:3], bs.ap[:, 0:1], ALU.subtract), reads=[bs.res], writes=[bs.res])
                        P.op("dve", E.scalar_tensor_tensor(bs.ap[:, 0:1], bs.ap[:, 5:6], bs.ap[:, 4:5], bs.ap[:, 0:1], ALU.mult, ALU.add),
                             reads=[bs.res], writes=[bs.res])
                        P.op("dve", E.tensor_tensor(bs.ap[:, 5:6], bs.ap[:, 1:2], bs.ap[:, 2:3], ALU.subtract), reads=[bs.res], writes=[bs.res])
                        P.op("dve", E.scalar_tensor_tensor(bs.ap[:, 1:2], bs.ap[:, 5:6], bs.ap[:, 4:5], bs.ap[:, 2:3], ALU.mult, ALU.add),
                             reads=[bs.res], writes=[bs.res])
                else:
                    P.op("dve", E.memset(bs.ap[:, 0:1], -1e29), reads=[bs.res], writes=[bs.res])
                P.op("dve", E.tensor_scalar(mb.ap[:, 0:ncol], acc.ap[:, 0:ncol], bs.ap[:, 0:1], -30000.0, ALU.is_lt, ALU.mult),
                     reads=accr + [bs.res], writes=[mb.res])
                for sg in range(tile + 1):
                    b = gbank()
                    P.op("pe", E.transpose(bank(b, BF16)[:, 0:128], mb.ap[:, sg * 128:(sg + 1) * 128], identb.ap), reads=[mb.res, identb.res], writes=[psb[b]])
                    P.op("act", E.copy(mT.ap[:, sg, :], bank(b, BF16)[:, 0:128]), reads=[psb[b]], writes=[mT.r(sg)])
                ne = 0
                for hg in range(4):
                    bo = 4 + hg % 2
                    br = 6 + hg % 2
                    for sg in range(tile + 1):
                        bl = 2 + ne % 2
                        e_ = eT[ne % 2]
                        ne += 1
                        for hh in range(4):
                            h = hg * 4 + hh
                            reg = bank(bl)[:, hh * 128:(hh + 1) * 128]
                            wband = (sg == tile) or (sg == tile - 1)
                            P.op("pe", E.matmul(reg, ckvF.ap[:, sg * 128:(sg + 1) * 128], qlat.ap[:, h, tcs], start=True, stop=False),
                                 reads=[ckvF.r(sg), qlat.r(h)], writes=[psb[bl]])
                            P.op("pe", E.matmul(reg, identb.ap, mT.ap[:, sg, :], start=False, stop=(not wband)),
                                 reads=[identb.res, mT.r(sg)], writes=[psb[bl]])
                            if wband:
                                w_ = 0 if sg == tile else 1
                                P.op("pe", E.matmul(reg, identb.ap, band.ap[:, h, w_ * 128:(w_ + 1) * 128], start=False, stop=True),
                                     reads=[identb.res, band.res], writes=[psb[bl]])
                        P.op("act", E.activation(e_.ap, bank(bl).rearrange("p (a b) -> p a b", b=128), AF.Exp), reads=[psb[bl]], writes=[e_.res])
                        e2 = e_.ap.rearrange("p a b -> p (a b)")
                        P.op("pe", E.matmul(bank(bo), ckvT.ap[:, sg, :], e2, start=(sg == 0), stop=(sg == tile)),
                             reads=[ckvT.r(sg), e_.res], writes=[psb[bo]])
                        P.op("pe", E.matmul(bank(br), ones.ap, e2, start=(sg == 0), stop=(sg == tile)),
                             reads=[ones.res, e_.res], writes=[psb[br]])
                    P.op("dve", E.reciprocal(rinv.ap, bank(br)), reads=[psb[br]], writes=[rinv.res])
                    P.op("dve", E.tensor_tensor(olTn.ap.rearrange("p a b -> p (a b)"), bank(bo), rinv.ap, ALU.mult),
                         reads=[psb[bo], rinv.res], writes=[olTn.res])
                    for hh in range(4):
                        h = hg * 4 + hh
                        bq = gbank()
                        P.op("pe", E.matmul(bank(bq)[:, 0:64], olTn.ap[:, hh, :], wuv.ap[:, h, :], start=True, stop=True),
                             reads=[olTn.res, wuv.res], writes=[psb[bq]])
                        P.op("act", E.copy(ogt.ap[:, h * 64:(h + 1) * 64], bank(bq)[:, 0:64]), reads=[psb[bq]], writes=[ogt.res])
                P.dma("sp", self.ogdram[s, tile * 128:(tile + 1) * 128, :], ogt.ap, reads=[ogt.res], writes=[self.og_res[s][tile]])
        self.phase_reset()
        Wo = self.sb([128, DC, D], BF16, "Wo")
        P.dma("pool", Wo.ap, self.dsa_w_out[j].rearrange("(k p) n -> p k n", p=128), writes=[Wo.res])
        self.out_proj_ln(L, s, Wo, self.ogdram[s], self.og_res[s], x_src, x_dst, last)
        self.phase_reset()

    def build(self):
        P = self.P
        self.prologue()
        for s in range(NSEQ):
            src = self.x_in[s]
            nsub = len(self.plan)
            for i, (kind, L) in enumerate(self.plan):
                last = (i == nsub - 1)
                dst = self.out[s] if last else self.xbuf[s]
                if kind == "ffn":
                    self.ffn(L, s, src, dst, last)
                elif kind == "rwkv":
                    self.rwkv(L, s, src, dst, last)
                elif kind == "dsa":
                    self.dsa(L, s, src, dst, last)
                src = self.xbuf[s]
        P.emit()
        self.es.close()
        return self.nc


FULL_PLAN = [("rwkv", 0), ("ffn", 0), ("dsa", 1), ("ffn", 1), ("rwkv", 2), ("ffn", 2), ("dsa", 3), ("ffn", 3)]


def make_consts():
    c = np.zeros((128, NCONST), np.float32)
    s_ = np.arange(128)[:, None]
    t_ = np.arange(128)[None, :]
    same = (s_ // 64) == (t_ // 64)
    su = (same & (s_ < t_)).astype(np.float32)
    iu = (same & (s_ <= t_)).astype(np.float32)
    c[:, 0:128] = su
    c[:, 128:256] = iu
    c[:, 256:384] = su
    c[:, 384:512] = iu
    c[:, 512:640] = su.T
    c[:, 640:768] = iu
    c[63, 768] = 1.0
    c[127, 769] = 1.0
    c[:, 832:896] = np.tile(np.eye(64, dtype=np.float32), (2, 1))
    c[0:64, 896:960] = 1.0
    c[64:128, 960:1024] = 1.0
    return c


def host_inputs(inputs, plan):
    f = lambda a: np.ascontiguousarray(np.asarray(a, dtype=np.float32))
    common = {
        "ada_w": f(inputs["ada_w"]), "ada_b": f(inputs["ada_b"]),
        "ln_g": f(inputs["ln_g"]), "ln_b": f(inputs["ln_b"]),
        "ffn_w_in": f(inputs["ffn_w_in"]), "ffn_w_out": f(inputs["ffn_w_out"]),
        "ident_f": np.eye(128, dtype=np.float32),
        "consts": make_consts(),
    }
    kinds = set(k for k, _ in plan)
    if "dsa" in kinds:
        for k_ in ("dsa_w_in", "dsa_kv_norm", "dsa_w_uk", "dsa_w_uv", "dsa_w_out"):
            common[k_] = f(inputs[k_])
        rb = f(inputs["rel_bias"])
        tt_ = np.arange(128)[None, :]; ss_ = np.arange(128)[:, None]
        caus = np.zeros((128, 4, 512), np.float32)
        for q4 in range(4):
            cc = np.arange(512)[None, :]
            caus[:, q4, :] = np.where(cc <= q4 * 128 + np.arange(128)[:, None], 0.0, -1e30)
        common["dsa_caus"] = caus
        band = np.zeros((128, 16, 2, 128), np.float32)
        for w_ in range(2):
            n = np.maximum(tt_ - ss_ + 128 * w_, 0)
            nf = np.maximum(n, 1).astype(np.float32)
            large = 16 + (np.log(nf / 16) / math.log(128 / 16) * 16).astype(np.int32)
            bucket = np.where(n < 16, n, np.minimum(large, 31))
            band[:, :, w_, :] = rb[bucket].transpose(0, 2, 1)
        common["dsa_band"] = np.ascontiguousarray(band.reshape(128, 16, 256))
        common["dsa_c31"] = np.ascontiguousarray(np.broadcast_to(rb[31][None, :], (128, 16)))
    if "rwkv" in kinds:
        mu = f(inputs["rwkv_mu"])
        common["rw_mu"] = np.ascontiguousarray(mu.reshape(2, 6, DC, 128).transpose(0, 3, 1, 2))
        common["rwkv_w_rkv"] = f(inputs["rwkv_w_rkv"])
        common["rwkv_w1"] = f(inputs["rwkv_w1"])
        common["rw_w2a"] = np.concatenate([f(inputs["rwkv_w2"]), f(inputs["rwkv_w0"])[:, None, :]], axis=1)
        common["rwkv_a1"] = f(inputs["rwkv_a1"])
        common["rw_a2a"] = np.concatenate([f(inputs["rwkv_a2"]), f(inputs["rwkv_a0"])[:, None, :]], axis=1)
        common["rwkv_v1"] = f(inputs["rwkv_v1"])
        common["rw_v2a"] = np.concatenate([f(inputs["rwkv_v2"]), f(inputs["rwkv_v0"])[:, None, :]], axis=1)
        common["rwkv_g1"] = f(inputs["rwkv_g1"])
        common["rwkv_g2"] = f(inputs["rwkv_g2"])
        common["rw_vec"] = np.stack([f(inputs["rwkv_k_k"]), f(inputs["rwkv_k_a"]), f(inputs["rwkv_r_k"]).reshape(2, D),
                                     f(inputs["rwkv_lnx_g"]), f(inputs["rwkv_lnx_b"])], axis=1)
        common["rwkv_w_out"] = f(inputs["rwkv_w_out"])
    x = f(inputs["x"])
    c = f(inputs["c"])
    maps = []
    for core in range(8):
        m = dict(common)
        m["x"] = x[core * NSEQ:(core + 1) * NSEQ]
        cc = c[core * NSEQ:(core + 1) * NSEQ]
        m["c_col"] = np.ascontiguousarray(cc.reshape(NSEQ, DC, 128).transpose(0, 2, 1))
        maps.append(m)
    return maps


def run(inputs, plan=FULL_PLAN, ncores=8):
    b = Builder(plan)
    nc = b.build()
    print("instr stats", b.P.stats())
    maps = host_inputs(inputs, plan)[:ncores]
    res = run_bass_kernel_spmd(nc, maps, core_ids=list(range(ncores)))
    if DBG.get("dump"):
        np.save("_dbg_og.npy", np.asarray(res.results[0]["ogdram"]).astype(np.float32))
    return np.concatenate([r["out"] for r in res.results], axis=0)


def kernel(**inputs):
    return run(inputs, FULL_PLAN)
```

```python
import contextlib
import math
import numpy as np
import concourse.bass as bass
import concourse.mybir as mybir
from concourse.bass_utils import run_bass_kernel_spmd

F32 = mybir.dt.float32
BF16 = mybir.dt.bfloat16
U8 = mybir.dt.uint8
AF = mybir.ActivationFunctionType
ALU = mybir.AluOpType
AX = mybir.AxisListType

D = 1024
T = 2048
NT = T // 128
DC = D // 128
DEPTH = 4
FH = 2816
ALPHA = (2 * DEPTH) ** 0.25
LN_EPS = 1e-5
GN_EPS = 64 * 1e-5
NCONST = 1024
WDECAY = math.exp(-0.5)
NSEQ = 2
DBG = {"stage": 99, "tiles": NT, "sub": 99}
ENGS = ("pe", "act", "pool", "dve", "sp")
DT_SIZE = {F32: 4, BF16: 2, U8: 1}


class _Rec:
    def __getattr__(self, name):
        def f(*args, **kw):
            return (name, args, kw)
        return f


E = _Rec()


class Ev:
    __slots__ = ("kind", "eng", "seq", "needed", "value")

    def __init__(self, kind, eng, seq):
        self.kind = kind
        self.eng = eng
        self.seq = seq
        self.needed = False
        self.value = None


class Res:
    __slots__ = ("name", "w", "r", "excl")

    def __init__(self, name="", excl=False):
        self.name = name
        self.w = None
        self.r = []
        self.excl = excl


class Prog:
    def __init__(self, nc, n_dma_sems=48):
        self.nc = nc
        self.streams = {e: [] for e in ENGS}
        self.n_dma_sems = n_dma_sems
        self.dma_last = [None] * n_dma_sems
        self.dma_cnt = [0] * n_dma_sems
        self.dma_rr = 0
        self.dma_rr_sw = 0
        self.known = {e: {} for e in ENGS}
        self.last_ev = {e: None for e in ENGS}

    def _collect(self, eng, reads, writes, include_same=False):
        deps = []
        for r in reads:
            if r.w is not None:
                deps.append(r.w)
        for w in writes:
            if w.w is not None:
                deps.append(w.w)
            deps.extend(w.r)
        kn = self.known[eng]
        best = {}
        for d in deps:
            if d.kind == "eng" and d.eng == eng and not include_same:
                continue
            key = (d.kind, d.eng)
            if kn.get(key, -1) >= d.seq:
                continue
            if key not in best or best[key].seq < d.seq:
                best[key] = d
        out = []
        for key, d in best.items():
            kn[key] = d.seq
            d.needed = True
            out.append(d)
        return out

    def op(self, eng, fn, reads=(), writes=()):
        ex = [r for r in reads if r.excl]
        if ex:
            writes = list(writes) + ex
        waits = self._collect(eng, reads, writes)
        if eng != "pe":
            kn = self.known[eng]
            key = ("eng", eng)
            m = None
            for r in reads:
                d = r.w
                if d is not None and d.kind == "eng" and d.eng == eng and kn.get(key, -1) < d.seq:
                    if m is None or m.seq < d.seq:
                        m = d
            if m is not None:
                kn[key] = m.seq
                m.needed = True
                waits.append(m)
        st = self.streams[eng]
        ev = Ev("eng", eng, len(st))
        st.append((fn, waits, ev))
        self.last_ev[eng] = ev
        for r in reads:
            r.r.append(ev)
        for w in writes:
            w.w = ev
            w.r = []
        return ev

    def dma(self, q, out_ap, in_ap, reads=(), writes=(), **kw):
        nh = self.n_dma_sems // 2
        if q == "pool":
            k = nh + self.dma_rr_sw
            self.dma_rr_sw = (self.dma_rr_sw + 1) % (self.n_dma_sems - nh)
        else:
            k = self.dma_rr
            self.dma_rr = (self.dma_rr + 1) % nh
        waits = self._collect(q, reads, writes, include_same=True)
        prev = self.dma_last[k]
        if prev is not None:
            key = ("dma", k)
            if self.known[q].get(key, -1) < prev.seq:
                self.known[q][key] = prev.seq
                waits.append(prev)
        self.dma_cnt[k] += 1
        ev = Ev("dma", k, self.dma_cnt[k])
        ev.needed = True
        ev.value = 16 * self.dma_cnt[k]
        self.dma_last[k] = ev

        def fn(e, out_ap=out_ap, in_ap=in_ap, kw=kw):
            return e.dma_start(out=out_ap, in_=in_ap, **kw)
        self.streams[q].append((fn, waits, ev))
        for r in reads:
            r.r.append(ev)
        for w in writes:
            w.w = ev
            w.r = []
        return ev

    def barrier(self):
        evs = [self.last_ev[e] for e in ENGS if self.last_ev[e] is not None]
        evs += [d for d in self.dma_last if d is not None]
        for e in ENGS:
            waits = []
            kn = self.known[e]
            for d in evs:
                if d.kind == "eng" and d.eng == e:
                    continue
                key = (d.kind, d.eng)
                if kn.get(key, -1) >= d.seq:
                    continue
                kn[key] = d.seq
                d.needed = True
                waits.append(d)
            if waits:
                self.streams[e].append((None, waits, None))

    def emit(self):
        nc = self.nc
        self.barrier()
        for e in ENGS:
            c = 0
            for fn, waits, ev in self.streams[e]:
                if ev is not None and ev.kind == "eng" and ev.needed:
                    c += 1
                    ev.value = c
        with contextlib.ExitStack() as es:
            esem = {e: es.enter_context(nc.semaphore("s_" + e)) for e in ENGS}
            dsem = [es.enter_context(nc.semaphore("d_%d" % k)) for k in range(self.n_dma_sems)]

            def semof(ev):
                return esem[ev.eng] if ev.kind == "eng" else dsem[ev.eng]

            block = es.enter_context(nc.Block())

            def make(ename):
                def body(eh):
                    for fn, waits, ev in self.streams[ename]:
                        for w in waits:
                            eh.wait_ge(semof(w), w.value)
                        if fn is None:
                            continue
                        if isinstance(fn, tuple):
                            ins = getattr(eh, fn[0])(*fn[1], **fn[2])
                        else:
                            ins = fn(eh)
                        if ev.kind == "dma":
                            ins.then_inc(dsem[ev.eng], 16)
                        elif ev.needed:
                            ins.then_inc(esem[ename], 1)
                return body

            block.tensor(make("pe"))
            block.scalar(make("act"))
            block.gpsimd(make("pool"))
            block.vector(make("dve"))
            block.sync(make("sp"))

    def stats(self):
        return {e: len(self.streams[e]) for e in ENGS}


class Tile:
    def __init__(self, ap, name=""):
        self.ap = ap
        self.res = Res(name)
        self.sub = {}

    def r(self, key=None):
        if key is None:
            return self.res
        if key not in self.sub:
            self.sub[key] = Res()
        return self.sub[key]


class Builder:
    def __init__(self, plan, debug=False):
        self.plan = plan
        self.nc = nc = bass.Bass("TRN2", target_bir_lowering=False)
        self.P = Prog(nc)
        self.es = contextlib.ExitStack()
        self.ARENA = 206 * 1024
        self.arena = self.es.enter_context(nc.sbuf_tensor("arena", [128, self.ARENA], U8))
        self.psum = self.es.enter_context(nc.psum_tensor("psum", [128, 8, 512], F32))
        self.off = 0
        self.persist_off = 0
        self.rr = {"ev": 0}
        self.psb = [Res("bank%d" % i, excl=True) for i in range(8)]
        self.moddram_res = Res("moddram")
        self.outres = Res("out")
        self.xres_ = [[Res() for _ in range(NT)] for _ in range(NSEQ)]
        self.vf_res = [[Res() for _ in range(NT)] for _ in range(NSEQ)]
        self.og_res = [[Res() for _ in range(NT)] for _ in range(NSEQ)]
        self.decl_io()

    def sb(self, shape, dt, name=""):
        n = int(np.prod(shape[1:])) * DT_SIZE[dt]
        n_al = (n + 63) // 64 * 64
        assert self.off + n_al <= self.ARENA, ("SBUF arena overflow", name, self.off, n_al)
        v = self.arena[0:shape[0], self.off:self.off + n].bitcast(dt)
        if not hasattr(self, 'alloc_log'):
            self.alloc_log = []
        self.alloc_log.append((name, self.off, n, dt, list(shape)))
        self.off += n_al
        if len(shape) == 3:
            v = v.rearrange("p (a b) -> p a b", b=shape[2])
        elif len(shape) == 4:
            v = v.rearrange("p (a b c) -> p a b c", b=shape[2], c=shape[3])
        elif len(shape) == 5:
            v = v.rearrange("p (a b c d) -> p a b c d", b=shape[2], c=shape[3], d=shape[4])
        return Tile(v, name)

    def phase_reset(self):
        self.P.barrier()
        self.off = self.persist_off

    def bank(self, i, dt=F32):
        v = self.psum[:, i, :]
        if dt == BF16:
            v = v.bitcast(BF16)
        return v

    def dram_in(self, name, shape, dt=F32):
        return self.nc.dram_tensor(name, list(shape), dt, kind="ExternalInput").ap()

    def dram_tmp(self, name, shape, dt=F32):
        return self.nc.dram_tensor(name, list(shape), dt, kind="Internal").ap()

    def decl_io(self):
        di = self.dram_in
        self.x_in = di("x", [NSEQ, T, D])
        self.c_col = di("c_col", [NSEQ, 128, DC])
        self.ada_w = di("ada_w", [DEPTH, D, 6 * D])
        self.ada_b = di("ada_b", [DEPTH, 6 * D])
        self.ln_g = di("ln_g", [DEPTH, 2, D])
        self.ln_b = di("ln_b", [DEPTH, 2, D])
        self.ffn_w_in = di("ffn_w_in", [DEPTH, D, 2 * FH])
        self.ffn_w_out = di("ffn_w_out", [DEPTH, FH, D])
        self.ident_f = di("ident_f", [128, 128])
        self.consts = di("consts", [128, NCONST])
        kinds = set(k for k, _ in self.plan)
        if "dsa" in kinds:
            self.dsa_w_in = di("dsa_w_in", [2, D, 1736])
            self.dsa_kv_norm = di("dsa_kv_norm", [2, 128])
            self.dsa_w_uk = di("dsa_w_uk", [2, 16, 64, 128])
            self.dsa_w_uv = di("dsa_w_uv", [2, 16, 128, 64])
            self.dsa_w_out = di("dsa_w_out", [2, D, D])
            self.dsa_caus = di("dsa_caus", [128, 4, 512])
            self.dsa_band = di("dsa_band", [128, 16, 256])
            self.dsa_c31 = di("dsa_c31", [128, 16])
            if "rwkv" not in kinds:
                self.ogdram = (self.nc.dram_tensor("ogdram", [NSEQ, T, D], BF16, kind="ExternalOutput").ap() if DBG.get("dump") else self.dram_tmp("ogdram", [NSEQ, T, D], BF16))
        if "rwkv" in kinds:
            self.rw_mu = di("rw_mu", [2, 128, 6, DC])
            self.rw_wrkv = di("rwkv_w_rkv", [2, 3, D, D])
            self.rw_w1 = di("rwkv_w1", [2, D, 64])
            self.rw_w2a = di("rw_w2a", [2, 65, D])
            self.rw_a1 = di("rwkv_a1", [2, D, 64])
            self.rw_a2a = di("rw_a2a", [2, 65, D])
            self.rw_v1 = di("rwkv_v1", [1, D, 32])
            self.rw_v2a = di("rw_v2a", [1, 33, D])
            self.rw_g1 = di("rwkv_g1", [2, D, 160])
            self.rw_g2 = di("rwkv_g2", [2, 160, D])
            self.rw_vec = di("rw_vec", [2, 5, D])
            self.rw_wout = di("rwkv_w_out", [2, D, D])
            self.vfirst = self.dram_tmp("vfirst", [NSEQ, T, D])
            self.ogdram = (self.nc.dram_tensor("ogdram", [NSEQ, T, D], BF16, kind="ExternalOutput").ap() if DBG.get("dump") else self.dram_tmp("ogdram", [NSEQ, T, D], BF16))
        self.out = self.nc.dram_tensor("out", [NSEQ, T, D], F32, kind="ExternalOutput").ap()
        self.xbuf = self.dram_tmp("xbuf", [NSEQ, T, D])
        self.moddram = self.dram_tmp("moddram", [DEPTH, NSEQ, 2, D])

    def ev_eng(self):
        self.rr["ev"] ^= 1
        return "dve" if self.rr["ev"] else "act"

    def prologue(self):
        P = self.P
        self.ident = self.sb([128, 128], F32, "ident")
        P.dma("sp", self.ident.ap, self.ident_f, writes=[self.ident.res])
        self.identb = self.sb([128, 128], BF16, "identb")
        P.op("dve", E.tensor_copy(self.identb.ap, self.ident.ap),
             reads=[self.ident.res], writes=[self.identb.res])
        self.modcol = self.sb([128, DEPTH * NSEQ * 4, DC], F32, "modcol")
        self.persist_off = self.off
        ccol = self.sb([128, NSEQ, DC], F32, "ccol")
        cond = self.sb([128, DC, NSEQ], F32, "cond")
        P.dma("sp", ccol.ap, self.c_col.rearrange("s p k -> p s k"), writes=[ccol.res])
        for s in range(NSEQ):
            P.op("act", E.activation(cond.ap[:, :, s], ccol.ap[:, s, :], AF.Silu),
                 reads=[ccol.res], writes=[cond.res])
        wbuf = [self.sb([128, DC, 512], F32, "adaw%d" % i) for i in range(2)]
        bbuf = [self.sb([NSEQ, 512], F32, "adab%d" % i) for i in range(2)]
        rowb = [self.sb([NSEQ, 512], F32, "modrow%d" % i) for i in range(2)]
        cnt = 0
        for L in range(DEPTH):
            for ch in range(12):
                wb, bb, rb = wbuf[cnt % 2], bbuf[cnt % 2], rowb[cnt % 2]
                pb = cnt % 2
                cnt += 1
                P.dma("sp", wb.ap, self.ada_w[L, :, ch * 512:(ch + 1) * 512].rearrange("(k p) n -> p k n", p=128),
                      writes=[wb.res])
                P.dma("act", bb.ap, self.ada_b[L:L + 1, ch * 512:(ch + 1) * 512].broadcast_to([NSEQ, 512]),
                      writes=[bb.res])
                pr = self.psb[pb]
                for k in range(DC):
                    P.op("pe", E.matmul(self.bank(pb)[0:NSEQ, :], cond.ap[:, k, :], wb.ap[:, k, :],
                                                                  start=(k == 0), stop=(k == DC - 1)),
                         reads=[cond.res, wb.res], writes=[pr])
                vec = ch // 2
                half = ch % 2
                addone = 0.0 if vec in (0, 3) else 1.0
                P.op("dve", E.scalar_tensor_tensor(
                    rb.ap, self.bank(pb)[0:NSEQ, :], addone, bb.ap, ALU.add, ALU.add),
                    reads=[pr, bb.res], writes=[rb.res])
                if vec in (2, 5):
                    P.dma("sp", self.moddram[L, :, 0 if vec == 2 else 1, half * 512:(half + 1) * 512], rb.ap,
                          reads=[rb.res], writes=[self.moddram_res])
                else:
                    vi = {0: 0, 1: 1, 3: 2, 4: 3}[vec]
                    pc = self.psb[2 + pb]
                    for s in range(NSEQ):
                        for kk in range(4):
                            P.op("pe", E.matmul(
                                self.bank(2 + pb)[:, s * 4 + kk:s * 4 + kk + 1], rb.ap[:, kk * 128:(kk + 1) * 128],
                                self.ident.ap[0:NSEQ, s:s + 1], start=True, stop=True),
                                reads=[rb.res, self.ident.res], writes=[pc])
                    for s in range(NSEQ):
                        idx = (L * NSEQ + s) * 4 + vi
                        P.op("dve", E.tensor_copy(
                            self.modcol.ap[:, idx, half * 4:(half + 1) * 4], self.bank(2 + pb)[:, s * 4:(s + 1) * 4]),
                            reads=[pc], writes=[self.modcol.res])
        self.phase_reset()

    def mcol(self, L, s, vi, k):
        return self.modcol.ap[:, (L * NSEQ + s) * 4 + vi, k:k + 1]

    def make_hT(self, x_src, L, s, vsh, vsc, hT):
        P = self.P
        xt = self.xring
        n = 0
        for tg in range(4):
            tiles = []
            for j in range(4):
                t_ = xt[n % 4]
                n += 1
                tok = (tg * 4 + j) * 128
                P.dma("sp", t_.ap, x_src[tok:tok + 128, :], reads=[self.xres(s, tg * 4 + j)], writes=[t_.res])
                tiles.append(t_)
            for dc in range(DC):
                pb = dc % 2
                pr = self.psb[pb]
                for j in range(4):
                    P.op("pe", E.transpose(
                        self.bank(pb)[:, j * 128:(j + 1) * 128], tiles[j].ap[:, dc * 128:(dc + 1) * 128], self.ident.ap),
                        reads=[tiles[j].res, self.ident.res], writes=[pr])
                eng = self.ev_eng()
                dst = hT.ap[:, dc, tg * 512:(tg + 1) * 512]
                sc = self.mcol(L, s, vsc, dc)
                sh = self.mcol(L, s, vsh, dc)
                if eng == "dve":
                    P.op("dve", E.tensor_scalar(
                        dst, self.bank(pb), sc, sh, ALU.mult, ALU.add),
                        reads=[pr, self.modcol.res], writes=[hT.r(tg)])
                else:
                    P.op("act", E.activation(
                        dst, self.bank(pb), AF.Identity, bias=sh, scale=sc),
                        reads=[pr, self.modcol.res], writes=[hT.r(tg)])

    def load_ln_consts(self, L, s, which, need_z=True):
        P = self.P
        G = self.sb([128, D], F32, "G")
        g = self.sb([128, D], F32, "lng")
        b = self.sb([128, D], F32, "lnb")
        P.dma("sp", G.ap, self.moddram[L, s, which:which + 1, :].broadcast_to([128, D]),
              reads=[self.moddram_res], writes=[G.res])
        P.dma("sp", g.ap, self.ln_g[L, which:which + 1, :].broadcast_to([128, D]), writes=[g.res])
        P.dma("sp", b.ap, self.ln_b[L, which:which + 1, :].broadcast_to([128, D]), writes=[b.res])
        self.lnc = (G, g, b)
        self.ln_x = self.xring
        self.ln_z = [self.sb([128, D], F32, "lnz%d" % i) for i in range(3)] if need_z else None
        self.ln_st = [self.sb([128, 16], F32, "lnst%d" % i) for i in range(4)]
        self.ln_n = 0

    def ln_epilogue(self, s, tile, y_aps, y_res, x_src, x_dst, dst_res=None, z_tile=None):
        P = self.P
        G, g, b = self.lnc
        i = self.ln_n
        self.ln_n += 1
        xt, st = self.ln_x[i % 4], self.ln_st[i % 4]
        z = self.ln_z[i % 3] if z_tile is None else z_tile
        tok = tile * 128
        P.dma("sp", xt.ap, x_src[tok:tok + 128, :], reads=[self.xres(s, tile)], writes=[xt.res])
        for ap, c0, w in y_aps:
            P.op("dve", E.tensor_tensor(z.ap[:, c0:c0 + w], ap, G.ap[:, c0:c0 + w], ALU.mult),
                 reads=list(y_res) + [G.res], writes=[z.res])
        P.op("dve", E.scalar_tensor_tensor(z.ap, xt.ap, float(ALPHA), z.ap, ALU.mult, ALU.add),
             reads=[xt.res, z.res], writes=[z.res])
        P.op("dve", E.bn_stats(st.ap[:, 0:6], z.ap[:, 0:512]), reads=[z.res], writes=[st.r("a")])
        P.op("dve", E.bn_stats(st.ap[:, 6:12], z.ap[:, 512:1024]), reads=[z.res], writes=[st.r("b")])
        P.op("dve", E.bn_aggr(st.ap[:, 12:14], st.ap[:, 0:12]), reads=[st.r("a"), st.r("b")], writes=[st.r("c")])
        P.op("act", E.activation(st.ap[:, 14:15], st.ap[:, 13:14], AF.Sqrt, bias=LN_EPS),
             reads=[st.r("c")], writes=[st.r("d")])
        P.op("dve", E.reciprocal(st.ap[:, 14:15], st.ap[:, 14:15]), reads=[st.r("d")], writes=[st.r("d")])
        P.op("dve", E.tensor_scalar(st.ap[:, 15:16], st.ap[:, 12:13], st.ap[:, 14:15], -1.0, ALU.mult, ALU.mult),
             reads=[st.r("c"), st.r("d")], writes=[st.r("e")])
        P.op("act", E.activation(z.ap, z.ap, AF.Identity, bias=st.ap[:, 15:16], scale=st.ap[:, 14:15]),
             reads=[z.res, st.r("d"), st.r("e")], writes=[z.res])
        P.op("pool", E.tensor_tensor(z.ap, z.ap, g.ap, ALU.mult), reads=[z.res, g.res], writes=[z.res])
        P.op("pool", E.tensor_tensor(z.ap, z.ap, b.ap, ALU.add), reads=[z.res, b.res], writes=[z.res])
        P.dma("pool", x_dst[tok:tok + 128, :], z.ap, reads=[z.res],
              writes=[dst_res if dst_res is not None else self.xres(s, tile)])

    def xres(self, s, tile):
        return self.xres_[s][tile]

    def ffn(self, L, s, x_src, x_dst, last=False):
        P = self.P
        hT = self.sb([128, DC, T], BF16, "hT")
        self.xring = [self.sb([128, D], F32, "xr%d" % i) for i in range(4)]
        self.make_hT(x_src, L, s, 2, 3, hT)
        yacc = self.sb([128, NT, D], F32, "yacc")
        wg = [self.sb([128, DC, 512], BF16, "wg%d" % i) for i in range(2)]
        wu = [self.sb([128, DC, 512], BF16, "wu%d" % i) for i in range(2)]
        wo = [self.sb([128, 4, D], BF16, "wo%d" % i) for i in range(2)]
        hh = [self.sb([128, 4, 512], BF16, "hh%d" % i) for i in range(2)]
        sg = [self.sb([128, 512], F32, "sg%d" % i) for i in range(2)]
        groups = [(g * 512, 512) for g in range(5)] + [(2560, 256)]
        nhh = 0
        nsg = 0
        npy = 0
        for gi, (h0, gs) in enumerate(groups):
            nhc = gs // 128
            a, u, o = wg[gi % 2], wu[gi % 2], wo[gi % 2]
            P.dma("pool", a.ap[:, :, 0:gs], self.ffn_w_in[L, :, h0:h0 + gs].rearrange("(k p) n -> p k n", p=128),
                  writes=[a.res])
            P.dma("pool", u.ap[:, :, 0:gs], self.ffn_w_in[L, :, FH + h0:FH + h0 + gs].rearrange("(k p) n -> p k n", p=128),
                  writes=[u.res])
            P.dma("pool", o.ap[:, 0:nhc, :], self.ffn_w_out[L, h0:h0 + gs, :].rearrange("(c p) n -> p c n", p=128),
                  writes=[o.res])
            for tb in range(4):
                hb = hh[nhh % 2]
                nhh += 1
                for hc in range(nhc):
                    pg, pu = self.psb[0 + (hc % 2)], self.psb[2 + (hc % 2)]
                    bg, bu = 0 + (hc % 2), 2 + (hc % 2)
                    for k in range(DC):
                        P.op("pe", E.matmul(
                            self.bank(bg), a.ap[:, k, hc * 128:(hc + 1) * 128], hT.ap[:, k, tb * 512:(tb + 1) * 512],
                            start=(k == 0), stop=(k == DC - 1)), reads=[a.res, hT.r(tb)], writes=[pg])
                    for k in range(DC):
                        P.op("pe", E.matmul(
                            self.bank(bu), u.ap[:, k, hc * 128:(hc + 1) * 128], hT.ap[:, k, tb * 512:(tb + 1) * 512],
                            start=(k == 0), stop=(k == DC - 1)), reads=[u.res, hT.r(tb)], writes=[pu])
                    sgt = sg[nsg % 2]
                    nsg += 1
                    P.op("act", E.activation(sgt.ap, self.bank(bg), AF.Silu),
                         reads=[pg], writes=[sgt.res])
                    P.op("dve", E.tensor_tensor(
                        hb.ap[:, hc, :], sgt.ap, self.bank(bu), ALU.mult),
                        reads=[sgt.res, pu], writes=[hb.res])
                for tt in range(4):
                    tile = tb * 4 + tt
                    for half in range(2):
                        bpy = 4 + (npy % 4)
                        npy += 1
                        py = self.psb[bpy]
                        for hc in range(nhc):
                            P.op("pe", E.matmul(
                                self.bank(bpy), hb.ap[:, hc, tt * 128:(tt + 1) * 128], o.ap[:, hc, half * 512:(half + 1) * 512],
                                start=(hc == 0), stop=(hc == nhc - 1)), reads=[hb.res, o.res], writes=[py])
                        dst = yacc.ap[:, tile, half * 512:(half + 1) * 512]
                        if gi == 0:
                            P.op("act", E.copy(dst, self.bank(bpy)),
                                 reads=[py], writes=[yacc.r((tile, half))])
                        else:
                            P.op("dve", E.tensor_tensor(dst, dst, self.bank(bpy), ALU.add),
                                 reads=[py, yacc.r((tile, half))], writes=[yacc.r((tile, half))])
        self.load_ln_consts(L, s, 1, need_z=False)
        for tile in range(NT):
            zt = Tile(yacc.ap[:, tile, :])
            zt.res = yacc.r((tile, 0))
            self.ln_epilogue(s, tile, [(yacc.ap[:, tile, :], 0, D)], [yacc.r((tile, 0)), yacc.r((tile, 1))],
                             x_src, x_dst, dst_res=(self.outres if last else None), z_tile=zt)
        self.phase_reset()

    def rwkv(self, L, s, x_src, x_dst, last=False):
        P = self.P
        j = L // 2
        CW = float(WDECAY)
        bank = self.bank
        psb = self.psb
        cst = self.sb([128, NCONST], F32, "cst")
        P.dma("sp", cst.ap, self.consts, writes=[cst.res])
        maskG = cst.ap[:, 0:512]
        maskNT = cst.ap[:, 512:640]
        triBD = cst.ap[:, 640:768]
        sel63 = cst.ap[:, 768:770]
        Wrkv = [self.sb([128, DC, D], BF16, "Wrkv%d" % p) for p in range(3)]
        for p in range(3):
            P.dma("pool", Wrkv[p].ap, self.rw_wrkv[j, p].rearrange("(k p) n -> p k n", p=128), writes=[Wrkv[p].res])
        w1 = self.sb([128, DC, 64], BF16, "w1")
        a1 = self.sb([128, DC, 64], BF16, "a1")
        g1 = self.sb([128, DC, 160], BF16, "g1")
        P.dma("pool", w1.ap, self.rw_w1[j].rearrange("(k p) n -> p k n", p=128), writes=[w1.res])
        P.dma("pool", a1.ap, self.rw_a1[j].rearrange("(k p) n -> p k n", p=128), writes=[a1.res])
        P.dma("pool", g1.ap, self.rw_g1[j].rearrange("(k p) n -> p k n", p=128), writes=[g1.res])
        w2a = self.sb([65, D], BF16, "w2a")
        a2a = self.sb([65, D], BF16, "a2a")
        g2a = self.sb([128, D], BF16, "g2a")
        g2b = self.sb([32, D], BF16, "g2b")
        P.dma("pool", w2a.ap, self.rw_w2a[j], writes=[w2a.res])
        P.dma("pool", a2a.ap, self.rw_a2a[j], writes=[a2a.res])
        P.dma("pool", g2a.ap, self.rw_g2[j, 0:128, :], writes=[g2a.res])
        P.dma("pool", g2b.ap, self.rw_g2[j, 128:160, :], writes=[g2b.res])
        if j > 0:
            v1 = self.sb([128, DC, 32], BF16, "v1")
            v2a = self.sb([33, D], BF16, "v2a")
            P.dma("pool", v1.ap, self.rw_v1[j - 1].rearrange("(k p) n -> p k n", p=128), writes=[v1.res])
            P.dma("pool", v2a.ap, self.rw_v2a[j - 1], writes=[v2a.res])
        mu = self.sb([128, 6, DC], F32, "mu")
        P.dma("sp", mu.ap, self.rw_mu[j], writes=[mu.res])
        vec = self.sb([128, 5, D], F32, "vec")
        P.dma("sp", vec.ap, self.rw_vec[j:j + 1].broadcast_to([128, 5, D]), writes=[vec.res])
        hT = self.sb([128, DC, T], BF16, "hT")
        ring_off = self.off
        self.xring = [self.sb([128, D], F32, "xr%d" % i) for i in range(4)]
        self.make_hT(x_src, L, s, 0, 1, hT)
        P.barrier()
        self.off = ring_off
        f32t = lambda n: self.sb([128, 512], F32, n)
        bft = lambda n: self.sb([128, 512], BF16, n)
        xx = self.sb([128, DC, 128], BF16, "xx")
        xs = self.sb([128, 6, DC, 128], BF16, "xs")
        r_sb, k_sb, v_sb, a_sb, sgw, kk, kp, u_, rk, eL, enL, eLm, g_sb, sq = [
            f32t(n) for n in "r k v a sgw kk kp u rk eL enL eLm g sq".split()]
        y_sb, yc = eLm, u_
        bonus = rk
        At, Bt, Kt, Rt, Vt = [bft(n) for n in "At Bt Kt Rt Vt".split()]
        FT = self.sb([128, 4, 4, 128], BF16, "FT")
        Gm = self.sb([128, 4, 512], BF16, "Gm")
        Pm = [self.sb([128, 2, 4, 128], BF16, "Pm%d" % i) for i in range(2)]
        X = [self.sb([128, 4, 128], BF16, "X%d" % i) for i in range(2)]
        MT = self.sb([128, 2, 4, 64], BF16, "MT")
        Zs = self.sb([128, 2, 4, 64], F32, "Zs")
        RhF = self.sb([128, 4, 128], BF16, "RhF")
        Y2s = self.sb([128, 512], F32, "Y2s")
        gcol = self.sb([128, 2, 16], F32, "gcol")
        MTbd = self.sb([128, 2, 4, 2, 64], BF16, "MTbd")
        Zbd = self.sb([128, 2, 4, 2, 64], F32, "Zbd")
        mbd = cst.ap[:, 896:1024].rearrange("p (a b) -> p a b", b=64)
        vf, vg = Y2s, sq
        S = self.sb([128, 2, 4, 128], F32, "S")
        Sb = self.sb([128, 2, 4, 128], BF16, "Sb")
        sm = self.sb([128, 64], F32, "sm")
        l1w = self.sb([65, 128], BF16, "l1w")
        l1a = self.sb([65, 128], BF16, "l1a")
        l1v = self.sb([33, 128], BF16, "l1v")
        l1g = self.sb([128, 128], BF16, "l1g")
        l1g2 = self.sb([32, 128], BF16, "l1g2")
        P.op("pool", E.memset(S.ap, 0.0), writes=[S.r(0), S.r(1)])
        P.op("pool", E.memset(Sb.ap, 0.0), writes=[Sb.r(0), Sb.r(1)])
        P.op("pool", E.memset(l1w.ap, 1.0), writes=[l1w.res])
        P.op("pool", E.memset(l1a.ap, 1.0), writes=[l1a.res])
        P.op("pool", E.memset(l1v.ap, 1.0), writes=[l1v.res])
        identb = self.identb
        nb = [0]

        def gbank():
            nb[0] ^= 1
            return nb[0]

        def bc8(ap8):
            return ap8.unsqueeze(2).to_broadcast([128, 8, 64])

        def v3(ap):
            return ap.rearrange("p (a b) -> p a b", b=64)

        for tile in range(min(NT, DBG["tiles"])):
            t0 = tile * 128
            tg = tile // 4
            if tile == 0:
                P.op("dve", E.tensor_tensor(xx.ap[:, :, 1:128], hT.ap[:, :, 0:127], hT.ap[:, :, 1:128], ALU.subtract),
                     reads=[hT.r(0)], writes=[xx.res])
                P.op("dve", E.tensor_scalar(xx.ap[:, :, 0:1], hT.ap[:, :, 0:1], -1.0, None, ALU.mult),
                     reads=[hT.r(0)], writes=[xx.res])
            else:
                P.op("dve", E.tensor_tensor(xx.ap, hT.ap[:, :, t0 - 1:t0 + 127], hT.ap[:, :, t0:t0 + 128], ALU.subtract),
                     reads=[hT.r(tg), hT.r((t0 - 1) // 512)], writes=[xx.res])
            for p in range(6):
                for dc in range(DC):
                    eng = "dve" if (p * DC + dc) % 2 == 0 else "dve"
                    P.op(eng, E.scalar_tensor_tensor(
                        xs.ap[:, p, dc, :], xx.ap[:, dc, :], mu.ap[:, p, dc:dc + 1], hT.ap[:, dc, t0:t0 + 128], ALU.mult, ALU.add),
                        reads=[xx.res, mu.res, hT.r(tg)], writes=[xs.r(p)])
            def lora1(wt, p, n, dst, func, b):
                for dc in range(DC):
                    P.op("pe", E.matmul(bank(b)[0:n, 0:128], wt.ap[:, dc, 0:n], xs.ap[:, p, dc, :],
                                                         start=(dc == 0), stop=(dc == DC - 1)),
                         reads=[wt.res, xs.r(p)], writes=[psb[b]])
                P.op("act", E.activation(dst.ap[0:n, :], bank(b)[0:n, 0:128], func),
                     reads=[psb[b]], writes=[dst.res])
            lora1(w1, 3, 64, l1w, AF.Tanh, gbank())
            lora1(a1, 4, 64, l1a, AF.Copy, gbank())
            if j > 0:
                lora1(v1, 2, 32, l1v, AF.Copy, gbank())
            b = gbank()
            for dc in range(DC):
                P.op("pe", E.matmul(bank(b)[:, 0:128], g1.ap[:, dc, 0:128], xs.ap[:, 5, dc, :],
                                                          start=(dc == 0), stop=(dc == DC - 1)),
                     reads=[g1.res, xs.r(5)], writes=[psb[b]])
            P.op("act", E.activation(l1g.ap, bank(b)[:, 0:128], AF.Sigmoid), reads=[psb[b]], writes=[l1g.res])
            b = gbank()
            for dc in range(DC):
                P.op("pe", E.matmul(bank(b)[0:32, 0:128], g1.ap[:, dc, 128:160], xs.ap[:, 5, dc, :],
                                                          start=(dc == 0), stop=(dc == DC - 1)),
                     reads=[g1.res, xs.r(5)], writes=[psb[b]])
            P.op("act", E.activation(l1g2.ap, bank(b)[0:32, 0:128], AF.Sigmoid), reads=[psb[b]], writes=[l1g2.res])

            if DBG["stage"] < 2:
                continue
            for hf in range(2):
                c0 = hf * 512
                def proj(p, W, dst, eng):
                    b = gbank()
                    for dc in range(DC):
                        P.op("pe", E.matmul(bank(b), xs.ap[:, p, dc, :], W.ap[:, dc, c0:c0 + 512],
                                                                  start=(dc == 0), stop=(dc == DC - 1)),
                             reads=[xs.r(p), W.res], writes=[psb[b]])
                    if eng == "act":
                        P.op("act", E.copy(dst.ap, bank(b)), reads=[psb[b]], writes=[dst.res])
                    else:
                        P.op("dve", E.tensor_copy(dst.ap, bank(b)), reads=[psb[b]], writes=[dst.res])
                proj(0, Wrkv[0], r_sb, "act")
                proj(1, Wrkv[1], k_sb, "dve")
                proj(2, Wrkv[2], v_sb, "act")

                def lora2(l1t, n, w2t, dst, func):
                    b = gbank()
                    P.op("pe", E.matmul(bank(b), l1t.ap[0:n, :], w2t.ap[0:n, c0:c0 + 512], start=True, stop=True),
                         reads=[l1t.res, w2t.res], writes=[psb[b]])
                    P.op("act", E.activation(dst.ap, bank(b), func), reads=[psb[b]], writes=[dst.res])
                lora2(l1w, 65, w2a, sgw, AF.Sigmoid)
                lora2(l1a, 65, a2a, a_sb, AF.Sigmoid)
                b = gbank()
                P.op("pe", E.matmul(bank(b), l1g.ap, g2a.ap[:, c0:c0 + 512], start=True, stop=False),
                     reads=[l1g.res, g2a.res], writes=[psb[b]])
                P.op("pe", E.matmul(bank(b), l1g2.ap, g2b.ap[:, c0:c0 + 512], start=False, stop=True),
                     reads=[l1g2.res, g2b.res], writes=[psb[b]])
                P.op("act", E.copy(g_sb.ap, bank(b)), reads=[psb[b]], writes=[g_sb.res])
                if j == 0:
                    P.dma("sp", self.vfirst[s, t0:t0 + 128, c0:c0 + 512], v_sb.ap, reads=[v_sb.res], writes=[self.vf_res[s][tile]])
                else:
                    lora2(l1v, 33, v2a, vg, AF.Sigmoid)
                    P.dma("sp", vf.ap, self.vfirst[s, t0:t0 + 128, c0:c0 + 512], reads=[self.vf_res[s][tile]], writes=[vf.res])
                    P.op("pool", E.tensor_tensor(vf.ap, vf.ap, v_sb.ap, ALU.subtract), reads=[vf.res, v_sb.res], writes=[vf.res])
                    P.op("pool", E.tensor_tensor(vf.ap, vf.ap, vg.ap, ALU.mult), reads=[vf.res, vg.res], writes=[vf.res])
                    P.op("pool", E.tensor_tensor(v_sb.ap, v_sb.ap, vf.ap, ALU.add), reads=[vf.res, v_sb.res], writes=[v_sb.res])
                bL = gbank()
                P.op("pe", E.matmul(bank(bL), triBD, sgw.ap, start=True, stop=True),
                     reads=[cst.res, sgw.res], writes=[psb[bL]])
                P.op("act", E.activation(eL.ap, bank(bL), AF.Exp, scale=-CW), reads=[psb[bL]], writes=[eL.res])
                P.op("act", E.activation(enL.ap, bank(bL), AF.Exp, scale=CW), reads=[psb[bL]], writes=[enL.res])
                P.op("dve", E.tensor_tensor(eLm.ap, bank(bL), sgw.ap, ALU.subtract), reads=[psb[bL], sgw.res], writes=[eLm.res])
                P.op("act", E.activation(eLm.ap, eLm.ap, AF.Exp, scale=-CW), reads=[eLm.res], writes=[eLm.res])
                P.op("pool", E.tensor_tensor(kk.ap, k_sb.ap, vec.ap[:, 0, c0:c0 + 512], ALU.mult),
                     reads=[k_sb.res, vec.res], writes=[kk.res])
                P.op("pool", E.tensor_tensor(sq.ap, kk.ap, kk.ap, ALU.mult), reads=[kk.res], writes=[sq.res])
                P.op("dve", E.tensor_reduce(sm.ap[:, 0:8], v3(sq.ap), AX.X, ALU.add), reads=[sq.res], writes=[sm.r("ss")])
                P.op("act", E.activation(sm.ap[:, 0:8], sm.ap[:, 0:8], AF.Sqrt), reads=[sm.r("ss")], writes=[sm.r("ss")])
                P.op("dve", E.tensor_scalar_max(sm.ap[:, 0:8], sm.ap[:, 0:8], 1e-12), reads=[sm.r("ss")], writes=[sm.r("ss")])
                P.op("dve", E.reciprocal(sm.ap[:, 0:8], sm.ap[:, 0:8]), reads=[sm.r("ss")], writes=[sm.r("ss")])
                P.op("dve", E.tensor_tensor(v3(kk.ap), v3(kk.ap), bc8(sm.ap[:, 0:8]), ALU.mult),
                     reads=[kk.res, sm.r("ss")], writes=[kk.res])
                P.op("dve", E.scalar_tensor_tensor(u_.ap, a_sb.ap, -1.0, vec.ap[:, 1, c0:c0 + 512], ALU.add, ALU.mult),
                     reads=[a_sb.res, vec.res], writes=[u_.res])
                P.op("dve", E.scalar_tensor_tensor(kp.ap, u_.ap, 1.0, k_sb.ap, ALU.add, ALU.mult),
                     reads=[u_.res, k_sb.res], writes=[kp.res])
                P.op("dve", E.scalar_tensor_tensor(At.ap, kk.ap, -1.0, eLm.ap, ALU.mult, ALU.mult),
                     reads=[kk.res, eLm.res], writes=[At.res])
                P.op("pool", E.tensor_tensor(u_.ap, kk.ap, a_sb.ap, ALU.mult), reads=[kk.res, a_sb.res, u_.res], writes=[u_.res])
                P.op("pool", E.tensor_tensor(Bt.ap, u_.ap, enL.ap, ALU.mult), reads=[u_.res, enL.res], writes=[Bt.res])
                P.op("pool", E.tensor_tensor(Kt.ap, kp.ap, enL.ap, ALU.mult), reads=[kp.res, enL.res], writes=[Kt.res])
                P.op("dve", E.tensor_tensor(Rt.ap, r_sb.ap, eL.ap, ALU.mult), reads=[r_sb.res, eL.res], writes=[Rt.res])
                P.op("act", E.copy(Vt.ap, v_sb.ap), reads=[v_sb.res], writes=[Vt.res])
                P.op("pool", E.tensor_tensor(rk.ap, r_sb.ap, kp.ap, ALU.mult), reads=[r_sb.res, kp.res], writes=[rk.res])
                P.op("pool", E.tensor_tensor(rk.ap, rk.ap, vec.ap[:, 2, c0:c0 + 512], ALU.mult), reads=[rk.res, vec.res], writes=[rk.res])
                P.op("dve", E.tensor_reduce(sm.ap[:, 16:24], v3(rk.ap), AX.X, ALU.add), reads=[rk.res], writes=[sm.r("rk")])
                P.op("dve", E.tensor_tensor(v3(bonus.ap), v3(v_sb.ap), bc8(sm.ap[:, 16:24]), ALU.mult),
                     reads=[v_sb.res, sm.r("rk")], writes=[rk.res])
                if DBG["stage"] < 3:
                    continue
                bg = gbank()
                for q in range(4):
                    P.op("pe", E.matmul(bank(bg)[:, q * 2:q * 2 + 2], eL.ap[:, q * 128:(q + 1) * 128], sel63,
                                                              start=True, stop=True),
                         reads=[eL.res, cst.res], writes=[psb[bg]])
                P.op("dve", E.tensor_copy(gcol.ap[:, :, 0:4], bank(bg)[:, 0:8].rearrange("p (q c) -> p c q", c=2)),
                     reads=[psb[bg]], writes=[gcol.res])
                ops4 = [At, Bt, Kt, Rt]
                for oi in range(4):
                    for q in range(4):
                        bt = 6 + (oi // 2)
                        col = ((oi % 2) * 4 + q) * 128
                        P.op("pe", E.transpose(
                            bank(bt, BF16)[:, col:col + 128], ops4[oi].ap[:, q * 128:(q + 1) * 128], identb.ap),
                            reads=[ops4[oi].res, identb.res], writes=[psb[bt]])
                P.op("act", E.copy(FT.ap[:, 0:2, :, :], bank(6, BF16).rearrange("p (a b c) -> p a b c", a=2, b=4)),
                     reads=[psb[6]], writes=[FT.r(0)])
                P.op("dve", E.tensor_copy(FT.ap[:, 2:4, :, :], bank(7, BF16).rearrange("p (a b c) -> p a b c", a=2, b=4)),
                     reads=[psb[7]], writes=[FT.r(1)])
                FTr = [FT.r(0), FT.r(1)]
                for grp in range(2 if DBG["stage"] >= 4 else 0):
                    for hl in range(4):
                        q = grp * 2 + hl // 2
                        m = hl % 2
                        rows = slice(m * 64, m * 64 + 64)
                        bgm = 2 + m
                        hcol = q * 128 + m * 64
                        for gi, (lo, ro) in enumerate([(1, 0), (1, 3), (2, 0), (2, 3)]):
                            P.op("pe", E.matmul(
                                bank(bgm)[:, gi * 128:(gi + 1) * 128], FT.ap[rows, lo, q, :], FT.ap[rows, ro, q, :],
                                start=True, stop=True, tile_position=(m * 64, 0)),
                                reads=FTr, writes=[psb[bgm]])
                        P.op("pe", E.matmul(
                            bank(4 + m)[:, (hl // 2) * 128:(hl // 2 + 1) * 128], FT.ap[rows, 0, q, :], FT.ap[rows, 1, q, :],
                            start=True, stop=True, tile_position=(m * 64, 0)),
                            reads=FTr, writes=[psb[4 + m]])
                        P.op("dve", E.tensor_tensor(Gm.ap[:, hl, :], bank(bgm), maskG, ALU.mult),
                             reads=[psb[bgm], cst.res], writes=[Gm.r(hl)])
                    for m in range(2):
                        P.op("dve", E.tensor_tensor(
                            Pm[0].ap[:, 1, m::2, :], bank(4 + m)[:, 0:256].rearrange("p (a b) -> p a b", b=128),
                            maskNT.unsqueeze(1).to_broadcast([128, 2, 128]), ALU.mult),
                            reads=[psb[4 + m], cst.res], writes=[Pm[0].r(("t", m))])
                    P.op("pool", E.tensor_copy(Pm[0].ap[:, 0, :, :], Gm.ap[:, :, 0:128]),
                         reads=[Gm.r(h_) for h_ in range(4)], writes=[Pm[0].r("p")])
                    if DBG["stage"] < 5:
                        continue
                    bw = gbank()
                    for hl in range(4):
                        hcol = (grp * 2 + hl // 2) * 128 + (hl % 2) * 64
                        P.op("pe", E.matmul(
                            bank(bw)[:, hl * 64:(hl + 1) * 64], Gm.ap[:, hl, 256:384], Vt.ap[:, hcol:hcol + 64], start=True, stop=True),
                            reads=[Gm.r(hl), Vt.res], writes=[psb[bw]])
                    P.op("act", E.copy(X[0].ap[:, :, 64:128], bank(bw)[:, 0:256].rearrange("p (a b) -> p a b", b=64)),
                         reads=[psb[bw]], writes=[X[0].r("u")])
                    P.op("pool", E.tensor_copy(
                        X[0].ap[:, :, 0:64], At.ap[:, grp * 256:(grp + 1) * 256].rearrange("p (a b) -> p a b", b=64)),
                        reads=[At.res], writes=[X[0].r("a")])
                    Xr = lambda i: [X[i].r("u"), X[i].r("a")]
                    Pr = lambda i: [Pm[i].r("p"), Pm[i].r(("t", 0)), Pm[i].r(("t", 1))]
                    for lvl in range(6):
                        ci, ni = lvl % 2, (lvl + 1) % 2
                        for hl in range(4):
                            P.op("pe", E.matmul(
                                bank(2)[:, hl * 128:(hl + 1) * 128], Pm[ci].ap[:, 0, hl, :], X[ci].ap[:, hl, :], start=True, stop=True),
                                reads=Pr(ci) + Xr(ci), writes=[psb[2]])
                        P.op("dve", E.tensor_tensor(
                            X[ni].ap, bank(2).rearrange("p (a b) -> p a b", b=128), X[ci].ap, ALU.add),
                            reads=[psb[2]] + Xr(ci), writes=Xr(ni))
                        if lvl < 5:
                            for hl in range(4):
                                P.op("pe", E.matmul(
                                    bank(4)[:, hl * 128:(hl + 1) * 128], Pm[ci].ap[:, 1, hl, :], Pm[ci].ap[:, 0, hl, :], start=True, stop=True),
                                    reads=Pr(ci), writes=[psb[4]])
                                P.op("pe", E.matmul(
                                    bank(5)[:, hl * 128:(hl + 1) * 128], Pm[ci].ap[:, 0, hl, :], Pm[ci].ap[:, 1, hl, :], start=True, stop=True),
                                    reads=Pr(ci), writes=[psb[5]])
                            P.op("act", E.copy(Pm[ni].ap[:, 0, :, :], bank(4).rearrange("p (a b) -> p a b", b=128)),
                                 reads=[psb[4]], writes=[Pm[ni].r("p")])
                            P.op("act", E.copy(Pm[ni].ap[:, 1, :, :], bank(5).rearrange("p (a b) -> p a b", b=128)),
                                 reads=[psb[5]], writes=[Pm[ni].r(("t", 0)), Pm[ni].r(("t", 1))])
                    if DBG["stage"] < 6:
                        continue
                    XF = X[0]
                    XFr = Xr(0)
                    for hl in range(4):
                        q = grp * 2 + hl // 2
                        m = hl % 2
                        hcol = q * 128 + m * 64
                        for c in range(2):
                            rc = slice(c * 64, c * 64 + 64)
                            bmz = 6 + c
                            P.op("pe", E.matmul(
                                bank(bmz)[m * 64:m * 64 + 64, q * 64:q * 64 + 64], XF.ap[rc, hl, 0:64], Bt.ap[rc, hcol:hcol + 64],
                                start=True, stop=True, tile_position=(c * 64, m * 64)),
                                reads=XFr + [Bt.res], writes=[psb[bmz]])
                            P.op("pe", E.matmul(
                                bank(bmz)[m * 64:m * 64 + 64, 256 + q * 64:256 + q * 64 + 64], Bt.ap[rc, hcol:hcol + 64], XF.ap[rc, hl, 64:128],
                                start=True, stop=False, tile_position=(c * 64, m * 64)),
                                reads=XFr + [Bt.res], writes=[psb[bmz]])
                            P.op("pe", E.matmul(
                                bank(bmz)[m * 64:m * 64 + 64, 256 + q * 64:256 + q * 64 + 64], Kt.ap[rc, hcol:hcol + 64], Vt.ap[rc, hcol:hcol + 64],
                                start=False, stop=True, tile_position=(c * 64, m * 64)),
                                reads=[Kt.res, Vt.res], writes=[psb[bmz]])
                        if DBG["sub"] >= 2:
                          P.op("pe", E.matmul(
                            bank(3)[m * 64:m * 64 + 64, (hl // 2) * 128:(hl // 2) * 128 + 128], XF.ap[:, hl, 0:64], Gm.ap[:, hl, 128:256],
                            start=True, stop=True, tile_position=(0, m * 64)),
                            reads=XFr + [Gm.r(hl)], writes=[psb[3]])
                    qs = slice(grp * 2, grp * 2 + 2)
                    for c in range(2):
                        bmz = 6 + c
                        P.op("dve", E.tensor_tensor(
                            MT.ap[:, c, qs, :], bank(bmz)[:, grp * 128:grp * 128 + 128].rearrange("p (a b) -> p a b", b=64),
                            cst.ap[:, 832:896].unsqueeze(1).to_broadcast([128, 2, 64]), ALU.add),
                            reads=[psb[bmz], cst.res], writes=[MT.r((c, grp))])
                        P.op("act", E.copy(
                            Zs.ap[:, c, qs, :], bank(bmz)[:, 256 + grp * 128:256 + grp * 128 + 128].rearrange("p (a b) -> p a b", b=64)),
                            reads=[psb[bmz]], writes=[Zs.r((c, grp))])
                        for ql in range(2):
                            q = grp * 2 + ql
                            P.op("pool", E.tensor_tensor(
                                MTbd.ap[:, c, q, :, :], MT.ap[:, c, q, :].unsqueeze(1).to_broadcast([128, 2, 64]), mbd, ALU.mult),
                                reads=[MT.r((c, grp)), cst.res], writes=[MTbd.r((c, q))])
                            P.op("pool", E.tensor_tensor(
                                Zbd.ap[:, c, q, :, :], Zs.ap[:, c, q, :].unsqueeze(1).to_broadcast([128, 2, 64]), mbd, ALU.mult),
                                reads=[Zs.r((c, grp)), cst.res], writes=[Zbd.r((c, q))])
                    P.op("dve", E.tensor_tensor(
                        RhF.ap[:, qs, :], bank(3)[:, 0:256].rearrange("p (a b) -> p a b", b=128), FT.ap[:, 3, qs, :], ALU.add),
                        reads=[psb[3]] + FTr, writes=[RhF.r(grp)])
                    by2 = gbank()
                    for hl in range(4):
                        hcol = (grp * 2 + hl // 2) * 128 + (hl % 2) * 64
                        P.op("pe", E.matmul(
                            bank(by2)[:, hl * 64:(hl + 1) * 64], Gm.ap[:, hl, 128:256], XF.ap[:, hl, 64:128], start=True, stop=False),
                            reads=[Gm.r(hl)] + XFr, writes=[psb[by2]])
                        P.op("pe", E.matmul(
                            bank(by2)[:, hl * 64:(hl + 1) * 64], Gm.ap[:, hl, 384:512], Vt.ap[:, hcol:hcol + 64], start=False, stop=True),
                            reads=[Gm.r(hl), Vt.res], writes=[psb[by2]])
                    P.op("act", E.copy(Y2s.ap[:, grp * 256:(grp + 1) * 256], bank(by2)[:, 0:256]),
                         reads=[psb[by2]], writes=[Y2s.res])
                if DBG["stage"] < 7:
                    continue
                allq = [MTbd.r((c_, q_)) for c_ in range(2) for q_ in range(4)]
                for c in range(2):
                    for q in range(4):
                        P.op("pe", E.matmul(
                            bank(2)[c * 64:c * 64 + 64, q * 128:(q + 1) * 128], RhF.ap[:, q, c * 64:c * 64 + 64], Sb.ap[:, hf, q, :],
                            start=True, stop=True, tile_position=(0, c * 64)),
                            reads=[RhF.r(0), RhF.r(1), Sb.r(hf)], writes=[psb[2]])
                    for q in range(4):
                        P.op("pe", E.matmul(
                            bank(3)[:, q * 128:(q + 1) * 128], MTbd.ap[:, c, q, :, :].rearrange("p a b -> p (a b)"), Sb.ap[:, hf, q, :],
                            start=True, stop=True),
                            reads=[MTbd.r((c, q)), Sb.r(hf)], writes=[psb[3]])
                    P.op("dve", E.tensor_tensor(
                        S.ap[:, hf, :, :], bank(3).rearrange("p (a b) -> p a b", b=128),
                        Zbd.ap[:, c, :, :, :].rearrange("p q a b -> p q (a b)"), ALU.add),
                        reads=[psb[3]] + [Zbd.r((c, q_)) for q_ in range(4)], writes=[S.r(hf)])
                    P.op("dve", E.tensor_tensor(
                        S.ap[:, hf, :, :], S.ap[:, hf, :, :], gcol.ap[:, c, 0:4].unsqueeze(2).to_broadcast([128, 4, 128]), ALU.mult),
                        reads=[S.r(hf), gcol.res], writes=[S.r(hf)])
                    P.op("act", E.copy(Sb.ap[:, hf, :, :], S.ap[:, hf, :, :]), reads=[S.r(hf)], writes=[Sb.r(hf)])
                P.op("dve", E.tensor_tensor(y_sb.ap, bank(2), Y2s.ap, ALU.add),
                     reads=[psb[2], Y2s.res], writes=[y_sb.res])
                if DBG["stage"] < 8:
                    continue
                yres = [y_sb.res]
                P.op("dve", E.tensor_reduce(sm.ap[:, 32:40], v3(y_sb.ap), AX.X, ALU.add), reads=yres, writes=[sm.r("m")])
                P.op("dve", E.tensor_scalar_mul(sm.ap[:, 32:40], sm.ap[:, 32:40], 1.0 / 64), reads=[sm.r("m")], writes=[sm.r("m")])
                P.op("dve", E.tensor_tensor(v3(yc.ap), v3(y_sb.ap), bc8(sm.ap[:, 32:40]), ALU.subtract),
                     reads=yres + [sm.r("m")], writes=[yc.res])
                P.op("pool", E.tensor_tensor(sq.ap, yc.ap, yc.ap, ALU.mult), reads=[yc.res], writes=[sq.res])
                P.op("dve", E.tensor_reduce(sm.ap[:, 48:56], v3(sq.ap), AX.X, ALU.add), reads=[sq.res], writes=[sm.r("v")])
                P.op("act", E.activation(sm.ap[:, 48:56], sm.ap[:, 48:56], AF.Sqrt, bias=GN_EPS, scale=1.0 / 64),
                     reads=[sm.r("v")], writes=[sm.r("v")])
                P.op("dve", E.reciprocal(sm.ap[:, 48:56], sm.ap[:, 48:56]), reads=[sm.r("v")], writes=[sm.r("v")])
                P.op("dve", E.tensor_tensor(v3(yc.ap), v3(yc.ap), bc8(sm.ap[:, 48:56]), ALU.mult),
                     reads=[yc.res, sm.r("v")], writes=[yc.res])
                P.op("pool", E.tensor_tensor(yc.ap, yc.ap, vec.ap[:, 3, c0:c0 + 512], ALU.mult), reads=[yc.res, vec.res], writes=[yc.res])
                P.op("pool", E.tensor_tensor(yc.ap, yc.ap, vec.ap[:, 4, c0:c0 + 512], ALU.add), reads=[yc.res, vec.res], writes=[yc.res])
                P.op("pool", E.tensor_tensor(yc.ap, yc.ap, bonus.ap, ALU.add), reads=[yc.res, rk.res], writes=[yc.res])
                P.op("pool", E.tensor_tensor(At.ap, yc.ap, g_sb.ap, ALU.mult), reads=[yc.res, g_sb.res, At.res], writes=[At.res])
                P.dma("pool", self.ogdram[s, t0:t0 + 128, c0:c0 + 512], At.ap, reads=[At.res], writes=[self.og_res[s][tile]])
        self.phase_reset()
        if DBG["stage"] < 9:
            return
        Wo = self.sb([128, DC, D], BF16, "Wo")
        P.dma("pool", Wo.ap, self.rw_wout[j].rearrange("(k p) n -> p k n", p=128), writes=[Wo.res])
        self.out_proj_ln(L, s, Wo, self.ogdram[s], self.og_res[s], x_src, x_dst, last)
        self.phase_reset()

    def out_proj_ln(self, L, s, Wo, og, og_res, x_src, x_dst, last):
        P = self.P
        bank = self.bank
        psb = self.psb
        self.xring = [self.sb([128, D], F32, "xr%d" % i) for i in range(4)]
        self.load_ln_consts(L, s, 0, need_z=True)
        ogt = [self.sb([128, D], BF16, "ogt%d" % i) for i in range(2)]
        ogT = [self.sb([128, DC, 128], BF16, "ogT%d" % i) for i in range(2)]
        for tile in range(NT):
            t0 = tile * 128
            o, oT = ogt[tile % 2], ogT[tile % 2]
            P.dma("sp", o.ap, og[t0:t0 + 128, :], reads=[og_res[tile]], writes=[o.res])
            bt = 4 + tile % 2
            for dc in range(DC):
                P.op("pe", E.transpose(bank(bt, BF16)[:, dc * 128:(dc + 1) * 128], o.ap[:, dc * 128:(dc + 1) * 128], self.identb.ap),
                     reads=[o.res, self.identb.res], writes=[psb[bt]])
            P.op("act", E.copy(oT.ap, bank(bt, BF16).rearrange("p (a b) -> p a b", b=128)),
                 reads=[psb[bt]], writes=[oT.res])
            b0 = (tile % 2) * 2
            for half in range(2):
                for dc in range(DC):
                    P.op("pe", E.matmul(
                        bank(b0 + half), oT.ap[:, dc, :], Wo.ap[:, dc, half * 512:(half + 1) * 512], start=(dc == 0), stop=(dc == DC - 1)),
                        reads=[oT.res, Wo.res], writes=[psb[b0 + half]])
            self.ln_epilogue(s, tile, [(bank(b0), 0, 512), (bank(b0 + 1), 512, 512)], [psb[b0], psb[b0 + 1]],
                             x_src, x_dst, dst_res=(self.outres if last else None))

    def dsa(self, L, s, x_src, x_dst, last=False):
        P = self.P
        j = L // 2
        bank = self.bank
        psb = self.psb
        identb = self.identb
        SCL = 0.125
        WSC = 1.0 / math.sqrt(8 * 64)
        win = self.dsa_w_in[j]
        ldw = lambda t, ap: P.dma("pool", t.ap, ap, writes=[t.res])
        Wq = self.sb([128, DC, 1024], BF16, "Wq")
        Wc = self.sb([128, DC, 128], BF16, "Wc")
        Wqi = self.sb([128, DC, 512], BF16, "Wqi")
        Wki = self.sb([128, DC, 64], BF16, "Wki")
        Wwi = self.sb([128, DC, 8], BF16, "Wwi")
        for t_, c0, c1 in [(Wq, 0, 1024), (Wc, 1024, 1152), (Wqi, 1152, 1664), (Wki, 1664, 1728), (Wwi, 1728, 1736)]:
            ldw(t_, win[:, c0:c1].rearrange("(k p) n -> p k n", p=128))
        wuk = self.sb([64, 16, 128], BF16, "wuk")
        ldw(wuk, self.dsa_w_uk[j].rearrange("h d c -> d h c"))
        wuv = self.sb([128, 16, 64], BF16, "wuv")
        ldw(wuv, self.dsa_w_uv[j].rearrange("h c d -> c h d"))
        gkv = self.sb([128, 128], F32, "gkv")
        P.dma("sp", gkv.ap, self.dsa_kv_norm[j:j + 1, :].broadcast_to([128, 128]), writes=[gkv.res])
        caus = self.sb([128, 4, 512], F32, "caus")
        P.dma("sp", caus.ap, self.dsa_caus, writes=[caus.res])
        braw = self.sb([128, 16, 256], F32, "braw")
        c31 = self.sb([128, 16], F32, "c31")
        band = self.sb([128, 16, 256], BF16, "band")
        P.dma("sp", braw.ap, self.dsa_band, writes=[braw.res])
        P.dma("sp", c31.ap, self.dsa_c31, writes=[c31.res])
        P.op("dve", E.tensor_tensor(band.ap, braw.ap, c31.ap.unsqueeze(2).to_broadcast([128, 16, 256]), ALU.subtract),
             reads=[braw.res, c31.res], writes=[band.res])
        ones = self.sb([128, 128], BF16, "ones")
        P.op("pool", E.memset(ones.ap, 1.0), writes=[ones.res])
        half = self.sb([128, 1], F32, "half")
        P.op("pool", E.memset(half.ap, 0.5), writes=[half.res])
        ckvT = self.sb([128, NT, 128], BF16, "ckvT")
        ckvF = self.sb([128, T], BF16, "ckvF")
        kiT = self.sb([64, T], BF16, "kiT")
        hT = self.sb([128, DC, T], BF16, "hT")
        ring_off = self.off
        self.xring = [self.sb([128, D], F32, "xr%d" % i) for i in range(4)]
        self.make_hT(x_src, L, s, 0, 1, hT)
        P.barrier()
        self.off = ring_off
        nb = [0]

        def gbank():
            nb[0] ^= 1
            return nb[0]
        ck = self.sb([128, 128], F32, "ck")
        cs = self.sb([128, 8], F32, "cs")
        for tile in range(NT):
            b = gbank()
            for dc in range(DC):
                P.op("pe", E.matmul(bank(b)[:, 0:128], hT.ap[:, dc, tile * 128:(tile + 1) * 128], Wc.ap[:, dc, :],
                                    start=(dc == 0), stop=(dc == DC - 1)), reads=[hT.r(tile // 4), Wc.res], writes=[psb[b]])
            P.op("act", E.activation(ck.ap, bank(b)[:, 0:128], AF.Square, accum_out=cs.ap[:, 0:1]), reads=[psb[b]], writes=[ck.res, cs.res])
            P.op("act", E.activation(cs.ap[:, 0:1], cs.ap[:, 0:1], AF.Sqrt, bias=1e-6, scale=1.0 / 128), reads=[cs.res], writes=[cs.res])
            P.op("dve", E.reciprocal(cs.ap[:, 0:1], cs.ap[:, 0:1]), reads=[cs.res], writes=[cs.res])
            P.op("dve", E.scalar_tensor_tensor(ckvT.ap[:, tile, :], bank(b)[:, 0:128], cs.ap[:, 0:1], gkv.ap, ALU.mult, ALU.mult),
                 reads=[psb[b], cs.res, gkv.res], writes=[ckvT.r(tile)])
            b2 = gbank()
            P.op("pe", E.transpose(bank(b2, BF16)[:, 0:128], ckvT.ap[:, tile, :], identb.ap), reads=[ckvT.r(tile), identb.res], writes=[psb[b2]])
            P.op("act", E.copy(ckvF.ap[:, tile * 128:(tile + 1) * 128], bank(b2, BF16)[:, 0:128]), reads=[psb[b2]], writes=[ckvF.r(tile)])
        for tb in range(4):
            b = gbank()
            for dc in range(DC):
                P.op("pe", E.matmul(bank(b)[0:64, :], Wki.ap[:, dc, :], hT.ap[:, dc, tb * 512:(tb + 1) * 512],
                                    start=(dc == 0), stop=(dc == DC - 1)), reads=[hT.r(tb), Wki.res], writes=[psb[b]])
            P.op("act", E.copy(kiT.ap[:, tb * 512:(tb + 1) * 512], bank(b)[0:64, :]), reads=[psb[b]], writes=[kiT.r(tb)])
        qTh = self.sb([64, 16, 512], BF16, "qTh")
        qlat = self.sb([128, 16, 512], BF16, "qlat")
        qiT = self.sb([64, 8, 512], BF16, "qiT")
        widx = self.sb([128, 4, 8], F32, "widx")
        acc = self.sb([128, T], F32, "acc")
        junk = self.sb([128, T], F32, "junk")
        relu = [self.sb([128, 512], F32, "relu%d" % i) for i in range(2)]
        mb = self.sb([128, T], BF16, "mb")
        mT = self.sb([128, NT, 128], BF16, "mT")
        eT = [self.sb([128, 4, 128], BF16, "eT%d" % i) for i in range(2)]
        olTn = self.sb([128, 4, 128], BF16, "olTn")
        rinv = self.sb([128, 512], F32, "rinv")
        ogt = self.sb([128, D], BF16, "ogt")
        bs = self.sb([128, 16], F32, "bs")
        ntl = min(NT, DBG["tiles"])
        for tg in range(4):
            if tg * 4 >= ntl:
                break
            tsl = slice(tg * 512, (tg + 1) * 512)
            for h in range(16):
                b = gbank()
                for dc in range(DC):
                    P.op("pe", E.matmul(bank(b)[0:64, :], Wq.ap[:, dc, h * 64:(h + 1) * 64], hT.ap[:, dc, tsl],
                                        start=(dc == 0), stop=(dc == DC - 1)), reads=[hT.r(tg), Wq.res], writes=[psb[b]])
                P.op(self.ev_eng(), E.copy(qTh.ap[:, h, :], bank(b)[0:64, :]) if self.rr["ev"] == 0 else E.tensor_copy(qTh.ap[:, h, :], bank(b)[0:64, :]),
                     reads=[psb[b]], writes=[qTh.r(h)])
            for h in range(16):
                b = gbank()
                P.op("pe", E.matmul(bank(b), wuk.ap[:, h, :], qTh.ap[:, h, :], start=True, stop=True, tile_position=(0, 0)),
                     reads=[wuk.res, qTh.r(h)], writes=[psb[b]])
                P.op("act", E.activation(qlat.ap[:, h, :], bank(b), AF.Identity, scale=SCL), reads=[psb[b]], writes=[qlat.r(h)])
            for hi in range(8):
                b = gbank()
                for dc in range(DC):
                    P.op("pe", E.matmul(bank(b)[0:64, :], Wqi.ap[:, dc, hi * 64:(hi + 1) * 64], hT.ap[:, dc, tsl],
                                        start=(dc == 0), stop=(dc == DC - 1)), reads=[hT.r(tg), Wqi.res], writes=[psb[b]])
                P.op("dve", E.tensor_copy(qiT.ap[:, hi, :], bank(b)[0:64, :]), reads=[psb[b]], writes=[qiT.r(hi)])
            for tt in range(4):
                b = gbank()
                tile = tg * 4 + tt
                for dc in range(DC):
                    P.op("pe", E.matmul(bank(b)[:, 0:8], hT.ap[:, dc, tile * 128:(tile + 1) * 128], Wwi.ap[:, dc, :],
                                        start=(dc == 0), stop=(dc == DC - 1)), reads=[hT.r(tg), Wwi.res], writes=[psb[b]])
                P.op("act", E.activation(widx.ap[:, tt, :], bank(b)[:, 0:8], AF.Identity, scale=WSC), reads=[psb[b]], writes=[widx.r(tt)])
            qlr = [qlat.r(h) for h in range(16)]
            qir = [qiT.r(h) for h in range(8)]
            for tt in range(4):
                tile = tg * 4 + tt
                if tile >= ntl:
                    break
                nsb = tile // 4 + 1
                ncol = nsb * 512
                tcs = slice(tt * 128, (tt + 1) * 128)
                nr = 0
                for sbk in range(nsb):
                    for hi in range(8):
                        b = gbank()
                        P.op("pe", E.matmul(bank(b), qiT.ap[:, hi, tcs], kiT.ap[:, sbk * 512:(sbk + 1) * 512], start=True, stop=True,
                                            tile_position=(0, 0)), reads=[qiT.r(hi), kiT.r(sbk)], writes=[psb[b]])
                        rl = relu[nr % 2]
                        nr += 1
                        P.op("act", E.activation(rl.ap, bank(b), AF.Relu), reads=[psb[b]], writes=[rl.res])
                        dst = acc.ap[:, sbk * 512:(sbk + 1) * 512]
                        if hi == 0:
                            P.op("dve", E.tensor_scalar(dst, rl.ap, widx.ap[:, tt, 0:1], None, ALU.mult), reads=[rl.res, widx.r(tt)], writes=[acc.r(sbk)])
                        else:
                            P.op("dve", E.scalar_tensor_tensor(dst, rl.ap, widx.ap[:, tt, hi:hi + 1], dst, ALU.mult, ALU.add),
                                 reads=[rl.res, widx.r(tt), acc.r(sbk)], writes=[acc.r(sbk)])
                accr = [acc.r(k_) for k_ in range(nsb)]
                if tile >= 2:
                    P.op("dve", E.tensor_reduce(bs.ap[:, 0:1], acc.ap[:, 0:ncol], AX.X, ALU.min), reads=accr, writes=[bs.res])
                    P.op("dve", E.tensor_reduce(bs.ap[:, 1:2], acc.ap[:, 0:ncol], AX.X, ALU.max), reads=accr, writes=[bs.res])
                    P.op("dve", E.tensor_scalar_add(bs.ap[:, 0:1], bs.ap[:, 0:1], -1e-3), reads=[bs.res], writes=[bs.res])
                    P.op("dve", E.tensor_scalar_add(bs.ap[:, 1:2], bs.ap[:, 1:2], 1e-3), reads=[bs.res], writes=[bs.res])
                lastb = acc.ap[:, (nsb - 1) * 512:nsb * 512]
                P.op("dve", E.tensor_tensor(lastb, lastb, caus.ap[:, tile % 4, :], ALU.add), reads=accr + [caus.res, bs.res], writes=[acc.r(nsb - 1)])
                if tile >= 2:
                    for it in range(16):
                        P.op("dve", E.scalar_tensor_tensor(bs.ap[:, 2:3], bs.ap[:, 0:1], bs.ap[:, 1:2], half.ap, ALU.add, ALU.mult),
                             reads=[bs.res, half.res], writes=[bs.res])
                        P.op("dve", E.tensor_scalar(junk.ap[:, 0:ncol], acc.ap[:, 0:ncol], bs.ap[:, 2:3], 0.0, ALU.is_ge, ALU.add, accum_out=bs.ap[:, 3:4]),
                             reads=accr + [bs.res], writes=[junk.res, bs.res])
                        P.op("dve", E.tensor_scalar(bs.ap[:, 4:5], bs.ap[:, 3:4], 256.0, None, ALU.is_ge), reads=[bs.res], writes=[bs.res])
                        P.op("dve", E.tensor_tensor(bs.ap[:, 5:6], bs.ap[:, 2:3], bs.ap[:, 0:1], ALU.subtract), reads=[bs.res], writes=[bs.res])
                        P.op("dve", E.scalar_tensor_tensor(bs.ap[:, 0:1], bs.ap[:, 5:6], bs.ap[:, 4:5], bs.ap[:, 0:1], ALU.mult, ALU.add),
                             reads=[bs.res], writes=[bs.res])
                        P.op("dve", E.tensor_tensor(bs.ap[:, 5:6], bs.ap[:, 1:2], bs.ap[:, 2:3], ALU.subtract), reads=[bs.res], writes=[bs.res])
                        P.op("dve", E.scalar_tensor_tensor(bs.ap[:, 1:2], bs.ap[:, 5:6], bs.ap[:, 4:5], bs.ap[:, 2:3], ALU.mult, ALU.add),
                             reads=[bs.res], writes=[bs.res])
                else:
                    P.op("dve", E.memset(bs.ap[:, 0:1], -1e29), reads=[bs.res], writes=[bs.res])
                P.op("dve", E.tensor_scalar(mb.ap[:, 0:ncol], acc.ap[:, 0:ncol], bs.ap[:, 0:1], -30000.0, ALU.is_lt, ALU.mult),
                     reads=accr + [bs.res], writes=[mb.res])
                for sg in range(tile + 1):
                    b = gbank()
                    P.op("pe", E.transpose(bank(b, BF16)[:, 0:128], mb.ap[:, sg * 128:(sg + 1) * 128], identb.ap), reads=[mb.res, identb.res], writes=[psb[b]])
                    P.op("act", E.copy(mT.ap[:, sg, :], bank(b, BF16)[:, 0:128]), reads=[psb[b]], writes=[mT.r(sg)])
                ne = 0
                for hg in range(4):
                    bo = 4 + hg % 2
                    br = 6 + hg % 2
                    for sg in range(tile + 1):
                        bl = 2 + ne % 2
                        e_ = eT[ne % 2]
                        ne += 1
                        for hh in range(4):
                            h = hg * 4 + hh
                            reg = bank(bl)[:, hh * 128:(hh + 1) * 128]
                            wband = (sg == tile) or (sg == tile - 1)
                            P.op("pe", E.matmul(reg, ckvF.ap[:, sg * 128:(sg + 1) * 128], qlat.ap[:, h, tcs], start=True, stop=False),
                                 reads=[ckvF.r(sg), qlat.r(h)], writes=[psb[bl]])
                            P.op("pe", E.matmul(reg, identb.ap, mT.ap[:, sg, :], start=False, stop=(not wband)),
                                 reads=[identb.res, mT.r(sg)], writes=[psb[bl]])
                            if wband:
                                w_ = 0 if sg == tile else 1
                                P.op("pe", E.matmul(reg, identb.ap, band.ap[:, h, w_ * 128:(w_ + 1) * 128], start=False, stop=True),
                                     reads=[identb.res, band.res], writes=[psb[bl]])
                        P.op("act", E.activation(e_.ap, bank(bl).rearrange("p (a b) -> p a b", b=128), AF.Exp), reads=[psb[bl]], writes=[e_.res])
                        e2 = e_.ap.rearrange("p a b -> p (a b)")
                        P.op("pe", E.matmul(bank(bo), ckvT.ap[:, sg, :], e2, start=(sg == 0), stop=(sg == tile)),
                             reads=[ckvT.r(sg), e_.res], writes=[psb[bo]])
                        P.op("pe", E.matmul(bank(br), ones.ap, e2, start=(sg == 0), stop=(sg == tile)),
                             reads=[ones.res, e_.res], writes=[psb[br]])
                    P.op("dve", E.reciprocal(rinv.ap, bank(br)), reads=[psb[br]], writes=[rinv.res])
                    P.op("dve", E.tensor_tensor(olTn.ap.rearrange("p a b -> p (a b)"), bank(bo), rinv.ap, ALU.mult),
                         reads=[psb[bo], rinv.res], writes=[olTn.res])
                    for hh in range(4):
                        h = hg * 4 + hh
                        bq = gbank()
                        P.op("pe", E.matmul(bank(bq)[:, 0:64], olTn.ap[:, hh, :], wuv.ap[:, h, :], start=True, stop=True),
                             reads=[olTn.res, wuv.res], writes=[psb[bq]])
                        P.op("act", E.copy(ogt.ap[:, h * 64:(h + 1) * 64], bank(bq)[:, 0:64]), reads=[psb[bq]], writes=[ogt.res])
                P.dma("sp", self.ogdram[s, tile * 128:(tile + 1) * 128, :], ogt.ap, reads=[ogt.res], writes=[self.og_res[s][tile]])
        self.phase_reset()
        Wo = self.sb([128, DC, D], BF16, "Wo")
        P.dma("pool", Wo.ap, self.dsa_w_out[j].rearrange("(k p) n -> p k n", p=128), writes=[Wo.res])
        self.out_proj_ln(L, s, Wo, self.ogdram[s], self.og_res[s], x_src, x_dst, last)
        self.phase_reset()

    def build(self):
        P = self.P
        self.prologue()
        for s in range(NSEQ):
            src = self.x_in[s]
            nsub = len(self.plan)
            for i, (kind, L) in enumerate(self.plan):
                last = (i == nsub - 1)
                dst = self.out[s] if last else self.xbuf[s]
                if kind == "ffn":
                    self.ffn(L, s, src, dst, last)
                elif kind == "rwkv":
                    self.rwkv(L, s, src, dst, last)
                elif kind == "dsa":
                    self.dsa(L, s, src, dst, last)
                src = self.xbuf[s]
        P.emit()
        self.es.close()
        return self.nc


FULL_PLAN = [("rwkv", 0), ("ffn", 0), ("dsa", 1), ("ffn", 1), ("rwkv", 2), ("ffn", 2), ("dsa", 3), ("ffn", 3)]


def make_consts():
    c = np.zeros((128, NCONST), np.float32)
    s_ = np.arange(128)[:, None]
    t_ = np.arange(128)[None, :]
    same = (s_ // 64) == (t_ // 64)
    su = (same & (s_ < t_)).astype(np.float32)
    iu = (same & (s_ <= t_)).astype(np.float32)
    c[:, 0:128] = su
    c[:, 128:256] = iu
    c[:, 256:384] = su
    c[:, 384:512] = iu
    c[:, 512:640] = su.T
    c[:, 640:768] = iu
    c[63, 768] = 1.0
    c[127, 769] = 1.0
    c[:, 832:896] = np.tile(np.eye(64, dtype=np.float32), (2, 1))
    c[0:64, 896:960] = 1.0
    c[64:128, 960:1024] = 1.0
    return c


def host_inputs(inputs, plan):
    f = lambda a: np.ascontiguousarray(np.asarray(a, dtype=np.float32))
    common = {
        "ada_w": f(inputs["ada_w"]), "ada_b": f(inputs["ada_b"]),
        "ln_g": f(inputs["ln_g"]), "ln_b": f(inputs["ln_b"]),
        "ffn_w_in": f(inputs["ffn_w_in"]), "ffn_w_out": f(inputs["ffn_w_out"]),
        "ident_f": np.eye(128, dtype=np.float32),
        "consts": make_consts(),
    }
    kinds = set(k for k, _ in plan)
    if "dsa" in kinds:
        for k_ in ("dsa_w_in", "dsa_kv_norm", "dsa_w_uk", "dsa_w_uv", "dsa_w_out"):
            common[k_] = f(inputs[k_])
        rb = f(inputs["rel_bias"])
        tt_ = np.arange(128)[None, :]; ss_ = np.arange(128)[:, None]
        caus = np.zeros((128, 4, 512), np.float32)
        for q4 in range(4):
            cc = np.arange(512)[None, :]
            caus[:, q4, :] = np.where(cc <= q4 * 128 + np.arange(128)[:, None], 0.0, -1e30)
        common["dsa_caus"] = caus
        band = np.zeros((128, 16, 2, 128), np.float32)
        for w_ in range(2):
            n = np.maximum(tt_ - ss_ + 128 * w_, 0)
            nf = np.maximum(n, 1).astype(np.float32)
            large = 16 + (np.log(nf / 16) / math.log(128 / 16) * 16).astype(np.int32)
            bucket = np.where(n < 16, n, np.minimum(large, 31))
            band[:, :, w_, :] = rb[bucket].transpose(0, 2, 1)
        common["dsa_band"] = np.ascontiguousarray(band.reshape(128, 16, 256))
        common["dsa_c31"] = np.ascontiguousarray(np.broadcast_to(rb[31][None, :], (128, 16)))
    if "rwkv" in kinds:
        mu = f(inputs["rwkv_mu"])
        common["rw_mu"] = np.ascontiguousarray(mu.reshape(2, 6, DC, 128).transpose(0, 3, 1, 2))
        common["rwkv_w_rkv"] = f(inputs["rwkv_w_rkv"])
        common["rwkv_w1"] = f(inputs["rwkv_w1"])
        common["rw_w2a"] = np.concatenate([f(inputs["rwkv_w2"]), f(inputs["rwkv_w0"])[:, None, :]], axis=1)
        common["rwkv_a1"] = f(inputs["rwkv_a1"])
        common["rw_a2a"] = np.concatenate([f(inputs["rwkv_a2"]), f(inputs["rwkv_a0"])[:, None, :]], axis=1)
        common["rwkv_v1"] = f(inputs["rwkv_v1"])
        common["rw_v2a"] = np.concatenate([f(inputs["rwkv_v2"]), f(inputs["rwkv_v0"])[:, None, :]], axis=1)
        common["rwkv_g1"] = f(inputs["rwkv_g1"])
        common["rwkv_g2"] = f(inputs["rwkv_g2"])
        common["rw_vec"] = np.stack([f(inputs["rwkv_k_k"]), f(inputs["rwkv_k_a"]), f(inputs["rwkv_r_k"]).reshape(2, D),
                                     f(inputs["rwkv_lnx_g"]), f(inputs["rwkv_lnx_b"])], axis=1)
        common["rwkv_w_out"] = f(inputs["rwkv_w_out"])
    x = f(inputs["x"])
    c = f(inputs["c"])
    maps = []
    for core in range(8):
        m = dict(common)
        m["x"] = x[core * NSEQ:(core + 1) * NSEQ]
        cc = c[core * NSEQ:(core + 1) * NSEQ]
        m["c_col"] = np.ascontiguousarray(cc.reshape(NSEQ, DC, 128).transpose(0, 2, 1))
        maps.append(m)
    return maps


def run(inputs, plan=FULL_PLAN, ncores=8):
    b = Builder(plan)
    nc = b.build()
    print("instr stats", b.P.stats())
    maps = host_inputs(inputs, plan)[:ncores]
    res = run_bass_kernel_spmd(nc, maps, core_ids=list(range(ncores)))
    if DBG.get("dump"):
        np.save("_dbg_og.npy", np.asarray(res.results[0]["ogdram"]).astype(np.float32))
    return np.concatenate([r["out"] for r in res.results], axis=0)


def kernel(**inputs):
    return run(inputs, FULL_PLAN)
```

```python
import contextlib
import math
import numpy as np
import concourse.bass as bass
import concourse.mybir as mybir
from concourse.bass_utils import run_bass_kernel_spmd

F32 = mybir.dt.float32
BF16 = mybir.dt.bfloat16
U8 = mybir.dt.uint8
AF = mybir.ActivationFunctionType
ALU = mybir.AluOpType
AX = mybir.AxisListType

D = 1024
T = 2048
NT = T // 128
DC = D // 128
DEPTH = 4
FH = 2816
ALPHA = (2 * DEPTH) ** 0.25
LN_EPS = 1e-5
GN_EPS = 64 * 1e-5
NCONST = 1024
WDECAY = math.exp(-0.5)
NSEQ = 2
DBG = {"stage": 99, "tiles": NT, "sub": 99}
ENGS = ("pe", "act", "pool", "dve", "sp")
DT_SIZE = {F32: 4, BF16: 2, U8: 1}


class _Rec:
    def __getattr__(self, name):
        def f(*args, **kw):
            return (name, args, kw)
        return f


E = _Rec()


class Ev:
    __slots__ = ("kind", "eng", "seq", "needed", "value")

    def __init__(self, kind, eng, seq):
        self.kind = kind
        self.eng = eng
        self.seq = seq
        self.needed = False
        self.value = None


class Res:
    __slots__ = ("name", "w", "r", "excl")

    def __init__(self, name="", excl=False):
        self.name = name
        self.w = None
        self.r = []
        self.excl = excl


class Prog:
    def __init__(self, nc, n_dma_sems=48):
        self.nc = nc
        self.streams = {e: [] for e in ENGS}
        self.n_dma_sems = n_dma_sems
        self.dma_last = [None] * n_dma_sems
        self.dma_cnt = [0] * n_dma_sems
        self.dma_rr = 0
        self.dma_rr_sw = 0
        self.known = {e: {} for e in ENGS}
        self.last_ev = {e: None for e in ENGS}

    def _collect(self, eng, reads, writes, include_same=False):
        deps = []
        for r in reads:
            if r.w is not None:
                deps.append(r.w)
        for w in writes:
            if w.w is not None:
                deps.append(w.w)
            deps.extend(w.r)
        kn = self.known[eng]
        best = {}
        for d in deps:
            if d.kind == "eng" and d.eng == eng and not include_same:
                continue
            key = (d.kind, d.eng)
            if kn.get(key, -1) >= d.seq:
                continue
            if key not in best or best[key].seq < d.seq:
                best[key] = d
        out = []
        for key, d in best.items():
            kn[key] = d.seq
            d.needed = True
            out.append(d)
        return out

    def op(self, eng, fn, reads=(), writes=()):
        ex = [r for r in reads if r.excl]
        if ex:
            writes = list(writes) + ex
        waits = self._collect(eng, reads, writes)
        if eng != "pe":
            kn = self.known[eng]
            key = ("eng", eng)
            m = None
            for r in reads:
                d = r.w
                if d is not None and d.kind == "eng" and d.eng == eng and kn.get(key, -1) < d.seq:
                    if m is None or m.seq < d.seq:
                        m = d
            if m is not None:
                kn[key] = m.seq
                m.needed = True
                waits.append(m)
        st = self.streams[eng]
        ev = Ev("eng", eng, len(st))
        st.append((fn, waits, ev))
        self.last_ev[eng] = ev
        for r in reads:
            r.r.append(ev)
        for w in writes:
            w.w = ev
            w.r = []
        return ev

    def dma(self, q, out_ap, in_ap, reads=(), writes=(), **kw):
        nh = self.n_dma_sems // 2
        if q == "pool":
            k = nh + self.dma_rr_sw
            self.dma_rr_sw = (self.dma_rr_sw + 1) % (self.n_dma_sems - nh)
        else:
            k = self.dma_rr
            self.dma_rr = (self.dma_rr + 1) % nh
        waits = self._collect(q, reads, writes, include_same=True)
        prev = self.dma_last[k]
        if prev is not None:
            key = ("dma", k)
            if self.known[q].get(key, -1) < prev.seq:
                self.known[q][key] = prev.seq
                waits.append(prev)
        self.dma_cnt[k] += 1
        ev = Ev("dma", k, self.dma_cnt[k])
        ev.needed = True
        ev.value = 16 * self.dma_cnt[k]
        self.dma_last[k] = ev

        def fn(e, out_ap=out_ap, in_ap=in_ap, kw=kw):
            return e.dma_start(out=out_ap, in_=in_ap, **kw)
        self.streams[q].append((fn, waits, ev))
        for r in reads:
            r.r.append(ev)
        for w in writes:
            w.w = ev
            w.r = []
        return ev

    def barrier(self):
        evs = [self.last_ev[e] for e in ENGS if self.last_ev[e] is not None]
        evs += [d for d in self.dma_last if d is not None]
        for e in ENGS:
            waits = []
            kn = self.known[e]
            for d in evs:
                if d.kind == "eng" and d.eng == e:
                    continue
                key = (d.kind, d.eng)
                if kn.get(key, -1) >= d.seq:
                    continue
                kn[key] = d.seq
                d.needed = True
                waits.append(d)
            if waits:
                self.streams[e].append((None, waits, None))

    def emit(self):
        nc = self.nc
        self.barrier()
        for e in ENGS:
            c = 0
            for fn, waits, ev in self.streams[e]:
                if ev is not None and ev.kind == "eng" and ev.needed:
                    c += 1
                    ev.value = c
        with contextlib.ExitStack() as es:
            esem = {e: es.enter_context(nc.semaphore("s_" + e)) for e in ENGS}
            dsem = [es.enter_context(nc.semaphore("d_%d" % k)) for k in range(self.n_dma_sems)]

            def semof(ev):
                return esem[ev.eng] if ev.kind == "eng" else dsem[ev.eng]

            block = es.enter_context(nc.Block())

            def make(ename):
                def body(eh):
                    for fn, waits, ev in self.streams[ename]:
                        for w in waits:
                            eh.wait_ge(semof(w), w.value)
                        if fn is None:
                            continue
                        if isinstance(fn, tuple):
                            ins = getattr(eh, fn[0])(*fn[1], **fn[2])
                        else:
                            ins = fn(eh)
                        if ev.kind == "dma":
                            ins.then_inc(dsem[ev.eng], 16)
                        elif ev.needed:
                            ins.then_inc(esem[ename], 1)
                return body

            block.tensor(make("pe"))
            block.scalar(make("act"))
            block.gpsimd(make("pool"))
            block.vector(make("dve"))
            block.sync(make("sp"))

    def stats(self):
        return {e: len(self.streams[e]) for e in ENGS}


class Tile:
    def __init__(self, ap, name=""):
        self.ap = ap
        self.res = Res(name)
        self.sub = {}

    def r(self, key=None):
        if key is None:
            return self.res
        if key not in self.sub:
            self.sub[key] = Res()
        return self.sub[key]


class Builder:
    def __init__(self, plan, debug=False):
        self.plan = plan
        self.nc = nc = bass.Bass("TRN2", target_bir_lowering=False)
        self.P = Prog(nc)
        self.es = contextlib.ExitStack()
        self.ARENA = 206 * 1024
        self.arena = self.es.enter_context(nc.sbuf_tensor("arena", [128, self.ARENA], U8))
        self.psum = self.es.enter_context(nc.psum_tensor("psum", [128, 8, 512], F32))
        self.off = 0
        self.persist_off = 0
        self.rr = {"ev": 0}
        self.psb = [Res("bank%d" % i, excl=True) for i in range(8)]
        self.moddram_res = Res("moddram")
        self.outres = Res("out")
        self.xres_ = [[Res() for _ in range(NT)] for _ in range(NSEQ)]
        self.vf_res = [[Res() for _ in range(NT)] for _ in range(NSEQ)]
        self.og_res = [[Res() for _ in range(NT)] for _ in range(NSEQ)]
        self.decl_io()

    def sb(self, shape, dt, name=""):
        n = int(np.prod(shape[1:])) * DT_SIZE[dt]
        n_al = (n + 63) // 64 * 64
        assert self.off + n_al <= self.ARENA, ("SBUF arena overflow", name, self.off, n_al)
        v = self.arena[0:shape[0], self.off:self.off + n].bitcast(dt)
        if not hasattr(self, 'alloc_log'):
            self.alloc_log = []
        self.alloc_log.append((name, self.off, n, dt, list(shape)))
        self.off += n_al
        if len(shape) == 3:
            v = v.rearrange("p (a b) -> p a b", b=shape[2])
        elif len(shape) == 4:
            v = v.rearrange("p (a b c) -> p a b c", b=shape[2], c=shape[3])
        elif len(shape) == 5:
            v = v.rearrange("p (a b c d) -> p a b c d", b=shape[2], c=shape[3], d=shape[4])
        return Tile(v, name)

    def phase_reset(self):
        self.P.barrier()
        self.off = self.persist_off

    def bank(self, i, dt=F32):
        v = self.psum[:, i, :]
        if dt == BF16:
            v = v.bitcast(BF16)
        return v

    def dram_in(self, name, shape, dt=F32):
        return self.nc.dram_tensor(name, list(shape), dt, kind="ExternalInput").ap()

    def dram_tmp(self, name, shape, dt=F32):
        return self.nc.dram_tensor(name, list(shape), dt, kind="Internal").ap()

    def decl_io(self):
        di = self.dram_in
        self.x_in = di("x", [NSEQ, T, D])
        self.c_col = di("c_col", [NSEQ, 128, DC])
        self.ada_w = di("ada_w", [DEPTH, D, 6 * D])
        self.ada_b = di("ada_b", [DEPTH, 6 * D])
        self.ln_g = di("ln_g", [DEPTH, 2, D])
        self.ln_b = di("ln_b", [DEPTH, 2, D])
        self.ffn_w_in = di("ffn_w_in", [DEPTH, D, 2 * FH])
        self.ffn_w_out = di("ffn_w_out", [DEPTH, FH, D])
        self.ident_f = di("ident_f", [128, 128])
        self.consts = di("consts", [128, NCONST])
        kinds = set(k for k, _ in self.plan)
        if "dsa" in kinds:
            self.dsa_w_in = di("dsa_w_in", [2, D, 1736])
            self.dsa_kv_norm = di("dsa_kv_norm", [2, 128])
            self.dsa_w_uk = di("dsa_w_uk", [2, 16, 64, 128])
            self.dsa_w_uv = di("dsa_w_uv", [2, 16, 128, 64])
            self.dsa_w_out = di("dsa_w_out", [2, D, D])
            self.dsa_caus = di("dsa_caus", [128, 4, 512])
            self.dsa_band = di("dsa_band", [128, 16, 256])
            self.dsa_c31 = di("dsa_c31", [128, 16])
            if "rwkv" not in kinds:
                self.ogdram = (self.nc.dram_tensor("ogdram", [NSEQ, T, D], BF16, kind="ExternalOutput").ap() if DBG.get("dump") else self.dram_tmp("ogdram", [NSEQ, T, D], BF16))
        if "rwkv" in kinds:
            self.rw_mu = di("rw_mu", [2, 128, 6, DC])
            self.rw_wrkv = di("rwkv_w_rkv", [2, 3, D, D])
            self.rw_w1 = di("rwkv_w1", [2, D, 64])
            self.rw_w2a = di("rw_w2a", [2, 65, D])
            self.rw_a1 = di("rwkv_a1", [2, D, 64])
            self.rw_a2a = di("rw_a2a", [2, 65, D])
            self.rw_v1 = di("rwkv_v1", [1, D, 32])
            self.rw_v2a = di("rw_v2a", [1, 33, D])
            self.rw_g1 = di("rwkv_g1", [2, D, 160])
            self.rw_g2 = di("rwkv_g2", [2, 160, D])
            self.rw_vec = di("rw_vec", [2, 5, D])
            self.rw_wout = di("rwkv_w_out", [2, D, D])
            self.vfirst = self.dram_tmp("vfirst", [NSEQ, T, D])
            self.ogdram = (self.nc.dram_tensor("ogdram", [NSEQ, T, D], BF16, kind="ExternalOutput").ap() if DBG.get("dump") else self.dram_tmp("ogdram", [NSEQ, T, D], BF16))
        self.out = self.nc.dram_tensor("out", [NSEQ, T, D], F32, kind="ExternalOutput").ap()
        self.xbuf = self.dram_tmp("xbuf", [NSEQ, T, D])
        self.moddram = self.dram_tmp("moddram", [DEPTH, NSEQ, 2, D])

    def ev_eng(self):
        self.rr["ev"] ^= 1
        return "dve" if self.rr["ev"] else "act"

    def prologue(self):
        P = self.P
        self.ident = self.sb([128, 128], F32, "ident")
        P.dma("sp", self.ident.ap, self.ident_f, writes=[self.ident.res])
        self.identb = self.sb([128, 128], BF16, "identb")
        P.op("dve", E.tensor_copy(self.identb.ap, self.ident.ap),
             reads=[self.ident.res], writes=[self.identb.res])
        self.modcol = self.sb([128, DEPTH * NSEQ * 4, DC], F32, "modcol")
        self.persist_off = self.off
        ccol = self.sb([128, NSEQ, DC], F32, "ccol")
        cond = self.sb([128, DC, NSEQ], F32, "cond")
        P.dma("sp", ccol.ap, self.c_col.rearrange("s p k -> p s k"), writes=[ccol.res])
        for s in range(NSEQ):
            P.op("act", E.activation(cond.ap[:, :, s], ccol.ap[:, s, :], AF.Silu),
                 reads=[ccol.res], writes=[cond.res])
        wbuf = [self.sb([128, DC, 512], F32, "adaw%d" % i) for i in range(2)]
        bbuf = [self.sb([NSEQ, 512], F32, "adab%d" % i) for i in range(2)]
        rowb = [self.sb([NSEQ, 512], F32, "modrow%d" % i) for i in range(2)]
        cnt = 0
        for L in range(DEPTH):
            for ch in range(12):
                wb, bb, rb = wbuf[cnt % 2], bbuf[cnt % 2], rowb[cnt % 2]
                pb = cnt % 2
                cnt += 1
                P.dma("sp", wb.ap, self.ada_w[L, :, ch * 512:(ch + 1) * 512].rearrange("(k p) n -> p k n", p=128),
                      writes=[wb.res])
                P.dma("act", bb.ap, self.ada_b[L:L + 1, ch * 512:(ch + 1) * 512].broadcast_to([NSEQ, 512]),
                      writes=[bb.res])
                pr = self.psb[pb]
                for k in range(DC):
                    P.op("pe", E.matmul(self.bank(pb)[0:NSEQ, :], cond.ap[:, k, :], wb.ap[:, k, :],
                                                                  start=(k == 0), stop=(k == DC - 1)),
                         reads=[cond.res, wb.res], writes=[pr])
                vec = ch // 2
                half = ch % 2
                addone = 0.0 if vec in (0, 3) else 1.0
                P.op("dve", E.scalar_tensor_tensor(
                    rb.ap, self.bank(pb)[0:NSEQ, :], addone, bb.ap, ALU.add, ALU.add),
                    reads=[pr, bb.res], writes=[rb.res])
                if vec in (2, 5):
                    P.dma("sp", self.moddram[L, :, 0 if vec == 2 else 1, half * 512:(half + 1) * 512], rb.ap,
                          reads=[rb.res], writes=[self.moddram_res])
                else:
                    vi = {0: 0, 1: 1, 3: 2, 4: 3}[vec]
                    pc = self.psb[2 + pb]
                    for s in range(NSEQ):
                        for kk in range(4):
                            P.op("pe", E.matmul(
                                self.bank(2 + pb)[:, s * 4 + kk:s * 4 + kk + 1], rb.ap[:, kk * 128:(kk + 1) * 128],
                                self.ident.ap[0:NSEQ, s:s + 1], start=True, stop=True),
                                reads=[rb.res, self.ident.res], writes=[pc])
                    for s in range(NSEQ):
                        idx = (L * NSEQ + s) * 4 + vi
                        P.op("dve", E.tensor_copy(
                            self.modcol.ap[:, idx, half * 4:(half + 1) * 4], self.bank(2 + pb)[:, s * 4:(s + 1) * 4]),
                            reads=[pc], writes=[self.modcol.res])
        self.phase_reset()

    def mcol(self, L, s, vi, k):
        return self.modcol.ap[:, (L * NSEQ + s) * 4 + vi, k:k + 1]

    def make_hT(self, x_src, L, s, vsh, vsc, hT):
        P = self.P
        xt = self.xring
        n = 0
        for tg in range(4):
            tiles = []
            for j in range(4):
                t_ = xt[n % 4]
                n += 1
                tok = (tg * 4 + j) * 128
                P.dma("sp", t_.ap, x_src[tok:tok + 128, :], reads=[self.xres(s, tg * 4 + j)], writes=[t_.res])
                tiles.append(t_)
            for dc in range(DC):
                pb = dc % 2
                pr = self.psb[pb]
                for j in range(4):
                    P.op("pe", E.transpose(
                        self.bank(pb)[:, j * 128:(j + 1) * 128], tiles[j].ap[:, dc * 128:(dc + 1) * 128], self.ident.ap),
                        reads=[tiles[j].res, self.ident.res], writes=[pr])
                eng = self.ev_eng()
                dst = hT.ap[:, dc, tg * 512:(tg + 1) * 512]
                sc = self.mcol(L, s, vsc, dc)
                sh = self.mcol(L, s, vsh, dc)
                if eng == "dve":
                    P.op("dve", E.tensor_scalar(
                        dst, self.bank(pb), sc, sh, ALU.mult, ALU.add),
                        reads=[pr, self.modcol.res], writes=[hT.r(tg)])
                else:
                    P.op("act", E.activation(
                        dst, self.bank(pb), AF.Identity, bias=sh, scale=sc),
                        reads=[pr, self.modcol.res], writes=[hT.r(tg)])

    def load_ln_consts(self, L, s, which, need_z=True):
        P = self.P
        G = self.sb([128, D], F32, "G")
        g = self.sb([128, D], F32, "lng")
        b = self.sb([128, D], F32, "lnb")
        P.dma("sp", G.ap, self.moddram[L, s, which:which + 1, :].broadcast_to([128, D]),
              reads=[self.moddram_res], writes=[G.res])
        P.dma("sp", g.ap, self.ln_g[L, which:which + 1, :].broadcast_to([128, D]), writes=[g.res])
        P.dma("sp", b.ap, self.ln_b[L, which:which + 1, :].broadcast_to([128, D]), writes=[b.res])
        self.lnc = (G, g, b)
        self.ln_x = self.xring
        self.ln_z = [self.sb([128, D], F32, "lnz%d" % i) for i in range(3)] if need_z else None
        self.ln_st = [self.sb([128, 16], F32, "lnst%d" % i) for i in range(4)]
        self.ln_n = 0

    def ln_epilogue(self, s, tile, y_aps, y_res, x_src, x_dst, dst_res=None, z_tile=None):
        P = self.P
        G, g, b = self.lnc
        i = self.ln_n
        self.ln_n += 1
        xt, st = self.ln_x[i % 4], self.ln_st[i % 4]
        z = self.ln_z[i % 3] if z_tile is None else z_tile
        tok = tile * 128
        P.dma("sp", xt.ap, x_src[tok:tok + 128, :], reads=[self.xres(s, tile)], writes=[xt.res])
        for ap, c0, w in y_aps:
            P.op("dve", E.tensor_tensor(z.ap[:, c0:c0 + w], ap, G.ap[:, c0:c0 + w], ALU.mult),
                 reads=list(y_res) + [G.res], writes=[z.res])
        P.op("dve", E.scalar_tensor_tensor(z.ap, xt.ap, float(ALPHA), z.ap, ALU.mult, ALU.add),
             reads=[xt.res, z.res], writes=[z.res])
        P.op("dve", E.bn_stats(st.ap[:, 0:6], z.ap[:, 0:512]), reads=[z.res], writes=[st.r("a")])
        P.op("dve", E.bn_stats(st.ap[:, 6:12], z.ap[:, 512:1024]), reads=[z.res], writes=[st.r("b")])
        P.op("dve", E.bn_aggr(st.ap[:, 12:14], st.ap[:, 0:12]), reads=[st.r("a"), st.r("b")], writes=[st.r("c")])
        P.op("act", E.activation(st.ap[:, 14:15], st.ap[:, 13:14], AF.Sqrt, bias=LN_EPS),
             reads=[st.r("c")], writes=[st.r("d")])
        P.op("dve", E.reciprocal(st.ap[:, 14:15], st.ap[:, 14:15]), reads=[st.r("d")], writes=[st.r("d")])
        P.op("dve", E.tensor_scalar(st.ap[:, 15:16], st.ap[:, 12:13], st.ap[:, 14:15], -1.0, ALU.mult, ALU.mult),
             reads=[st.r("c"), st.r("d")], writes=[st.r("e")])
        P.op("act", E.activation(z.ap, z.ap, AF.Identity, bias=st.ap[:, 15:16], scale=st.ap[:, 14:15]),
             reads=[z.res, st.r("d"), st.r("e")], writes=[z.res])
        P.op("pool", E.tensor_tensor(z.ap, z.ap, g.ap, ALU.mult), reads=[z.res, g.res], writes=[z.res])
        P.op("pool", E.tensor_tensor(z.ap, z.ap, b.ap, ALU.add), reads=[z.res, b.res], writes=[z.res])
        P.dma("pool", x_dst[tok:tok + 128, :], z.ap, reads=[z.res],
              writes=[dst_res if dst_res is not None else self.xres(s, tile)])

    def xres(self, s, tile):
        return self.xres_[s][tile]

    def ffn(self, L, s, x_src, x_dst, last=False):
        P = self.P
        hT = self.sb([128, DC, T], BF16, "hT")
        self.xring = [self.sb([128, D], F32, "xr%d" % i) for i in range(4)]
        self.make_hT(x_src, L, s, 2, 3, hT)
        yacc = self.sb([128, NT, D], F32, "yacc")
        wg = [self.sb([128, DC, 512], BF16, "wg%d" % i) for i in range(2)]
        wu = [self.sb([128, DC, 512], BF16, "wu%d" % i) for i in range(2)]
        wo = [self.sb([128, 4, D], BF16, "wo%d" % i) for i in range(2)]
        hh = [self.sb([128, 4, 512], BF16, "hh%d" % i) for i in range(2)]
        sg = [self.sb([128, 512], F32, "sg%d" % i) for i in range(2)]
        groups = [(g * 512, 512) for g in range(5)] + [(2560, 256)]
        nhh = 0
        nsg = 0
        npy = 0
        for gi, (h0, gs) in enumerate(groups):
            nhc = gs // 128
            a, u, o = wg[gi % 2], wu[gi % 2], wo[gi % 2]
            P.dma("pool", a.ap[:, :, 0:gs], self.ffn_w_in[L, :, h0:h0 + gs].rearrange("(k p) n -> p k n", p=128),
                  writes=[a.res])
            P.dma("pool", u.ap[:, :, 0:gs], self.ffn_w_in[L, :, FH + h0:FH + h0 + gs].rearrange("(k p) n -> p k n", p=128),
                  writes=[u.res])
            P.dma("pool", o.ap[:, 0:nhc, :], self.ffn_w_out[L, h0:h0 + gs, :].rearrange("(c p) n -> p c n", p=128),
                  writes=[o.res])
            for tb in range(4):
                hb = hh[nhh % 2]
                nhh += 1
                for hc in range(nhc):
                    pg, pu = self.psb[0 + (hc % 2)], self.psb[2 + (hc % 2)]
                    bg, bu = 0 + (hc % 2), 2 + (hc % 2)
                    for k in range(DC):
                        P.op("pe", E.matmul(
                            self.bank(bg), a.ap[:, k, hc * 128:(hc + 1) * 128], hT.ap[:, k, tb * 512:(tb + 1) * 512],
                            start=(k == 0), stop=(k == DC - 1)), reads=[a.res, hT.r(tb)], writes=[pg])
                    for k in range(DC):
                        P.op("pe", E.matmul(
                            self.bank(bu), u.ap[:, k, hc * 128:(hc + 1) * 128], hT.ap[:, k, tb * 512:(tb + 1) * 512],
                            start=(k == 0), stop=(k == DC - 1)), reads=[u.res, hT.r(tb)], writes=[pu])
                    sgt = sg[nsg % 2]
                    nsg += 1
                    P.op("act", E.activation(sgt.ap, self.bank(bg), AF.Silu),
                         reads=[pg], writes=[sgt.res])
                    P.op("dve", E.tensor_tensor(
                        hb.ap[:, hc, :], sgt.ap, self.bank(bu), ALU.mult),
                        reads=[sgt.res, pu], writes=[hb.res])
                for tt in range(4):
                    tile = tb * 4 + tt
                    for half in range(2):
                        bpy = 4 + (npy % 4)
                        npy += 1
                        py = self.psb[bpy]
                        for hc in range(nhc):
                            P.op("pe", E.matmul(
                                self.bank(bpy), hb.ap[:, hc, tt * 128:(tt + 1) * 128], o.ap[:, hc, half * 512:(half + 1) * 512],
                                start=(hc == 0), stop=(hc == nhc - 1)), reads=[hb.res, o.res], writes=[py])
                        dst = yacc.ap[:, tile, half * 512:(half + 1) * 512]
                        if gi == 0:
                            P.op("act", E.copy(dst, self.bank(bpy)),
                                 reads=[py], writes=[yacc.r((tile, half))])
                        else:
                            P.op("dve", E.tensor_tensor(dst, dst, self.bank(bpy), ALU.add),
                                 reads=[py, yacc.r((tile, half))], writes=[yacc.r((tile, half))])
        self.load_ln_consts(L, s, 1, need_z=False)
        for tile in range(NT):
            zt = Tile(yacc.ap[:, tile, :])
            zt.res = yacc.r((tile, 0))
            self.ln_epilogue(s, tile, [(yacc.ap[:, tile, :], 0, D)], [yacc.r((tile, 0)), yacc.r((tile, 1))],
                             x_src, x_dst, dst_res=(self.outres if last else None), z_tile=zt)
        self.phase_reset()

    def rwkv(self, L, s, x_src, x_dst, last=False):
        P = self.P
        j = L // 2
        CW = float(WDECAY)
        bank = self.bank
        psb = self.psb
        cst = self.sb([128, NCONST], F32, "cst")
        P.dma("sp", cst.ap, self.consts, writes=[cst.res])
        maskG = cst.ap[:, 0:512]
        maskNT = cst.ap[:, 512:640]
        triBD = cst.ap[:, 640:768]
        sel63 = cst.ap[:, 768:770]
        Wrkv = [self.sb([128, DC, D], BF16, "Wrkv%d" % p) for p in range(3)]
        for p in range(3):
            P.dma("pool", Wrkv[p].ap, self.rw_wrkv[j, p].rearrange("(k p) n -> p k n", p=128), writes=[Wrkv[p].res])
        w1 = self.sb([128, DC, 64], BF16, "w1")
        a1 = self.sb([128, DC, 64], BF16, "a1")
        g1 = self.sb([128, DC, 160], BF16, "g1")
        P.dma("pool", w1.ap, self.rw_w1[j].rearrange("(k p) n -> p k n", p=128), writes=[w1.res])
        P.dma("pool", a1.ap, self.rw_a1[j].rearrange("(k p) n -> p k n", p=128), writes=[a1.res])
        P.dma("pool", g1.ap, self.rw_g1[j].rearrange("(k p) n -> p k n", p=128), writes=[g1.res])
        w2a = self.sb([65, D], BF16, "w2a")
        a2a = self.sb([65, D], BF16, "a2a")
        g2a = self.sb([128, D], BF16, "g2a")
        g2b = self.sb([32, D], BF16, "g2b")
        P.dma("pool", w2a.ap, self.rw_w2a[j], writes=[w2a.res])
        P.dma("pool", a2a.ap, self.rw_a2a[j], writes=[a2a.res])
        P.dma("pool", g2a.ap, self.rw_g2[j, 0:128, :], writes=[g2a.res])
        P.dma("pool", g2b.ap, self.rw_g2[j, 128:160, :], writes=[g2b.res])
        if j > 0:
            v1 = self.sb([128, DC, 32], BF16, "v1")
            v2a = self.sb([33, D], BF16, "v2a")
            P.dma("pool", v1.ap, self.rw_v1[j - 1].rearrange("(k p) n -> p k n", p=128), writes=[v1.res])
            P.dma("pool", v2a.ap, self.rw_v2a[j - 1], writes=[v2a.res])
        mu = self.sb([128, 6, DC], F32, "mu")
        P.dma("sp", mu.ap, self.rw_mu[j], writes=[mu.res])
        vec = self.sb([128, 5, D], F32, "vec")
        P.dma("sp", vec.ap, self.rw_vec[j:j + 1].broadcast_to([128, 5, D]), writes=[vec.res])
        hT = self.sb([128, DC, T], BF16, "hT")
        ring_off = self.off
        self.xring = [self.sb([128, D], F32, "xr%d" % i) for i in range(4)]
        self.make_hT(x_src, L, s, 0, 1, hT)
        P.barrier()
        self.off = ring_off
        f32t = lambda n: self.sb([128, 512], F32, n)
        bft = lambda n: self.sb([128, 512], BF16, n)
        xx = self.sb([128, DC, 128], BF16, "xx")
        xs = self.sb([128, 6, DC, 128], BF16, "xs")
        r_sb, k_sb, v_sb, a_sb, sgw, kk, kp, u_, rk, eL, enL, eLm, g_sb, sq = [
            f32t(n) for n in "r k v a sgw kk kp u rk eL enL eLm g sq".split()]
        y_sb, yc = eLm, u_
        bonus = rk
        At, Bt, Kt, Rt, Vt = [bft(n) for n in "At Bt Kt Rt Vt".split()]
        FT = self.sb([128, 4, 4, 128], BF16, "FT")
        Gm = self.sb([128, 4, 512], BF16, "Gm")
        Pm = [self.sb([128, 2, 4, 128], BF16, "Pm%d" % i) for i in range(2)]
        X = [self.sb([128, 4, 128], BF16, "X%d" % i) for i in range(2)]
        MT = self.sb([128, 2, 4, 64], BF16, "MT")
        Zs = self.sb([128, 2, 4, 64], F32, "Zs")
        RhF = self.sb([128, 4, 128], BF16, "RhF")
        Y2s = self.sb([128, 512], F32, "Y2s")
        gcol = self.sb([128, 2, 16], F32, "gcol")
        MTbd = self.sb([128, 2, 4, 2, 64], BF16, "MTbd")
        Zbd = self.sb([128, 2, 4, 2, 64], F32, "Zbd")
        mbd = cst.ap[:, 896:1024].rearrange("p (a b) -> p a b", b=64)
        vf, vg = Y2s, sq
        S = self.sb([128, 2, 4, 128], F32, "S")
        Sb = self.sb([128, 2, 4, 128], BF16, "Sb")
        sm = self.sb([128, 64], F32, "sm")
        l1w = self.sb([65, 128], BF16, "l1w")
        l1a = self.sb([65, 128], BF16, "l1a")
        l1v = self.sb([33, 128], BF16, "l1v")
        l1g = self.sb([128, 128], BF16, "l1g")
        l1g2 = self.sb([32, 128], BF16, "l1g2")
        P.op("pool", E.memset(S.ap, 0.0), writes=[S.r(0), S.r(1)])
        P.op("pool", E.memset(Sb.ap, 0.0), writes=[Sb.r(0), Sb.r(1)])
        P.op("pool", E.memset(l1w.ap, 1.0), writes=[l1w.res])
        P.op("pool", E.memset(l1a.ap, 1.0), writes=[l1a.res])
        P.op("pool", E.memset(l1v.ap, 1.0), writes=[l1v.res])
        identb = self.identb
        nb = [0]

        def gbank():
            nb[0] ^= 1
            return nb[0]

        def bc8(ap8):
            return ap8.unsqueeze(2).to_broadcast([128, 8, 64])

        def v3(ap):
            return ap.rearrange("p (a b) -> p a b", b=64)

        for tile in range(min(NT, DBG["tiles"])):
            t0 = tile * 128
            tg = tile // 4
            if tile == 0:
                P.op("dve", E.tensor_tensor(xx.ap[:, :, 1:128], hT.ap[:, :, 0:127], hT.ap[:, :, 1:128], ALU.subtract),
                     reads=[hT.r(0)], writes=[xx.res])
                P.op("dve", E.tensor_scalar(xx.ap[:, :, 0:1], hT.ap[:, :, 0:1], -1.0, None, ALU.mult),
                     reads=[hT.r(0)], writes=[xx.res])
            else:
                P.op("dve", E.tensor_tensor(xx.ap, hT.ap[:, :, t0 - 1:t0 + 127], hT.ap[:, :, t0:t0 + 128], ALU.subtract),
                     reads=[hT.r(tg), hT.r((t0 - 1) // 512)], writes=[xx.res])
            for p in range(6):
                for dc in range(DC):
                    eng = "dve" if (p * DC + dc) % 2 == 0 else "dve"
                    P.op(eng, E.scalar_tensor_tensor(
                        xs.ap[:, p, dc, :], xx.ap[:, dc, :], mu.ap[:, p, dc:dc + 1], hT.ap[:, dc, t0:t0 + 128], ALU.mult, ALU.add),
                        reads=[xx.res, mu.res, hT.r(tg)], writes=[xs.r(p)])
            def lora1(wt, p, n, dst, func, b):
                for dc in range(DC):
                    P.op("pe", E.matmul(bank(b)[0:n, 0:128], wt.ap[:, dc, 0:n], xs.ap[:, p, dc, :],
                                                         start=(dc == 0), stop=(dc == DC - 1)),
                         reads=[wt.res, xs.r(p)], writes=[psb[b]])
                P.op("act", E.activation(dst.ap[0:n, :], bank(b)[0:n, 0:128], func),
                     reads=[psb[b]], writes=[dst.res])
            lora1(w1, 3, 64, l1w, AF.Tanh, gbank())
            lora1(a1, 4, 64, l1a, AF.Copy, gbank())
            if j > 0:
                lora1(v1, 2, 32, l1v, AF.Copy, gbank())
            b = gbank()
            for dc in range(DC):
                P.op("pe", E.matmul(bank(b)[:, 0:128], g1.ap[:, dc, 0:128], xs.ap[:, 5, dc, :],
                                                          start=(dc == 0), stop=(dc == DC - 1)),
                     reads=[g1.res, xs.r(5)], writes=[psb[b]])
            P.op("act", E.activation(l1g.ap, bank(b)[:, 0:128], AF.Sigmoid), reads=[psb[b]], writes=[l1g.res])
            b = gbank()
            for dc in range(DC):
                P.op("pe", E.matmul(bank(b)[0:32, 0:128], g1.ap[:, dc, 128:160], xs.ap[:, 5, dc, :],
                                                          start=(dc == 0), stop=(dc == DC - 1)),
                     reads=[g1.res, xs.r(5)], writes=[psb[b]])
            P.op("act", E.activation(l1g2.ap, bank(b)[0:32, 0:128], AF.Sigmoid), reads=[psb[b]], writes=[l1g2.res])

            if DBG["stage"] < 2:
                continue
            for hf in range(2):
                c0 = hf * 512
                def proj(p, W, dst, eng):
                    b = gbank()
                    for dc in range(DC):
                        P.op("pe", E.matmul(bank(b), xs.ap[:, p, dc, :], W.ap[:, dc, c0:c0 + 512],
                                                                  start=(dc == 0), stop=(dc == DC - 1)),
                             reads=[xs.r(p), W.res], writes=[psb[b]])
                    if eng == "act":
                        P.op("act", E.copy(dst.ap, bank(b)), reads=[psb[b]], writes=[dst.res])
                    else:
                        P.op("dve", E.tensor_copy(dst.ap, bank(b)), reads=[psb[b]], writes=[dst.res])
                proj(0, Wrkv[0], r_sb, "act")
                proj(1, Wrkv[1], k_sb, "dve")
                proj(2, Wrkv[2], v_sb, "act")

                def lora2(l1t, n, w2t, dst, func):
                    b = gbank()
                    P.op("pe", E.matmul(bank(b), l1t.ap[0:n, :], w2t.ap[0:n, c0:c0 + 512], start=True, stop=True),
                         reads=[l1t.res, w2t.res], writes=[psb[b]])
                    P.op("act", E.activation(dst.ap, bank(b), func), reads=[psb[b]], writes=[dst.res])
                lora2(l1w, 65, w2a, sgw, AF.Sigmoid)
                lora2(l1a, 65, a2a, a_sb, AF.Sigmoid)
                b = gbank()
                P.op("pe", E.matmul(bank(b), l1g.ap, g2a.ap[:, c0:c0 + 512], start=True, stop=False),
                     reads=[l1g.res, g2a.res], writes=[psb[b]])
                P.op("pe", E.matmul(bank(b), l1g2.ap, g2b.ap[:, c0:c0 + 512], start=False, stop=True),
                     reads=[l1g2.res, g2b.res], writes=[psb[b]])
                P.op("act", E.copy(g_sb.ap, bank(b)), reads=[psb[b]], writes=[g_sb.res])
                if j == 0:
                    P.dma("sp", self.vfirst[s, t0:t0 + 128, c0:c0 + 512], v_sb.ap, reads=[v_sb.res], writes=[self.vf_res[s][tile]])
                else:
                    lora2(l1v, 33, v2a, vg, AF.Sigmoid)
                    P.dma("sp", vf.ap, self.vfirst[s, t0:t0 + 128, c0:c0 + 512], reads=[self.vf_res[s][tile]], writes=[vf.res])
                    P.op("pool", E.tensor_tensor(vf.ap, vf.ap, v_sb.ap, ALU.subtract), reads=[vf.res, v_sb.res], writes=[vf.res])
                    P.op("pool", E.tensor_tensor(vf.ap, vf.ap, vg.ap, ALU.mult), reads=[vf.res, vg.res], writes=[vf.res])
                    P.op("pool", E.tensor_tensor(v_sb.ap, v_sb.ap, vf.ap, ALU.add), reads=[vf.res, v_sb.res], writes=[v_sb.res])
                bL = gbank()
                P.op("pe", E.matmul(bank(bL), triBD, sgw.ap, start=True, stop=True),
                     reads=[cst.res, sgw.res], writes=[psb[bL]])
                P.op("act", E.activation(eL.ap, bank(bL), AF.Exp, scale=-CW), reads=[psb[bL]], writes=[eL.res])
                P.op("act", E.activation(enL.ap, bank(bL), AF.Exp, scale=CW), reads=[psb[bL]], writes=[enL.res])
                P.op("dve", E.tensor_tensor(eLm.ap, bank(bL), sgw.ap, ALU.subtract), reads=[psb[bL], sgw.res], writes=[eLm.res])
                P.op("act", E.activation(eLm.ap, eLm.ap, AF.Exp, scale=-CW), reads=[eLm.res], writes=[eLm.res])
                P.op("pool", E.tensor_tensor(kk.ap, k_sb.ap, vec.ap[:, 0, c0:c0 + 512], ALU.mult),
                     reads=[k_sb.res, vec.res], writes=[kk.res])
                P.op("pool", E.tensor_tensor(sq.ap, kk.ap, kk.ap, ALU.mult), reads=[kk.res], writes=[sq.res])
                P.op("dve", E.tensor_reduce(sm.ap[:, 0:8], v3(sq.ap), AX.X, ALU.add), reads=[sq.res], writes=[sm.r("ss")])
                P.op("act", E.activation(sm.ap[:, 0:8], sm.ap[:, 0:8], AF.Sqrt), reads=[sm.r("ss")], writes=[sm.r("ss")])
                P.op("dve", E.tensor_scalar_max(sm.ap[:, 0:8], sm.ap[:, 0:8], 1e-12), reads=[sm.r("ss")], writes=[sm.r("ss")])
                P.op("dve", E.reciprocal(sm.ap[:, 0:8], sm.ap[:, 0:8]), reads=[sm.r("ss")], writes=[sm.r("ss")])
                P.op("dve", E.tensor_tensor(v3(kk.ap), v3(kk.ap), bc8(sm.ap[:, 0:8]), ALU.mult),
                     reads=[kk.res, sm.r("ss")], writes=[kk.res])
                P.op("dve", E.scalar_tensor_tensor(u_.ap, a_sb.ap, -1.0, vec.ap[:, 1, c0:c0 + 512], ALU.add, ALU.mult),
                     reads=[a_sb.res, vec.res], writes=[u_.res])
                P.op("dve", E.scalar_tensor_tensor(kp.ap, u_.ap, 1.0, k_sb.ap, ALU.add, ALU.mult),
                     reads=[u_.res, k_sb.res], writes=[kp.res])
                P.op("dve", E.scalar_tensor_tensor(At.ap, kk.ap, -1.0, eLm.ap, ALU.mult, ALU.mult),
                     reads=[kk.res, eLm.res], writes=[At.res])
                P.op("pool", E.tensor_tensor(u_.ap, kk.ap, a_sb.ap, ALU.mult), reads=[kk.res, a_sb.res, u_.res], writes=[u_.res])
                P.op("pool", E.tensor_tensor(Bt.ap, u_.ap, enL.ap, ALU.mult), reads=[u_.res, enL.res], writes=[Bt.res])
                P.op("pool", E.tensor_tensor(Kt.ap, kp.ap, enL.ap, ALU.mult), reads=[kp.res, enL.res], writes=[Kt.res])
                P.op("dve", E.tensor_tensor(Rt.ap, r_sb.ap, eL.ap, ALU.mult), reads=[r_sb.res, eL.res], writes=[Rt.res])
                P.op("act", E.copy(Vt.ap, v_sb.ap), reads=[v_sb.res], writes=[Vt.res])
                P.op("pool", E.tensor_tensor(rk.ap, r_sb.ap, kp.ap, ALU.mult), reads=[r_sb.res, kp.res], writes=[rk.res])
                P.op("pool", E.tensor_tensor(rk.ap, rk.ap, vec.ap[:, 2, c0:c0 + 512], ALU.mult), reads=[rk.res, vec.res], writes=[rk.res])
                P.op("dve", E.tensor_reduce(sm.ap[:, 16:24], v3(rk.ap), AX.X, ALU.add), reads=[rk.res], writes=[sm.r("rk")])
                P.op("dve", E.tensor_tensor(v3(bonus.ap), v3(v_sb.ap), bc8(sm.ap[:, 16:24]), ALU.mult),
                     reads=[v_sb.res, sm.r("rk")], writes=[rk.res])
                if DBG["stage"] < 3:
                    continue
                bg = gbank()
                for q in range(4):
                    P.op("pe", E.matmul(bank(bg)[:, q * 2:q * 2 + 2], eL.ap[:, q * 128:(q + 1) * 128], sel63,
                                                              start=True, stop=True),
                         reads=[eL.res, cst.res], writes=[psb[bg]])
                P.op("dve", E.tensor_copy(gcol.ap[:, :, 0:4], bank(bg)[:, 0:8].rearrange("p (q c) -> p c q", c=2)),
                     reads=[psb[bg]], writes=[gcol.res])
                ops4 = [At, Bt, Kt, Rt]
                for oi in range(4):
                    for q in range(4):
                        bt = 6 + (oi // 2)
                        col = ((oi % 2) * 4 + q) * 128
                        P.op("pe", E.transpose(
                            bank(bt, BF16)[:, col:col + 128], ops4[oi].ap[:, q * 128:(q + 1) * 128], identb.ap),
                            reads=[ops4[oi].res, identb.res], writes=[psb[bt]])
                P.op("act", E.copy(FT.ap[:, 0:2, :, :], bank(6, BF16).rearrange("p (a b c) -> p a b c", a=2, b=4)),
                     reads=[psb[6]], writes=[FT.r(0)])
                P.op("dve", E.tensor_copy(FT.ap[:, 2:4, :, :], bank(7, BF16).rearrange("p (a b c) -> p a b c", a=2, b=4)),
                     reads=[psb[7]], writes=[FT.r(1)])
                FTr = [FT.r(0), FT.r(1)]
                for grp in range(2 if DBG["stage"] >= 4 else 0):
                    for hl in range(4):
                        q = grp * 2 + hl // 2
                        m = hl % 2
                        rows = slice(m * 64, m * 64 + 64)
                        bgm = 2 + m
                        hcol = q * 128 + m * 64
                        for gi, (lo, ro) in enumerate([(1, 0), (1, 3), (2, 0), (2, 3)]):
                            P.op("pe", E.matmul(
                                bank(bgm)[:, gi * 128:(gi + 1) * 128], FT.ap[rows, lo, q, :], FT.ap[rows, ro, q, :],
                                start=True, stop=True, tile_position=(m * 64, 0)),
                                reads=FTr, writes=[psb[bgm]])
                        P.op("pe", E.matmul(
                            bank(4 + m)[:, (hl // 2) * 128:(hl // 2 + 1) * 128], FT.ap[rows, 0, q, :], FT.ap[rows, 1, q, :],
                            start=True, stop=True, tile_position=(m * 64, 0)),
                            reads=FTr, writes=[psb[4 + m]])
                        P.op("dve", E.tensor_tensor(Gm.ap[:, hl, :], bank(bgm), maskG, ALU.mult),
                             reads=[psb[bgm], cst.res], writes=[Gm.r(hl)])
                    for m in range(2):
                        P.op("dve", E.tensor_tensor(
                            Pm[0].ap[:, 1, m::2, :], bank(4 + m)[:, 0:256].rearrange("p (a b) -> p a b", b=128),
                            maskNT.unsqueeze(1).to_broadcast([128, 2, 128]), ALU.mult),
                            reads=[psb[4 + m], cst.res], writes=[Pm[0].r(("t", m))])
                    P.op("pool", E.tensor_copy(Pm[0].ap[:, 0, :, :], Gm.ap[:, :, 0:128]),
                         reads=[Gm.r(h_) for h_ in range(4)], writes=[Pm[0].r("p")])
                    if DBG["stage"] < 5:
                        continue
                    bw = gbank()
                    for hl in range(4):
                        hcol = (grp * 2 + hl // 2) * 128 + (hl % 2) * 64
                        P.op("pe", E.matmul(
                            bank(bw)[:, hl * 64:(hl + 1) * 64], Gm.ap[:, hl, 256:384], Vt.ap[:, hcol:hcol + 64], start=True, stop=True),
                            reads=[Gm.r(hl), Vt.res], writes=[psb[bw]])
                    P.op("act", E.copy(X[0].ap[:, :, 64:128], bank(bw)[:, 0:256].rearrange("p (a b) -> p a b", b=64)),
                         reads=[psb[bw]], writes=[X[0].r("u")])
                    P.op("pool", E.tensor_copy(
                        X[0].ap[:, :, 0:64], At.ap[:, grp * 256:(grp + 1) * 256].rearrange("p (a b) -> p a b", b=64)),
                        reads=[At.res], writes=[X[0].r("a")])
                    Xr = lambda i: [X[i].r("u"), X[i].r("a")]
                    Pr = lambda i: [Pm[i].r("p"), Pm[i].r(("t", 0)), Pm[i].r(("t", 1))]
                    for lvl in range(6):
                        ci, ni = lvl % 2, (lvl + 1) % 2
                        for hl in range(4):
                            P.op("pe", E.matmul(
                                bank(2)[:, hl * 128:(hl + 1) * 128], Pm[ci].ap[:, 0, hl, :], X[ci].ap[:, hl, :], start=True, stop=True),
                                reads=Pr(ci) + Xr(ci), writes=[psb[2]])
                        P.op("dve", E.tensor_tensor(
                            X[ni].ap, bank(2).rearrange("p (a b) -> p a b", b=128), X[ci].ap, ALU.add),
                            reads=[psb[2]] + Xr(ci), writes=Xr(ni))
                        if lvl < 5:
                            for hl in range(4):
                                P.op("pe", E.matmul(
                                    bank(4)[:, hl * 128:(hl + 1) * 128], Pm[ci].ap[:, 1, hl, :], Pm[ci].ap[:, 0, hl, :], start=True, stop=True),
                                    reads=Pr(ci), writes=[psb[4]])
                                P.op("pe", E.matmul(
                                    bank(5)[:, hl * 128:(hl + 1) * 128], Pm[ci].ap[:, 0, hl, :], Pm[ci].ap[:, 1, hl, :], start=True, stop=True),
                                    reads=Pr(ci), writes=[psb[5]])
                            P.op("act", E.copy(Pm[ni].ap[:, 0, :, :], bank(4).rearrange("p (a b) -> p a b", b=128)),
                                 reads=[psb[4]], writes=[Pm[ni].r("p")])
                            P.op("act", E.copy(Pm[ni].ap[:, 1, :, :], bank(5).rearrange("p (a b) -> p a b", b=128)),
                                 reads=[psb[5]], writes=[Pm[ni].r(("t", 0)), Pm[ni].r(("t", 1))])
                    if DBG["stage"] < 6:
                        continue
                    XF = X[0]
                    XFr = Xr(0)
                    for hl in range(4):
                        q = grp * 2 + hl // 2
                        m = hl % 2
                        hcol = q * 128 + m * 64
                        for c in range(2):
                            rc = slice(c * 64, c * 64 + 64)
                            bmz = 6 + c
                            P.op("pe", E.matmul(
                                bank(bmz)[m * 64:m * 64 + 64, q * 64:q * 64 + 64], XF.ap[rc, hl, 0:64], Bt.ap[rc, hcol:hcol + 64],
                                start=True, stop=True, tile_position=(c * 64, m * 64)),
                                reads=XFr + [Bt.res], writes=[psb[bmz]])
                            P.op("pe", E.matmul(
                                bank(bmz)[m * 64:m * 64 + 64, 256 + q * 64:256 + q * 64 + 64], Bt.ap[rc, hcol:hcol + 64], XF.ap[rc, hl, 64:128],
                                start=True, stop=False, tile_position=(c * 64, m * 64)),
                                reads=XFr + [Bt.res], writes=[psb[bmz]])
                            P.op("pe", E.matmul(
                                bank(bmz)[m * 64:m * 64 + 64, 256 + q * 64:256 + q * 64 + 64], Kt.ap[rc, hcol:hcol + 64], Vt.ap[rc, hcol:hcol + 64],
                                start=False, stop=True, tile_position=(c * 64, m * 64)),
                                reads=[Kt.res, Vt.res], writes=[psb[bmz]])
                        if DBG["sub"] >= 2:
                          P.op("pe", E.matmul(
                            bank(3)[m * 64:m * 64 + 64, (hl // 2) * 128:(hl // 2) * 128 + 128], XF.ap[:, hl, 0:64], Gm.ap[:, hl, 128:256],
                            start=True, stop=True, tile_position=(0, m * 64)),
                            reads=XFr + [Gm.r(hl)], writes=[psb[3]])
                    qs = slice(grp * 2, grp * 2 + 2)
                    for c in range(2):
                        bmz = 6 + c
                        P.op("dve", E.tensor_tensor(
                            MT.ap[:, c, qs, :], bank(bmz)[:, grp * 128:grp * 128 + 128].rearrange("p (a b) -> p a b", b=64),
                            cst.ap[:, 832:896].unsqueeze(1).to_broadcast([128, 2, 64]), ALU.add),
                            reads=[psb[bmz], cst.res], writes=[MT.r((c, grp))])
                        P.op("act", E.copy(
                            Zs.ap[:, c, qs, :], bank(bmz)[:, 256 + grp * 128:256 + grp * 128 + 128].rearrange("p (a b) -> p a b", b=64)),
                            reads=[psb[bmz]], writes=[Zs.r((c, grp))])
                        for ql in range(2):
                            q = grp * 2 + ql
                            P.op("pool", E.tensor_tensor(
                                MTbd.ap[:, c, q, :, :], MT.ap[:, c, q, :].unsqueeze(1).to_broadcast([128, 2, 64]), mbd, ALU.mult),
                                reads=[MT.r((c, grp)), cst.res], writes=[MTbd.r((c, q))])
                            P.op("pool", E.tensor_tensor(
                                Zbd.ap[:, c, q, :, :], Zs.ap[:, c, q, :].unsqueeze(1).to_broadcast([128, 2, 64]), mbd, ALU.mult),
                                reads=[Zs.r((c, grp)), cst.res], writes=[Zbd.r((c, q))])
                    P.op("dve", E.tensor_tensor(
                        RhF.ap[:, qs, :], bank(3)[:, 0:256].rearrange("p (a b) -> p a b", b=128), FT.ap[:, 3, qs, :], ALU.add),
                        reads=[psb[3]] + FTr, writes=[RhF.r(grp)])
                    by2 = gbank()
                    for hl in range(4):
                        hcol = (grp * 2 + hl // 2) * 128 + (hl % 2) * 64
                        P.op("pe", E.matmul(
                            bank(by2)[:, hl * 64:(hl + 1) * 64], Gm.ap[:, hl, 128:256], XF.ap[:, hl, 64:128], start=True, stop=False),
                            reads=[Gm.r(hl)] + XFr, writes=[psb[by2]])
                        P.op("pe", E.matmul(
                            bank(by2)[:, hl * 64:(hl + 1) * 64], Gm.ap[:, hl, 384:512], Vt.ap[:, hcol:hcol + 64], start=False, stop=True),
                            reads=[Gm.r(hl), Vt.res], writes=[psb[by2]])
                    P.op("act", E.copy(Y2s.ap[:, grp * 256:(grp + 1) * 256], bank(by2)[:, 0:256]),
                         reads=[psb[by2]], writes=[Y2s.res])
                if DBG["stage"] < 7:
                    continue
                allq = [MTbd.r((c_, q_)) for c_ in range(2) for q_ in range(4)]
                for c in range(2):
                    for q in range(4):
                        P.op("pe", E.matmul(
                            bank(2)[c * 64:c * 64 + 64, q * 128:(q + 1) * 128], RhF.ap[:, q, c * 64:c * 64 + 64], Sb.ap[:, hf, q, :],
                            start=True, stop=True, tile_position=(0, c * 64)),
                            reads=[RhF.r(0), RhF.r(1), Sb.r(hf)], writes=[psb[2]])
                    for q in range(4):
                        P.op("pe", E.matmul(
                            bank(3)[:, q * 128:(q + 1) * 128], MTbd.ap[:, c, q, :, :].rearrange("p a b -> p (a b)"), Sb.ap[:, hf, q, :],
                            start=True, stop=True),
                            reads=[MTbd.r((c, q)), Sb.r(hf)], writes=[psb[3]])
                    P.op("dve", E.tensor_tensor(
                        S.ap[:, hf, :, :], bank(3).rearrange("p (a b) -> p a b", b=128),
                        Zbd.ap[:, c, :, :, :].rearrange("p q a b -> p q (a b)"), ALU.add),
                        reads=[psb[3]] + [Zbd.r((c, q_)) for q_ in range(4)], writes=[S.r(hf)])
                    P.op("dve", E.tensor_tensor(
                        S.ap[:, hf, :, :], S.ap[:, hf, :, :], gcol.ap[:, c, 0:4].unsqueeze(2).to_broadcast([128, 4, 128]), ALU.mult),
                        reads=[S.r(hf), gcol.res], writes=[S.r(hf)])
                    P.op("act", E.copy(Sb.ap[:, hf, :, :], S.ap[:, hf, :, :]), reads=[S.r(hf)], writes=[Sb.r(hf)])
                P.op("dve", E.tensor_tensor(y_sb.ap, bank(2), Y2s.ap, ALU.add),
                     reads=[psb[2], Y2s.res], writes=[y_sb.res])
                if DBG["stage"] < 8:
                    continue
                yres = [y_sb.res]
                P.op("dve", E.tensor_reduce(sm.ap[:, 32:40], v3(y_sb.ap), AX.X, ALU.add), reads=yres, writes=[sm.r("m")])
                P.op("dve", E.tensor_scalar_mul(sm.ap[:, 32:40], sm.ap[:, 32:40], 1.0 / 64), reads=[sm.r("m")], writes=[sm.r("m")])
                P.op("dve", E.tensor_tensor(v3(yc.ap), v3(y_sb.ap), bc8(sm.ap[:, 32:40]), ALU.subtract),
                     reads=yres + [sm.r("m")], writes=[yc.res])
                P.op("pool", E.tensor_tensor(sq.ap, yc.ap, yc.ap, ALU.mult), reads=[yc.res], writes=[sq.res])
                P.op("dve", E.tensor_reduce(sm.ap[:, 48:56], v3(sq.ap), AX.X, ALU.add), reads=[sq.res], writes=[sm.r("v")])
                P.op("act", E.activation(sm.ap[:, 48:56], sm.ap[:, 48:56], AF.Sqrt, bias=GN_EPS, scale=1.0 / 64),
                     reads=[sm.r("v")], writes=[sm.r("v")])
                P.op("dve", E.reciprocal(sm.ap[:, 48:56], sm.ap[:, 48:56]), reads=[sm.r("v")], writes=[sm.r("v")])
                P.op("dve", E.tensor_tensor(v3(yc.ap), v3(yc.ap), bc8(sm.ap[:, 48:56]), ALU.mult),
                     reads=[yc.res, sm.r("v")], writes=[yc.res])
                P.op("pool", E.tensor_tensor(yc.ap, yc.ap, vec.ap[:, 3, c0:c0 + 512], ALU.mult), reads=[yc.res, vec.res], writes=[yc.res])
                P.op("pool", E.tensor_tensor(yc.ap, yc.ap, vec.ap[:, 4, c0:c0 + 512], ALU.add), reads=[yc.res, vec.res], writes=[yc.res])
                P.op("pool", E.tensor_tensor(yc.ap, yc.ap, bonus.ap, ALU.add), reads=[yc.res, rk.res], writes=[yc.res])
                P.op("pool", E.tensor_tensor(At.ap, yc.ap, g_sb.ap, ALU.mult), reads=[yc.res, g_sb.res, At.res], writes=[At.res])
                P.dma("pool", self.ogdram[s, t0:t0 + 128, c0:c0 + 512], At.ap, reads=[At.res], writes=[self.og_res[s][tile]])
        self.phase_reset()
        if DBG["stage"] < 9:
            return
        Wo = self.sb([128, DC, D], BF16, "Wo")
        P.dma("pool", Wo.ap, self.rw_wout[j].rearrange("(k p) n -> p k n", p=128), writes=[Wo.res])
        self.out_proj_ln(L, s, Wo, self.ogdram[s], self.og_res[s], x_src, x_dst, last)
        self.phase_reset()

    def out_proj_ln(self, L, s, Wo, og, og_res, x_src, x_dst, last):
        P = self.P
        bank = self.bank
        psb = self.psb
        self.xring = [self.sb([128, D], F32, "xr%d" % i) for i in range(4)]
        self.load_ln_consts(L, s, 0, need_z=True)
        ogt = [self.sb([128, D], BF16, "ogt%d" % i) for i in range(2)]
        ogT = [self.sb([128, DC, 128], BF16, "ogT%d" % i) for i in range(2)]
        for tile in range(NT):
            t0 = tile * 128
            o, oT = ogt[tile % 2], ogT[tile % 2]
            P.dma("sp", o.ap, og[t0:t0 + 128, :], reads=[og_res[tile]], writes=[o.res])
            bt = 4 + tile % 2
            for dc in range(DC):
                P.op("pe", E.transpose(bank(bt, BF16)[:, dc * 128:(dc + 1) * 128], o.ap[:, dc * 128:(dc + 1) * 128], self.identb.ap),
                     reads=[o.res, self.identb.res], writes=[psb[bt]])
            P.op("act", E.copy(oT.ap, bank(bt, BF16).rearrange("p (a b) -> p a b", b=128)),
                 reads=[psb[bt]], writes=[oT.res])
            b0 = (tile % 2) * 2
            for half in range(2):
                for dc in range(DC):
                    P.op("pe", E.matmul(
                        bank(b0 + half), oT.ap[:, dc, :], Wo.ap[:, dc, half * 512:(half + 1) * 512], start=(dc == 0), stop=(dc == DC - 1)),
                        reads=[oT.res, Wo.res], writes=[psb[b0 + half]])
            self.ln_epilogue(s, tile, [(bank(b0), 0, 512), (bank(b0 + 1), 512, 512)], [psb[b0], psb[b0 + 1]],
                             x_src, x_dst, dst_res=(self.outres if last else None))

    def dsa(self, L, s, x_src, x_dst, last=False):
        P = self.P
        j = L // 2
        bank = self.bank
        psb = self.psb
        identb = self.identb
        SCL = 0.125
        WSC = 1.0 / math.sqrt(8 * 64)
        win = self.dsa_w_in[j]
        ldw = lambda t, ap: P.dma("pool", t.ap, ap, writes=[t.res])
        Wq = self.sb([128, DC, 1024], BF16, "Wq")
        Wc = self.sb([128, DC, 128], BF16, "Wc")
        Wqi = self.sb([128, DC, 512], BF16, "Wqi")
        Wki = self.sb([128, DC, 64], BF16, "Wki")
        Wwi = self.sb([128, DC, 8], BF16, "Wwi")
        for t_, c0, c1 in [(Wq, 0, 1024), (Wc, 1024, 1152), (Wqi, 1152, 1664), (Wki, 1664, 1728), (Wwi, 1728, 1736)]:
            ldw(t_, win[:, c0:c1].rearrange("(k p) n -> p k n", p=128))
        wuk = self.sb([64, 16, 128], BF16, "wuk")
        ldw(wuk, self.dsa_w_uk[j].rearrange("h d c -> d h c"))
        wuv = self.sb([128, 16, 64], BF16, "wuv")
        ldw(wuv, self.dsa_w_uv[j].rearrange("h c d -> c h d"))
        gkv = self.sb([128, 128], F32, "gkv")
        P.dma("sp", gkv.ap, self.dsa_kv_norm[j:j + 1, :].broadcast_to([128, 128]), writes=[gkv.res])
        caus = self.sb([128, 4, 512], F32, "caus")
        P.dma("sp", caus.ap, self.dsa_caus, writes=[caus.res])
        braw = self.sb([128, 16, 256], F32, "braw")
        c31 = self.sb([128, 16], F32, "c31")
        band = self.sb([128, 16, 256], BF16, "band")
        P.dma("sp", braw.ap, self.dsa_band, writes=[braw.res])
        P.dma("sp", c31.ap, self.dsa_c31, writes=[c31.res])
        P.op("dve", E.tensor_tensor(band.ap, braw.ap, c31.ap.unsqueeze(2).to_broadcast([128, 16, 256]), ALU.subtract),
             reads=[braw.res, c31.res], writes=[band.res])
        ones = self.sb([128, 128], BF16, "ones")
        P.op("pool", E.memset(ones.ap, 1.0), writes=[ones.res])
        half = self.sb([128, 1], F32, "half")
        P.op("pool", E.memset(half.ap, 0.5), writes=[half.res])
        ckvT = self.sb([128, NT, 128], BF16, "ckvT")
        ckvF = self.sb([128, T], BF16, "ckvF")
        kiT = self.sb([64, T], BF16, "kiT")
        hT = self.sb([128, DC, T], BF16, "hT")
        ring_off = self.off
        self.xring = [self.sb([128, D], F32, "xr%d" % i) for i in range(4)]
        self.make_hT(x_src, L, s, 0, 1, hT)
        P.barrier()
        self.off = ring_off
        nb = [0]

        def gbank():
            nb[0] ^= 1
            return nb[0]
        ck = self.sb([128, 128], F32, "ck")
        cs = self.sb([128, 8], F32, "cs")
        for tile in range(NT):
            b = gbank()
            for dc in range(DC):
                P.op("pe", E.matmul(bank(b)[:, 0:128], hT.ap[:, dc, tile * 128:(tile + 1) * 128], Wc.ap[:, dc, :],
                                    start=(dc == 0), stop=(dc == DC - 1)), reads=[hT.r(tile // 4), Wc.res], writes=[psb[b]])
            P.op("act", E.activation(ck.ap, bank(b)[:, 0:128], AF.Square, accum_out=cs.ap[:, 0:1]), reads=[psb[b]], writes=[ck.res, cs.res])
            P.op("act", E.activation(cs.ap[:, 0:1], cs.ap[:, 0:1], AF.Sqrt, bias=1e-6, scale=1.0 / 128), reads=[cs.res], writes=[cs.res])
            P.op("dve", E.reciprocal(cs.ap[:, 0:1], cs.ap[:, 0:1]), reads=[cs.res], writes=[cs.res])
            P.op("dve", E.scalar_tensor_tensor(ckvT.ap[:, tile, :], bank(b)[:, 0:128], cs.ap[:, 0:1], gkv.ap, ALU.mult, ALU.mult),
                 reads=[psb[b], cs.res, gkv.res], writes=[ckvT.r(tile)])
            b2 = gbank()
            P.op("pe", E.transpose(bank(b2, BF16)[:, 0:128], ckvT.ap[:, tile, :], identb.ap), reads=[ckvT.r(tile), identb.res], writes=[psb[b2]])
            P.op("act", E.copy(ckvF.ap[:, tile * 128:(tile + 1) * 128], bank(b2, BF16)[:, 0:128]), reads=[psb[b2]], writes=[ckvF.r(tile)])
        for tb in range(4):
            b = gbank()
            for dc in range(DC):
                P.op("pe", E.matmul(bank(b)[0:64, :], Wki.ap[:, dc, :], hT.ap[:, dc, tb * 512:(tb + 1) * 512],
                                    start=(dc == 0), stop=(dc == DC - 1)), reads=[hT.r(tb), Wki.res], writes=[psb[b]])
            P.op("act", E.copy(kiT.ap[:, tb * 512:(tb + 1) * 512], bank(b)[0:64, :]), reads=[psb[b]], writes=[kiT.r(tb)])
        qTh = self.sb([64, 16, 512], BF16, "qTh")
        qlat = self.sb([128, 16, 512], BF16, "qlat")
        qiT = self.sb([64, 8, 512], BF16, "qiT")
        widx = self.sb([128, 4, 8], F32, "widx")
        acc = self.sb([128, T], F32, "acc")
        junk = self.sb([128, T], F32, "junk")
        relu = [self.sb([128, 512], F32, "relu%d" % i) for i in range(2)]
        mb = self.sb([128, T], BF16, "mb")
        mT = self.sb([128, NT, 128], BF16, "mT")
        eT = [self.sb([128, 4, 128], BF16, "eT%d" % i) for i in range(2)]
        olTn = self.sb([128, 4, 128], BF16, "olTn")
        rinv = self.sb([128, 512], F32, "rinv")
        ogt = self.sb([128, D], BF16, "ogt")
        bs = self.sb([128, 16], F32, "bs")
        ntl = min(NT, DBG["tiles"])
        for tg in range(4):
            if tg * 4 >= ntl:
                break
            tsl = slice(tg * 512, (tg + 1) * 512)
            for h in range(16):
                b = gbank()
                for dc in range(DC):
                    P.op("pe", E.matmul(bank(b)[0:64, :], Wq.ap[:, dc, h * 64:(h + 1) * 64], hT.ap[:, dc, tsl],
                                        start=(dc == 0), stop=(dc == DC - 1)), reads=[hT.r(tg), Wq.res], writes=[psb[b]])
                P.op(self.ev_eng(), E.copy(qTh.ap[:, h, :], bank(b)[0:64, :]) if self.rr["ev"] == 0 else E.tensor_copy(qTh.ap[:, h, :], bank(b)[0:64, :]),
                     reads=[psb[b]], writes=[qTh.r(h)])
            for h in range(16):
                b = gbank()
                P.op("pe", E.matmul(bank(b), wuk.ap[:, h, :], qTh.ap[:, h, :], start=True, stop=True, tile_position=(0, 0)),
                     reads=[wuk.res, qTh.r(h)], writes=[psb[b]])
                P.op("act", E.activation(qlat.ap[:, h, :], bank(b), AF.Identity, scale=SCL), reads=[psb[b]], writes=[qlat.r(h)])
            for hi in range(8):
                b = gbank()
                for dc in range(DC):
                    P.op("pe", E.matmul(bank(b)[0:64, :], Wqi.ap[:, dc, hi * 64:(hi + 1) * 64], hT.ap[:, dc, tsl],
                                        start=(dc == 0), stop=(dc == DC - 1)), reads=[hT.r(tg), Wqi.res], writes=[psb[b]])
                P.op("dve", E.tensor_copy(qiT.ap[:, hi, :], bank(b)[0:64, :]), reads=[psb[b]], writes=[qiT.r(hi)])
            for tt in range(4):
                b = gbank()
                tile = tg * 4 + tt
                for dc in range(DC):
                    P.op("pe", E.matmul(bank(b)[:, 0:8], hT.ap[:, dc, tile * 128:(tile + 1) * 128], Wwi.ap[:, dc, :],
                                        start=(dc == 0), stop=(dc == DC - 1)), reads=[hT.r(tg), Wwi.res], writes=[psb[b]])
                P.op("act", E.activation(widx.ap[:, tt, :], bank(b)[:, 0:8], AF.Identity, scale=WSC), reads=[psb[b]], writes=[widx.r(tt)])
            qlr = [qlat.r(h) for h in range(16)]
            qir = [qiT.r(h) for h in range(8)]
            for tt in range(4):
                tile = tg * 4 + tt
                if tile >= ntl:
                    break
                nsb = tile // 4 + 1
                ncol = nsb * 512
                tcs = slice(tt * 128, (tt + 1) * 128)
                nr = 0
                for sbk in range(nsb):
                    for hi in range(8):
                        b = gbank()
                        P.op("pe", E.matmul(bank(b), qiT.ap[:, hi, tcs], kiT.ap[:, sbk * 512:(sbk + 1) * 512], start=True, stop=True,
                                            tile_position=(0, 0)), reads=[qiT.r(hi), kiT.r(sbk)], writes=[psb[b]])
                        rl = relu[nr % 2]
                        nr += 1
                        P.op("act", E.activation(rl.ap, bank(b), AF.Relu), reads=[psb[b]], writes=[rl.res])
                        dst = acc.ap[:, sbk * 512:(sbk + 1) * 512]
                        if hi == 0:
                            P.op("dve", E.tensor_scalar(dst, rl.ap, widx.ap[:, tt, 0:1], None, ALU.mult), reads=[rl.res, widx.r(tt)], writes=[acc.r(sbk)])
                        else:
                            P.op("dve", E.scalar_tensor_tensor(dst, rl.ap, widx.ap[:, tt, hi:hi + 1], dst, ALU.mult, ALU.add),
                                 reads=[rl.res, widx.r(tt), acc.r(sbk)], writes=[acc.r(sbk)])
                accr = [acc.r(k_) for k_ in range(nsb)]
                if tile >= 2:
                    P.op("dve", E.tensor_reduce(bs.ap[:, 0:1], acc.ap[:, 0:ncol], AX.X, ALU.min), reads=accr, writes=[bs.res])
                    P.op("dve", E.tensor_reduce(bs.ap[:, 1:2], acc.ap[:, 0:ncol], AX.X, ALU.max), reads=accr, writes=[bs.res])
                    P.op("dve", E.tensor_scalar_add(bs.ap[:, 0:1], bs.ap[:, 0:1], -1e-3), reads=[bs.res], writes=[bs.res])
                    P.op("dve", E.tensor_scalar_add(bs.ap[:, 1:2], bs.ap[:, 1:2], 1e-3), reads=[bs.res], writes=[bs.res])
                lastb = acc.ap[:, (nsb - 1) * 512:nsb * 512]
                P.op("dve", E.tensor_tensor(lastb, lastb, caus.ap[:, tile % 4, :], ALU.add), reads=accr + [caus.res, bs.res], writes=[acc.r(nsb - 1)])
                if tile >= 2:
                    P.op("dve", E.tensor_tensor(bs.ap[:, 6:7], bs.ap[:, 1:2], bs.ap[:, 0:1], ALU.subtract), reads=[bs.res], writes=[bs.res])
                    for it in range(14):
                        P.op("dve", E.tensor_scalar(bs.ap[:, 7:8], bs.ap[:, 6:7], 0.5 ** (it + 1), None, ALU.mult), reads=[bs.res], writes=[bs.res])
                        P.op("dve", E.tensor_tensor(bs.ap[:, 2:3], bs.ap[:, 0:1], bs.ap[:, 7:8], ALU.add), reads=[bs.res], writes=[bs.res])
                        P.op("dve", E.tensor_scalar(junk.ap[:, 0:ncol], acc.ap[:, 0:ncol], bs.ap[:, 2:3], 0.0, ALU.is_ge, ALU.add, accum_out=bs.ap[:, 3:4]),
                             reads=accr + [bs.res], writes=[junk.res, bs.res])
                        P.op("dve", E.scalar_tensor_tensor(bs.ap[:, 5:6], bs.ap[:, 3:4], 256.0, bs.ap[:, 7:8], ALU.is_ge, ALU.mult),
                             reads=[bs.res], writes=[bs.res])
                        P.op("dve", E.tensor_tensor(bs.ap[:, 0:1], bs.ap[:, 0:1], bs.ap[:, 5:6], ALU.add), reads=[bs.res], writes=[bs.res])
                else:
                    P.op("dve", E.memset(bs.ap[:, 0:1], -1e29), reads=[bs.res], writes=[bs.res])
                P.op("dve", E.tensor_scalar(mb.ap[:, 0:ncol], acc.ap[:, 0:ncol], bs.ap[:, 0:1], -30000.0, ALU.is_lt, ALU.mult),
                     reads=accr + [bs.res], writes=[mb.res])
                for sg in range(tile + 1):
                    b = gbank()
                    P.op("pe", E.transpose(bank(b, BF16)[:, 0:128], mb.ap[:, sg * 128:(sg + 1) * 128], identb.ap), reads=[mb.res, identb.res], writes=[psb[b]])
                    P.op("act", E.copy(mT.ap[:, sg, :], bank(b, BF16)[:, 0:128]), reads=[psb[b]], writes=[mT.r(sg)])
                ne = 0
                for hg in range(4):
                    bo = 4 + hg % 2
                    br = 6 + hg % 2
                    for sg in range(tile + 1):
                        bl = 2 + ne % 2
                        e_ = eT[ne % 2]
                        ne += 1
                        for hh in range(4):
                            h = hg * 4 + hh
                            reg = bank(bl)[:, hh * 128:(hh + 1) * 128]
                            wband = (sg == tile) or (sg == tile - 1)
                            P.op("pe", E.matmul(reg, ckvF.ap[:, sg * 128:(sg + 1) * 128], qlat.ap[:, h, tcs], start=True, stop=False),
                                 reads=[ckvF.r(sg), qlat.r(h)], writes=[psb[bl]])
                            P.op("pe", E.matmul(reg, identb.ap, mT.ap[:, sg, :], start=False, stop=(not wband)),
                                 reads=[identb.res, mT.r(sg)], writes=[psb[bl]])
                            if wband:
                                w_ = 0 if sg == tile else 1
                                P.op("pe", E.matmul(reg, identb.ap, band.ap[:, h, w_ * 128:(w_ + 1) * 128], start=False, stop=True),
                                     reads=[identb.res, band.res], writes=[psb[bl]])
                        P.op("act", E.activation(e_.ap, bank(bl).rearrange("p (a b) -> p a b", b=128), AF.Exp), reads=[psb[bl]], writes=[e_.res])
                        e2 = e_.ap.rearrange("p a b -> p (a b)")
                        P.op("pe", E.matmul(bank(bo), ckvT.ap[:, sg, :], e2, start=(sg == 0), stop=(sg == tile)),
                             reads=[ckvT.r(sg), e_.res], writes=[psb[bo]])
                        P.op("pe", E.matmul(bank(br), ones.ap, e2, start=(sg == 0), stop=(sg == tile)),
                             reads=[ones.res, e_.res], writes=[psb[br]])
                    P.op("dve", E.reciprocal(rinv.ap, bank(br)), reads=[psb[br]], writes=[rinv.res])
                    P.op("dve", E.tensor_tensor(olTn.ap.rearrange("p a b -> p (a b)"), bank(bo), rinv.ap, ALU.mult),
                         reads=[psb[bo], rinv.res], writes=[olTn.res])
                    for hh in range(4):
                        h = hg * 4 + hh
                        bq = gbank()
                        P.op("pe", E.matmul(bank(bq)[:, 0:64], olTn.ap[:, hh, :], wuv.ap[:, h, :], start=True, stop=True),
                             reads=[olTn.res, wuv.res], writes=[psb[bq]])
                        P.op("act", E.copy(ogt.ap[:, h * 64:(h + 1) * 64], bank(bq)[:, 0:64]), reads=[psb[bq]], writes=[ogt.res])
                P.dma("sp", self.ogdram[s, tile * 128:(tile + 1) * 128, :], ogt.ap, reads=[ogt.res], writes=[self.og_res[s][tile]])
        self.phase_reset()
        Wo = self.sb([128, DC, D], BF16, "Wo")
        P.dma("pool", Wo.ap, self.dsa_w_out[j].rearrange("(k p) n -> p k n", p=128), writes=[Wo.res])
        self.out_proj_ln(L, s, Wo, self.ogdram[s], self.og_res[s], x_src, x_dst, last)
        self.phase_reset()

    def build(self):
        P = self.P
        self.prologue()
        for s in range(NSEQ):
            src = self.x_in[s]
            nsub = len(self.plan)
            for i, (kind, L) in enumerate(self.plan):
                last = (i == nsub - 1)
                dst = self.out[s] if last else self.xbuf[s]
                if kind == "ffn":
                    self.ffn(L, s, src, dst, last)
                elif kind == "rwkv":
                    self.rwkv(L, s, src, dst, last)
                elif kind == "dsa":
                    self.dsa(L, s, src, dst, last)
                src = self.xbuf[s]
        P.emit()
        self.es.close()
        return self.nc


FULL_PLAN = [("rwkv", 0), ("ffn", 0), ("dsa", 1), ("ffn", 1), ("rwkv", 2), ("ffn", 2), ("dsa", 3), ("ffn", 3)]


def make_consts():
    c = np.zeros((128, NCONST), np.float32)
    s_ = np.arange(128)[:, None]
    t_ = np.arange(128)[None, :]
    same = (s_ // 64) == (t_ // 64)
    su = (same & (s_ < t_)).astype(np.float32)
    iu = (same & (s_ <= t_)).astype(np.float32)
    c[:, 0:128] = su
    c[:, 128:256] = iu
    c[:, 256:384] = su
    c[:, 384:512] = iu
    c[:, 512:640] = su.T
    c[:, 640:768] = iu
    c[63, 768] = 1.0
    c[127, 769] = 1.0
    c[:, 832:896] = np.tile(np.eye(64, dtype=np.float32), (2, 1))
    c[0:64, 896:960] = 1.0
    c[64:128, 960:1024] = 1.0
    return c


def host_inputs(inputs, plan):
    f = lambda a: np.ascontiguousarray(np.asarray(a, dtype=np.float32))
    common = {
        "ada_w": f(inputs["ada_w"]), "ada_b": f(inputs["ada_b"]),
        "ln_g": f(inputs["ln_g"]), "ln_b": f(inputs["ln_b"]),
        "ffn_w_in": f(inputs["ffn_w_in"]), "ffn_w_out": f(inputs["ffn_w_out"]),
        "ident_f": np.eye(128, dtype=np.float32),
        "consts": make_consts(),
    }
    kinds = set(k for k, _ in plan)
    if "dsa" in kinds:
        for k_ in ("dsa_w_in", "dsa_kv_norm", "dsa_w_uk", "dsa_w_uv", "dsa_w_out"):
            common[k_] = f(inputs[k_])
        rb = f(inputs["rel_bias"])
        tt_ = np.arange(128)[None, :]; ss_ = np.arange(128)[:, None]
        caus = np.zeros((128, 4, 512), np.float32)
        for q4 in range(4):
            cc = np.arange(512)[None, :]
            caus[:, q4, :] = np.where(cc <= q4 * 128 + np.arange(128)[:, None], 0.0, -1e30)
        common["dsa_caus"] = caus
        band = np.zeros((128, 16, 2, 128), np.float32)
        for w_ in range(2):
            n = np.maximum(tt_ - ss_ + 128 * w_, 0)
            nf = np.maximum(n, 1).astype(np.float32)
            large = 16 + (np.log(nf / 16) / math.log(128 / 16) * 16).astype(np.int32)
            bucket = np.where(n < 16, n, np.minimum(large, 31))
            band[:, :, w_, :] = rb[bucket].transpose(0, 2, 1)
        common["dsa_band"] = np.ascontiguousarray(band.reshape(128, 16, 256))
        common["dsa_c31"] = np.ascontiguousarray(np.broadcast_to(rb[31][None, :], (128, 16)))
    if "rwkv" in kinds:
        mu = f(inputs["rwkv_mu"])
        common["rw_mu"] = np.ascontiguousarray(mu.reshape(2, 6, DC, 128).transpose(0, 3, 1, 2))
        common["rwkv_w_rkv"] = f(inputs["rwkv_w_rkv"])
        common["rwkv_w1"] = f(inputs["rwkv_w1"])
        common["rw_w2a"] = np.concatenate([f(inputs["rwkv_w2"]), f(inputs["rwkv_w0"])[:, None, :]], axis=1)
        common["rwkv_a1"] = f(inputs["rwkv_a1"])
        common["rw_a2a"] = np.concatenate([f(inputs["rwkv_a2"]), f(inputs["rwkv_a0"])[:, None, :]], axis=1)
        common["rwkv_v1"] = f(inputs["rwkv_v1"])
        common["rw_v2a"] = np.concatenate([f(inputs["rwkv_v2"]), f(inputs["rwkv_v0"])[:, None, :]], axis=1)
        common["rwkv_g1"] = f(inputs["rwkv_g1"])
        common["rwkv_g2"] = f(inputs["rwkv_g2"])
        common["rw_vec"] = np.stack([f(inputs["rwkv_k_k"]), f(inputs["rwkv_k_a"]), f(inputs["rwkv_r_k"]).reshape(2, D),
                                     f(inputs["rwkv_lnx_g"]), f(inputs["rwkv_lnx_b"])], axis=1)
        common["rwkv_w_out"] = f(inputs["rwkv_w_out"])
    x = f(inputs["x"])
    c = f(inputs["c"])
    maps = []
    for core in range(8):
        m = dict(common)
        m["x"] = x[core * NSEQ:(core + 1) * NSEQ]
        cc = c[core * NSEQ:(core + 1) * NSEQ]
        m["c_col"] = np.ascontiguousarray(cc.reshape(NSEQ, DC, 128).transpose(0, 2, 1))
        maps.append(m)
    return maps


def run(inputs, plan=FULL_PLAN, ncores=8):
    b = Builder(plan)
    nc = b.build()
    print("instr stats", b.P.stats())
    maps = host_inputs(inputs, plan)[:ncores]
    res = run_bass_kernel_spmd(nc, maps, core_ids=list(range(ncores)))
    if DBG.get("dump"):
        np.save("_dbg_og.npy", np.asarray(res.results[0]["ogdram"]).astype(np.float32))
    return np.concatenate([r["out"] for r in res.results], axis=0)


def kernel(**inputs):
    return run(inputs, FULL_PLAN)
```
